# Optimizing a Trainium2 kernel written in Bass

```python
import jax, jax.numpy as jnp
from jax import lax
import numpy as np

D_MODEL = 2048
BATCH = 1
SEQ = 8192
DEPTH = 1

CHUNK = 64
Q_BLOCK = 128
HEAD_DIM = 128
FOX_HEADS = 8
FORGET_BIAS_INIT = 2.0
DSA_HEADS = 8
DSA_KV_HEADS = 2
IDX_HEADS = 16
IDX_DIM = 64
TOPK_MAX = 256
ROPE_THETA = 500000.0
ROT_FRACTION_DEN = 4
MIX_A = FOX_HEADS * HEAD_DIM
MIX_B = DSA_HEADS * HEAD_DIM
IN_SIZES = (MIX_A, MIX_A, MIX_A, FOX_HEADS,
            MIX_B, DSA_KV_HEADS * HEAD_DIM, DSA_KV_HEADS * HEAD_DIM,
            IDX_HEADS * IDX_DIM, IDX_DIM, IDX_HEADS,
            D_MODEL, D_MODEL)
D_IN = sum(IN_SIZES)
N_EXPERTS = 32
TOP_K = 4
D_FF = D_MODEL
SWIGLU_ALPHA = 1.702
SWIGLU_LIMIT = 7.0
MOE_BLOCK = 128
LN_EPS = 1e-5

kernel_name = "hybrid_fox_dsa_moe_deepnorm"


def layer_norm(x, g, b):
    xf = x.astype(jnp.float32)
    mu = jnp.mean(xf, axis=-1, keepdims=True)
    var = jnp.mean(jnp.square(xf - mu), axis=-1, keepdims=True)
    y = (xf - mu) * lax.rsqrt(var + LN_EPS) * g.astype(jnp.float32) + b.astype(jnp.float32)
    return y.astype(x.dtype)


def rope_tables(seq, rot_dim):
    pos = jnp.arange(seq, dtype=jnp.float32)
    inv = jnp.power(ROPE_THETA, -jnp.arange(0, rot_dim, 2, dtype=jnp.float32) / rot_dim)
    ang = pos[:, None] * inv[None, :]
    return jnp.cos(ang), jnp.sin(ang)


def partial_rope(x, cos, sin):
    half = cos.shape[-1]
    xf = x.astype(jnp.float32)
    x1, x2, rest = xf[..., :half], xf[..., half:2 * half], xf[..., 2 * half:]
    c, s = cos[None, :, None, :], sin[None, :, None, :]
    out = jnp.concatenate([x1 * c - x2 * s, x2 * c + x1 * s, rest], axis=-1)
    return out.astype(x.dtype)


def to_blocks(a):
    b, s = a.shape[:2]
    a = a.reshape((b, s // Q_BLOCK, Q_BLOCK) + a.shape[2:])
    return jnp.moveaxis(a, 1, 0)


def from_blocks(a):
    a = jnp.moveaxis(a, 0, 1)
    return a.reshape((a.shape[0], a.shape[1] * a.shape[2]) + a.shape[3:])


def forgetting_attention(q, k, v, f_logit, b_forget):
    S, d = q.shape[1], q.shape[-1]
    scale = d ** -0.5
    log_f = jax.nn.log_sigmoid(f_logit.astype(jnp.float32) + b_forget.astype(jnp.float32))
    c = jnp.cumsum(log_f, axis=1)
    c_k = jnp.transpose(c, (0, 2, 1))
    pos = jnp.arange(S)

    def block(args):
        qb, cqb, tb = args
        logits = jnp.einsum('bthd,bshd->bhts', qb, k).astype(jnp.float32) * scale
        logits = logits + jnp.transpose(cqb, (0, 2, 1))[..., None] - c_k[:, :, None, :]
        mask = pos[None, :] <= tb[:, None]
        logits = jnp.where(mask[None, None], logits, -jnp.inf)
        p = jax.nn.softmax(logits, axis=-1).astype(v.dtype)
        return jnp.einsum('bhts,bshd->bthd', p, v)

    out = lax.map(block, (to_blocks(q), to_blocks(c), pos.reshape(-1, Q_BLOCK)))
    return from_blocks(out)


def indexed_sparse_attention(q, k, v, iq, ik, iw):
    B, S, Hq, d = q.shape
    Hkv = k.shape[2]
    G = Hq // Hkv
    n_sel = min(TOPK_MAX, S // 4)
    scale = d ** -0.5
    idx_scale = (IDX_HEADS ** -0.5) * (IDX_DIM ** -0.5)
    pos = jnp.arange(S)
    chunk_id = pos // CHUNK
    gather = jax.vmap(lambda arr, ids: arr[ids])

    def block(args):
        qb, iqb, iwb, tb = args
        cq = tb // CHUNK
        rel = jax.nn.relu(jnp.einsum('bthe,bse->bths', iqb, ik).astype(jnp.float32))
        score = jnp.einsum('bths,bth->bts', rel, iwb.astype(jnp.float32) * idx_scale)
        adm = chunk_id[None, :] <= cq[:, None]
        score = jnp.where(adm[None], score, -jnp.inf)
        _, sel = lax.top_k(score, n_sel)
        kg = gather(k, sel)
        vg = gather(v, sel)
        valid = chunk_id[sel] <= cq[None, :, None]
        qg = qb.reshape(B, Q_BLOCK, Hkv, G, d)
        logits = jnp.einsum('btjgd,btnjd->btjgn', qg, kg).astype(jnp.float32) * scale
        logits = jnp.where(valid[:, :, None, None, :], logits, -jnp.inf)
        p = jax.nn.softmax(logits, axis=-1).astype(v.dtype)
        o = jnp.einsum('btjgn,btnjd->btjgd', p, vg)
        return o.reshape(B, Q_BLOCK, Hq, d)

    out = lax.map(block, (to_blocks(q), to_blocks(iq), to_blocks(iw), pos.reshape(-1, Q_BLOCK)))
    return from_blocks(out)


def moe_ffn(h, w_router, b_router, w_gate_up, b_gate_up, w_down, b_down):
    B, S, D = h.shape
    N = B * S
    hf = h.reshape(N, D)
    logits = (hf @ w_router).astype(jnp.float32) + b_router.astype(jnp.float32)
    top_vals, top_idx = lax.top_k(logits, TOP_K)
    gates = jax.nn.softmax(top_vals, axis=-1)
    nk = N * TOP_K
    e_flat = top_idx.reshape(nk)
    tok_flat = jnp.arange(nk, dtype=jnp.int32) // TOP_K
    g_flat = gates.reshape(nk)
    order = jnp.argsort(e_flat)
    e_s, tok_s, g_s = e_flat[order], tok_flat[order], g_flat[order]
    counts = jnp.bincount(e_flat, length=N_EXPERTS)
    starts = jnp.cumsum(counts) - counts
    padded = (counts + MOE_BLOCK - 1) // MOE_BLOCK * MOE_BLOCK
    pend = jnp.cumsum(padded)
    pstart = pend - padded
    dest = pstart[e_s] + jnp.arange(nk) - starts[e_s]
    n_blocks = (nk + MOE_BLOCK - 1) // MOE_BLOCK + N_EXPERTS
    n_slots = n_blocks * MOE_BLOCK
    slot_tok = jnp.zeros((n_slots,), jnp.int32).at[dest].set(tok_s)
    slot_gate = jnp.zeros((n_slots,), jnp.float32).at[dest].set(g_s)
    block_expert = jnp.minimum(
        jnp.searchsorted(pend, jnp.arange(n_blocks) * MOE_BLOCK, side='right'), N_EXPERTS - 1)
    xs = hf[slot_tok].reshape(n_blocks, MOE_BLOCK, D)

    def expert_block(args):
        xb, e = args
        gu = xb @ w_gate_up[e] + b_gate_up[e]
        g, u = jnp.split(gu, 2, axis=-1)
        g = jnp.minimum(g, SWIGLU_LIMIT)
        u = jnp.clip(u, -SWIGLU_LIMIT, SWIGLU_LIMIT)
        act = g * jax.nn.sigmoid(SWIGLU_ALPHA * g) * (u + 1.0)
        return act @ w_down[e] + b_down[e]

    ys = lax.map(expert_block, (xs, block_expert)).reshape(n_slots, D)
    y = jnp.zeros((N, D), h.dtype).at[slot_tok].add((ys * slot_gate[:, None]).astype(h.dtype))
    return y.reshape(B, S, D)


def setup_inputs(seed: int = 0) -> dict:
    key = jax.random.key(seed)
    ks = jax.random.split(key, 16)
    beta = (8.0 * DEPTH) ** -0.25

    def nrm(k, shape, scale):
        return jax.random.normal(k, shape, jnp.float32) * scale

    return {
        "x": nrm(ks[0], (BATCH, SEQ, D_MODEL), 1.0),
        "w_in": nrm(ks[1], (DEPTH, D_MODEL, D_IN), D_MODEL ** -0.5),
        "b_forget": FORGET_BIAS_INIT + nrm(ks[2], (DEPTH, FOX_HEADS), 0.1),
        "w_branch_a": nrm(ks[3], (DEPTH, MIX_A, D_MODEL), beta * MIX_A ** -0.5),
        "w_branch_b": nrm(ks[4], (DEPTH, MIX_B, D_MODEL), beta * MIX_B ** -0.5),
        "w_out": nrm(ks[5], (DEPTH, D_MODEL, D_MODEL), beta * D_MODEL ** -0.5),
        "ln1_g": 1.0 + nrm(ks[6], (DEPTH, D_MODEL), 0.02),
        "ln1_b": nrm(ks[7], (DEPTH, D_MODEL), 0.02),
        "w_router": nrm(ks[8], (DEPTH, D_MODEL, N_EXPERTS), D_MODEL ** -0.5),
        "b_router": nrm(ks[9], (DEPTH, N_EXPERTS), 0.01),
        "w_gate_up": nrm(ks[10], (DEPTH, N_EXPERTS, D_MODEL, 2 * D_FF), D_MODEL ** -0.5),
        "b_gate_up": nrm(ks[11], (DEPTH, N_EXPERTS, 2 * D_FF), 0.01),
        "w_down": nrm(ks[12], (DEPTH, N_EXPERTS, D_FF, D_MODEL), beta * D_FF ** -0.5),
        "b_down": nrm(ks[13], (DEPTH, N_EXPERTS, D_MODEL), 0.01),
        "ln2_g": 1.0 + nrm(ks[14], (DEPTH, D_MODEL), 0.02),
        "ln2_b": nrm(ks[15], (DEPTH, D_MODEL), 0.02),
    }


def reference(x, w_in, b_forget, w_branch_a, w_branch_b, w_out, ln1_g, ln1_b,
              w_router, b_router, w_gate_up, b_gate_up, w_down, b_down, ln2_g, ln2_b):
    B, S, _ = x.shape
    alpha = (2.0 * DEPTH) ** 0.25
    split_points = np.cumsum(IN_SIZES)[:-1].tolist()
    cos, sin = rope_tables(S, HEAD_DIM // ROT_FRACTION_DEN)
    cos_i, sin_i = rope_tables(S, IDX_DIM // ROT_FRACTION_DEN)

    def heads(t, n):
        return t.reshape(B, S, n, -1)

    for l in range(DEPTH):
        p = x @ w_in[l]
        aq, ak, av, af, bq, bk, bv, iq, ik, iw, ga, gb = jnp.split(p, split_points, axis=-1)
        a_out = forgetting_attention(heads(aq, FOX_HEADS), heads(ak, FOX_HEADS),
                                     heads(av, FOX_HEADS), af, b_forget[l]).reshape(B, S, MIX_A)
        bq_r = partial_rope(heads(bq, DSA_HEADS), cos, sin)
        bk_r = partial_rope(heads(bk, DSA_KV_HEADS), cos, sin)
        iq_r = partial_rope(heads(iq, IDX_HEADS), cos_i, sin_i)
        ik_r = partial_rope(ik[:, :, None, :], cos_i, sin_i)[:, :, 0, :]
        b_out = indexed_sparse_attention(bq_r, bk_r, heads(bv, DSA_KV_HEADS),
                                         iq_r, ik_r, iw).reshape(B, S, MIX_B)
        merged = (jax.nn.sigmoid(ga) * (a_out @ w_branch_a[l])
                  + jax.nn.sigmoid(gb) * (b_out @ w_branch_b[l]))
        x = layer_norm(alpha * x + merged @ w_out[l], ln1_g[l], ln1_b[l])
        moe = moe_ffn(x, w_router[l], b_router[l], w_gate_up[l], b_gate_up[l],
                      w_down[l], b_down[l])
        x = layer_norm(alpha * x + moe, ln2_g[l], ln2_b[l])
    return x
```

```python
import contextlib
import numpy as np
import ml_dtypes
import concourse.bass as bass
import concourse.mybir as mybir
from concourse.bass_utils import run_bass_kernel_spmd

F32 = mybir.dt.float32
BF16 = mybir.dt.bfloat16
AF = mybir.ActivationFunctionType
ALU = mybir.AluOpType
AX = mybir.AxisListType
NPBF = ml_dtypes.bfloat16

NCORES = 8
S = 8192
D = 2048
NQ = S // NCORES
NB = S // 128
NJ = NQ // 128
KT = D // 128
NEG = -60000.0
EPOCH = 30000


class Tk:
    __slots__ = ("name", "w", "rs", "excl")

    def __init__(self, name="", excl=False):
        self.name = name
        self.w = None
        self.rs = []
        self.excl = excl


class Ctx:
    def __init__(self, nc):
        self.nc = nc
        self.esems = {e: [] for e in Prog.ENG}
        self.ecount = {e: 0 for e in Prog.ENG}
        self.nsem = 0

    def esem(self, e, idx):
        k = idx // EPOCH
        while len(self.esems[e]) <= k:
            self.esems[e].append(self.nc.alloc_semaphore(name=f"s_{e}{len(self.esems[e])}"))
            self.nsem += 1
        return self.esems[e][k], idx % EPOCH + 1

    def csem(self):
        self.nsem += 1
        return self.nc.alloc_semaphore(name=f"c{self.nsem}")


class Prog:
    ENG = ("pe", "act", "dve", "pool", "sp")

    def __init__(self, nc, ctx=None):
        self.nc = nc
        self.ctx = ctx or Ctx(nc)
        self.ops = []
        self.stack = contextlib.ExitStack()
        self.chan_count = {}
        self.nt = 0

    def sb(self, shape, dt, name=None):
        self.nt += 1
        return self.stack.enter_context(self.nc.sbuf_tensor(name or f"t{self.nt}", list(shape), dt))

    def ps(self, shape, dt, name=None):
        self.nt += 1
        return self.stack.enter_context(self.nc.psum_tensor(name or f"p{self.nt}", list(shape), dt))

    def _rec(self, eng, fn, reads, writes, dma=False, chan=None, inc=16):
        writes = writes + [r for r in reads if r.excl and r not in writes]
        reads = [r for r in reads if not r.excl]
        deps = set()
        for r in reads:
            if r.w is not None:
                deps.add(r.w)
        for w in writes:
            if w.w is not None:
                deps.add(w.w)
            deps.update(w.rs)
        i = len(self.ops)
        deps.discard(i)
        if dma:
            if chan is None:
                chan = writes[0]
            ckey = (id(chan), eng, inc)
            n = self.chan_count.get(ckey, 0) + inc
            self.chan_count[ckey] = n
            tokv = n
        else:
            tokv = None
        self.ops.append(dict(eng=eng, fn=fn, deps=deps, dma=dma, chan=ckey if dma else None, tokv=tokv, inc=inc))
        for r in reads:
            r.rs.append(i)
        for w in writes:
            w.w = i
            w.rs = []
        return i

    def op(self, eng, fn, reads=(), writes=()):
        return self._rec(eng, fn, list(reads), list(writes))

    def dma(self, eng, out, in_, reads=(), writes=(), chan=None):
        return self._rec(eng, lambda e: e.dma_start(out=out, in_=in_), list(reads), list(writes), dma=True, chan=chan)

    def dma_fn(self, eng, fn, reads=(), writes=(), chan=None, inc=16):
        return self._rec(eng, fn, list(reads), list(writes), dma=True, chan=chan, inc=inc)

    def emit(self):
        nc = self.nc
        ctx = self.ctx
        ops = self.ops
        n = len(ops)
        per_eng = {e: [] for e in self.ENG}
        for i, o in enumerate(ops):
            per_eng[o["eng"]].append(i)
        need = [False] * n
        for i, o in enumerate(ops):
            for d in o["deps"]:
                od = ops[d]
                if od["dma"]:
                    continue
                if od["eng"] == o["eng"] and o["eng"] == "pe" and not o["dma"]:
                    continue
                need[d] = True
        for e in self.ENG:
            comp = [i for i in per_eng[e] if not ops[i]["dma"]]
            if comp:
                need[comp[-1]] = True
        last_chan = {}
        for i, o in enumerate(ops):
            if o["dma"]:
                last_chan[o["chan"]] = i
        sig = [None] * n
        base = dict(ctx.ecount)
        cnt = dict(ctx.ecount)
        for i, o in enumerate(ops):
            if not o["dma"] and need[i]:
                sig[i] = cnt[o["eng"]]
                cnt[o["eng"]] += 1
        csems = {ck: ctx.csem() for ck in self.chan_count}

        def run_engine(ename, e):
            known_e = {x: base[x] - 1 for x in self.ENG}
            known_c = {}
            for i in per_eng[ename]:
                o = ops[i]
                waits_e = {}
                waits_c = {}
                for d in o["deps"]:
                    od = ops[d]
                    if od["dma"]:
                        if od["tokv"] > known_c.get(od["chan"], 0):
                            waits_c[od["chan"]] = max(waits_c.get(od["chan"], 0), od["tokv"])
                    else:
                        if od["eng"] == ename and ename == "pe" and not o["dma"]:
                            continue
                        if sig[d] is not None and sig[d] > known_e[od["eng"]]:
                            waits_e[od["eng"]] = max(waits_e.get(od["eng"], -1), sig[d])
                for en, idx in waits_e.items():
                    sm, v = ctx.esem(en, idx)
                    e.wait_ge(sm, v)
                    known_e[en] = idx
                for ck, v in waits_c.items():
                    e.wait_ge(csems[ck], v)
                    known_c[ck] = v
                ins = o["fn"](e)
                if o["dma"]:
                    ins.then_inc(csems[o["chan"]], o["inc"])
                elif sig[i] is not None:
                    sm, v = ctx.esem(ename, sig[i])
                    ins.then_inc(sm, 1)
            for en in self.ENG:
                if cnt[en] > base[en] and cnt[en] - 1 > known_e[en]:
                    sm, v = ctx.esem(en, cnt[en] - 1)
                    e.wait_ge(sm, v)
            for ck, i in last_chan.items():
                v = ops[i]["tokv"]
                if v > known_c.get(ck, 0):
                    e.wait_ge(csems[ck], v)

        with nc.Block() as block:
            @block.tensor
            def _(e):
                run_engine("pe", e)

            @block.scalar
            def _(e):
                run_engine("act", e)

            @block.vector
            def _(e):
                run_engine("dve", e)

            @block.gpsimd
            def _(e):
                run_engine("pool", e)

            @block.sync
            def _(e):
                run_engine("sp", e)
        ctx.ecount = cnt
        self.stack.close()


def run_spmd(nc, in_maps):
    res = run_bass_kernel_spmd(nc, in_maps, core_ids=list(range(NCORES)))
    if getattr(res, "exec_time_ns", None):
        print(f"[launch] exec_time_ns={res.exec_time_ns}", flush=True)
    return res.results


SZ = dict(aq=1024, ak=1024, av=1024, af=8, bq=1024, bk=256, bv=256, iq=1024, ik=64, iw=16, ga=2048, gb=2048)
ORIG = ["aq", "ak", "av", "af", "bq", "bk", "bv", "iq", "ik", "iw", "ga", "gb"]
ORD_BF = ["bq", "bk", "iq", "ik", "aq", "ak", "av", "bv"]
ORD_F = ["af", "iw", "ga", "gb"]
NBF = sum(SZ[k] for k in ORD_BF)
NF = sum(SZ[k] for k in ORD_F)


def _orig_offsets():
    o, off = {}, 0
    for k in ORIG:
        o[k] = off
        off += SZ[k]
    return o


def _new_offsets():
    o, off = {}, 0
    for k in ORD_BF:
        o[k] = off
        off += SZ[k]
    off = 0
    for k in ORD_F:
        o[k] = off
        off += SZ[k]
    return o


def a_chunks():
    ch = []
    col = 0
    outc = 0
    for k in ORD_BF:
        kind = "rope128" if k in ("bq", "bk") else ("rope64" if k in ("iq", "ik") else "plain")
        n = SZ[k]
        o = 0
        while o < n:
            w = min(512, n - o)
            ch.append((col + o, w, kind, "bf", outc + o))
            o += w
        col += n
        outc += n
    ch.append((col, 24, "plain", "f", 0))
    col += 24
    outc = 24
    for k in ("ga", "gb"):
        for o in range(0, SZ[k], 512):
            ch.append((col + o, 512, "plain", "f", outc + o))
        col += SZ[k]
        outc += SZ[k]
    return ch


def build_A(nq=NQ, chunks=None):
    chunks = chunks or a_chunks()
    ncol = max(c[0] + c[1] for c in chunks)
    nbf = max([c[4] + c[1] for c in chunks if c[3] == "bf"] + [2])
    nf = max([c[4] + c[1] for c in chunks if c[3] == "f"] + [2])
    nblk = nq // 128
    nc = bass.Bass("TRN2", target_bir_lowering=False)
    xT = nc.dram_tensor("xT", [D, nq], F32, kind="ExternalInput").ap()
    w = nc.dram_tensor("w", [D, ncol], F32, kind="ExternalInput").ap()
    cs128 = nc.dram_tensor("cs128", [nq, 2, 4, 16], F32, kind="ExternalInput").ap()
    cs64 = nc.dram_tensor("cs64", [nq, 2, 8, 8], F32, kind="ExternalInput").ap()
    pbf = nc.dram_tensor("pbf", [nq, nbf], BF16, kind="ExternalOutput").ap()
    pf = nc.dram_tensor("pf", [nq, nf], F32, kind="ExternalOutput").ap()
    P = Prog(nc)
    xb = P.sb([128, KT, nq], BF16, "xb")
    t_xb = Tk("xb")
    xTv = xT.rearrange("(kt p) s -> p kt s", p=128)
    for h in range(2):
        P.dma("pool", xb[:, h * 8:(h + 1) * 8, :], xTv[:, h * 8:(h + 1) * 8, :], writes=[t_xb], chan=t_xb)
    c128 = P.sb([128, nblk, 2, 4, 16], F32, "c128")
    c64 = P.sb([128, nblk, 2, 8, 8], F32, "c64")
    t_cs = Tk("cs")
    P.dma("sp", c128[:], cs128.rearrange("(b p) a h r -> p b a h r", p=128), writes=[t_cs], chan=t_cs)
    P.dma("sp", c64[:], cs64.rearrange("(b p) a h r -> p b a h r", p=128), writes=[t_cs], chan=t_cs)
    NW = 3
    wb = [P.sb([128, KT, 512], BF16, f"wb{i}") for i in range(NW)]
    t_wb = [Tk(f"wb{i}") for i in range(NW)]
    NPS = 4
    psm = [P.ps([128, 512], F32, f"psA{i}") for i in range(NPS)]
    t_ps = [Tk(f"ps{i}", excl=True) for i in range(NPS)]
    NO = 4
    obf = [P.sb([128, 512], BF16, f"obf{i}") for i in range(NO)]
    of = [P.sb([128, 512], F32, f"of{i}") for i in range(NO)]
    t_o = [Tk(f"o{i}") for i in range(NO)]
    tmp = [P.sb([128, 8, 16], F32, f"rtmp{i}") for i in range(4)]
    t_tmp = Tk("rtmp")
    wv = w.rearrange("(kt p) c -> p kt c", p=128)
    it = 0
    for ci, (c0, cw, kind, grp, oc) in enumerate(chunks):
        wi = ci % NW
        for h in range(2):
            P.dma("pool", wb[wi][:, h * 8:(h + 1) * 8, :cw], wv[:, h * 8:(h + 1) * 8, c0:c0 + cw],
                  writes=[t_wb[wi]], chan=t_wb[wi])
        for b in range(nblk):
            pi = it % NPS
            oi = it % NO
            it += 1
            ps = psm[pi]
            for kt in range(KT):
                P.op("pe", lambda e, ps=ps, kt=kt, b=b, wi=wi, cw=cw: e.matmul(
                    ps[:, :cw], xb[:, kt, b * 128:(b + 1) * 128], wb[wi][:, kt, :cw],
                    start=(kt == 0), stop=(kt == KT - 1)),
                    reads=[t_xb, t_wb[wi]], writes=[t_ps[pi]])
            ot = obf[oi] if grp == "bf" else of[oi]
            if kind == "plain":
                eng = "act" if (it % 2 == 0) else "dve"
                if eng == "act":
                    P.op("act", lambda e, ot=ot, ps=ps, cw=cw: e.copy(ot[:, :cw], ps[:, :cw]),
                         reads=[t_ps[pi]], writes=[t_o[oi]])
                else:
                    P.op("dve", lambda e, ot=ot, ps=ps, cw=cw: e.tensor_copy(ot[:, :cw], ps[:, :cw]),
                         reads=[t_ps[pi]], writes=[t_o[oi]])
            else:
                hd, r = (128, 16) if kind == "rope128" else (64, 8)
                nh = cw // hd
                cst = c128 if kind == "rope128" else c64
                pv = ps[:, :cw].rearrange("p (h d) -> p h d", d=hd)
                ov = ot[:, :cw].rearrange("p (h d) -> p h d", d=hd)
                x1, x2 = pv[:, :, 0:r], pv[:, :, r:2 * r]
                cc, ss = cst[:, b, 0, :nh, :], cst[:, b, 1, :nh, :]
                tv = [t[:, :nh, :r] for t in tmp]
                P.op("act", lambda e, ov=ov, pv=pv, r=r: e.copy(ov[:, :, 2 * r:], pv[:, :, 2 * r:]),
                     reads=[t_ps[pi]], writes=[t_o[oi]])
                P.op("dve", lambda e, a=tv[0], x=x1, c=cc: e.tensor_tensor(a, x, c, ALU.mult),
                     reads=[t_ps[pi], t_cs], writes=[t_tmp])
                P.op("dve", lambda e, a=tv[1], x=x2, c=ss: e.tensor_tensor(a, x, c, ALU.mult),
                     reads=[t_ps[pi], t_cs], writes=[t_tmp])
                P.op("dve", lambda e, a=tv[2], x=x2, c=cc: e.tensor_tensor(a, x, c, ALU.mult),
                     reads=[t_ps[pi], t_cs], writes=[t_tmp])
                P.op("dve", lambda e, a=tv[3], x=x1, c=ss: e.tensor_tensor(a, x, c, ALU.mult),
                     reads=[t_ps[pi], t_cs], writes=[t_tmp])
                P.op("dve", lambda e, o=ov[:, :, 0:r], a=tv[0], b_=tv[1]: e.tensor_tensor(o, a, b_, ALU.subtract),
                     reads=[t_tmp], writes=[t_o[oi]])
                P.op("dve", lambda e, o=ov[:, :, r:2 * r], a=tv[2], b_=tv[3]: e.tensor_tensor(o, a, b_, ALU.add),
                     reads=[t_tmp], writes=[t_o[oi]])
            dst = pbf if grp == "bf" else pf
            P.dma("sp", dst[b * 128:(b + 1) * 128, oc:oc + cw], ot[:, :cw], reads=[t_o[oi]], writes=[Tk()], chan=t_o[oi])
    P.emit()
    return nc


def rope_tables(pos, rot_dim, theta=500000.0):
    inv = np.power(np.float32(theta), -np.arange(0, rot_dim, 2, dtype=np.float32) / np.float32(rot_dim)).astype(np.float32)
    ang = pos.astype(np.float32)[:, None] * inv[None, :]
    return np.cos(ang).astype(np.float32), np.sin(ang).astype(np.float32)


def own_tokens(c):
    return np.concatenate([np.arange((8 * j + c) * 128, (8 * j + c + 1) * 128) for j in range(NJ)])


def perm_w_in(w_in):
    oo = _orig_offsets()
    cols = []
    for k in ORD_BF + ORD_F:
        cols.append(np.arange(oo[k], oo[k] + SZ[k]))
    return np.ascontiguousarray(w_in[:, np.concatenate(cols)])


def launch_A(x, w_in):
    wp = perm_w_in(w_in)
    nc = build_A()
    in_maps = []
    for c in range(NCORES):
        tok = own_tokens(c)
        cos, sin = rope_tables(tok, 32)
        cs128 = np.stack([np.repeat(cos[:, None, :], 4, 1), np.repeat(sin[:, None, :], 4, 1)], 1)
        cos, sin = rope_tables(tok, 16)
        cs64 = np.stack([np.repeat(cos[:, None, :], 8, 1), np.repeat(sin[:, None, :], 8, 1)], 1)
        in_maps.append(dict(xT=np.ascontiguousarray(x[tok].T), w=wp,
                            cs128=np.ascontiguousarray(cs128, dtype=np.float32),
                            cs64=np.ascontiguousarray(cs64, dtype=np.float32)))
    res = run_spmd(nc, in_maps)
    return [(r["pbf"], r["pf"]) for r in res]


BIS_R = 16.0


def build_B(s=S, nsel=256, nheads=8, nbis=26, part="fox"):
    fox = part == "fox"
    dsa = part == "dsa"
    nb = s // 128
    nq = s // NCORES
    nj = nq // 128
    scale = 128 ** -0.5
    idx_scale = (16 ** -0.5) * (64 ** -0.5)
    nc = bass.Bass("TRN2", target_bir_lowering=False)

    def din(name, shape, dt):
        return nc.dram_tensor(name, list(shape), dt, kind="ExternalInput").ap()
    if fox:
        aqT = din("aqT", [128, 8, nq], BF16)
        akT = din("akT", [8, 128, s], BF16)
        av = din("av", [8, 128, nb, 128], BF16)
        af = din("af", [128, nb, 8], F32)
        bfg = din("bfg", [128, 8], F32)
        dfox = din("dfox", [128, 8, 128], BF16)
        ind = din("ind", [128, nj, nb, 8], F32)
        tri = din("tri", [128, 128], F32)
    if dsa:
        iqT = din("iqT", [128, 8, nq], BF16)
        ikT2 = din("ikT2", [128, s], BF16)
        iw = din("iw", [128, nj, 16], F32)
        bqT = din("bqT", [128, nj, 8, 128], BF16)
        bkT = din("bkT", [128, 2, s], BF16)
        bv = din("bv", [128, nb, 2, 128], BF16)
        dadm = din("dadm", [128, 8, 128], F32)
    ident4 = din("ident4", [128, 512], BF16)
    ab = nc.dram_tensor("ab", [nq, 1024], BF16, kind="ExternalOutput").ap()

    P = Prog(nc)
    banks = [P.ps([128, 512], F32, f"bank{i}") for i in range(8)]
    t_bank = [Tk(f"bank{i}", excl=True) for i in range(8)]

    def load(name, src, shape, dt, eng="sp"):
        t = P.sb(shape, dt, name)
        tk = Tk(name)
        P.dma(eng, t[:], src, writes=[tk])
        return t, tk
    id_sb, t_id = load("id_sb", ident4, [128, 512], BF16)
    if fox:
        aq_sb, t_aq = load("aq_sb", aqT, [128, 8, nq], BF16)
        af_sb, t_af = load("af_sb", af, [128, nb, 8], F32)
        bfg_sb, t_bfg = load("bfg_sb", bfg, [128, 8], F32)
        dfox_sb, t_dfox = load("dfox_sb", dfox, [128, 8, 128], BF16)
        ind_sb, t_ind = load("ind_sb", ind, [128, nj, nb, 8], F32)
        tri_sb, t_tri = load("tri_sb", tri, [128, 128], F32)
    ones_sb = P.sb([128, 128], F32, "ones_sb")
    t_ones = Tk("ones")
    P.op("pool", lambda e: e.memset(ones_sb[:], 1.0), writes=[t_ones])
    if fox:
        ab_sb = P.sb([128, nj, 1024], BF16, "ab_sb")
    t_ab = Tk("ab")

    rec = P.sb([128, 8], F32, "rec")
    t_rec = Tk("rec")
    if fox:
      L = P.sb([128, nb, 8], F32, "L")
      t_L = Tk("L")
      for b in range(nb):
          P.op("dve", lambda e, b=b: e.tensor_tensor(L[:, b, :], af_sb[:, b, :], bfg_sb[:], ALU.add),
               reads=[t_af, t_bfg], writes=[t_L])
      Lf = L[:].rearrange("p b h -> p (b h)")
      P.op("act", lambda e: e.activation(out=Lf, in_=Lf, func=AF.Exp, scale=-1.0), reads=[t_L], writes=[t_L])
      P.op("act", lambda e: e.activation(out=Lf, in_=Lf, func=AF.Ln, bias=1.0), reads=[t_L], writes=[t_L])
      CP = P.sb([128, nb, 8], F32, "CP")
      TOT = P.sb([128, nb, 8], F32, "TOT")
      PRE = P.sb([128, nb, 8], F32, "PRE")
      t_CP, t_TOT, t_PRE = Tk("CP"), Tk("TOT"), Tk("PRE")
      ncol = nb * 8
      for c0 in range(0, ncol, 512):
          cw = min(512, ncol - c0)
          P.op("pe", lambda e, c0=c0, cw=cw: e.matmul(banks[0][:, :cw], tri_sb[:], Lf[:, c0:c0 + cw], start=True, stop=True),
               reads=[t_tri, t_L], writes=[t_bank[0]])
          P.op("dve", lambda e, c0=c0, cw=cw: e.tensor_copy(CP[:].rearrange("p b h -> p (b h)")[:, c0:c0 + cw], banks[0][:, :cw]),
               reads=[t_bank[0]], writes=[t_CP])
          P.op("pe", lambda e, c0=c0, cw=cw: e.matmul(banks[1][:, :cw], ones_sb[:], Lf[:, c0:c0 + cw], start=True, stop=True),
               reads=[t_ones, t_L], writes=[t_bank[1]])
          P.op("dve", lambda e, c0=c0, cw=cw: e.tensor_copy(TOT[:].rearrange("p b h -> p (b h)")[:, c0:c0 + cw], banks[1][:, :cw]),
               reads=[t_bank[1]], writes=[t_TOT])
      P.op("dve", lambda e: e.memset(PRE[:, 0, :], 0.0), writes=[t_PRE])
      for b in range(1, nb):
          P.op("dve", lambda e, b=b: e.tensor_tensor(PRE[:, b, :], PRE[:, b - 1, :], TOT[:, b - 1, :], ALU.add),
               reads=[t_TOT, t_PRE], writes=[t_PRE])
      P.op("dve", lambda e: e.tensor_tensor(CP[:], CP[:], PRE[:], ALU.add), reads=[t_CP, t_PRE], writes=[t_CP])
      cref = P.sb([128, nj, 8], F32, "cref")
      t_cref = Tk("cref")
      tmpi = P.sb([128, nb, 8], F32, "tmpi")
      t_tmpi = Tk("tmpi")
      for j in range(nj):
          P.op("dve", lambda e, j=j: e.tensor_tensor(tmpi[:], TOT[:], ind_sb[:, j, :, :], ALU.mult),
               reads=[t_TOT, t_ind], writes=[t_tmpi])
          P.op("dve", lambda e, j=j: e.tensor_reduce(cref[:, j, :], tmpi[:].rearrange("p b h -> p h b"), AX.X, ALU.add),
               reads=[t_tmpi], writes=[t_cref])
      biasJ = P.sb([128, nj, nb, 8], F32, "biasJ")
      t_bias = Tk("biasJ")
      for j in range(nj):
          nk = NCORES * (j + 1)
          for h in range(8):
              P.op("dve", lambda e, j=j, h=h, nk=nk: e.tensor_scalar(
                  biasJ[:, j, :nk, h], CP[:, :nk, h], cref[:, j, h:h + 1], None, ALU.subtract),
                  reads=[t_CP, t_cref], writes=[t_bias])

      kT = [P.sb([128, s], BF16, f"kT{i}") for i in range(2)]
      t_kT = [Tk(f"kT{i}") for i in range(2)]
      vt = [P.sb([128, nb, 129], BF16, f"vt{i}") for i in range(2)]
      t_vt = [Tk(f"vt{i}") for i in range(2)]
      for i in range(2):
          P.op("pool", lambda e, i=i: e.memset(vt[i][:, :, 128:129], 1.0), writes=[t_vt[i]])
      pT = [P.sb([128, 128], BF16, f"pT{i}") for i in range(4)]
      t_pT = [Tk(f"pT{i}") for i in range(4)]
      ipt = 0
      for h in range(nheads):
          hi = h % 2
          P.dma("sp", kT[hi][:], akT[h], writes=[t_kT[hi]])
          P.dma("sp", vt[hi][:, :, 0:128], av[h], writes=[t_vt[hi]])
          for j in range(nj):
              nk = NCORES * (j + 1)
              ob = 2 + (j % 2)
              SB = (0, 1, 4, 5)
              LA = 2

              def qk(kb, j=j, h=h, hi=hi, nk=nk):
                  sbk = SB[(tile0 + kb) % 4]
                  diag = kb >= nk - NCORES
                  P.op("pe", lambda e, sbk=sbk, diag=diag: e.matmul(
                      banks[sbk][:, :128], kT[hi][:, kb * 128:(kb + 1) * 128], aq_sb[:, h, j * 128:(j + 1) * 128],
                      start=True, stop=not diag), reads=[t_kT[hi], t_aq], writes=[t_bank[sbk]])
                  if diag:
                      P.op("pe", lambda e, sbk=sbk: e.matmul(
                          banks[sbk][:, :128], id_sb[:, :128], dfox_sb[:, kb - (nk - NCORES), :],
                          start=False, stop=True), reads=[t_id, t_dfox], writes=[t_bank[sbk]])
              tile0 = ipt
              for kb in range(min(LA, nk)):
                  qk(kb)
              for kb in range(nk):
                  if kb + LA < nk:
                      qk(kb + LA)
                  sbk = SB[(tile0 + kb) % 4]
                  pi = (tile0 + kb) % 4
                  P.op("act", lambda e, pi=pi, sbk=sbk, j=j, kb=kb, h=h: e.activation(
                      out=pT[pi][:, :128], in_=banks[sbk][:, :128], func=AF.Exp,
                      bias=biasJ[:, j, kb, h:h + 1], scale=scale),
                      reads=[t_bank[sbk], t_bias], writes=[t_pT[pi]])
                  P.op("pe", lambda e, ob=ob, pi=pi, hi=hi, kb=kb, nk=nk: e.matmul(
                      banks[ob][:, :129], pT[pi][:, :128], vt[hi][:, kb, :],
                      start=(kb == 0), stop=(kb == nk - 1)), reads=[t_pT[pi], t_vt[hi]], writes=[t_bank[ob]])
              ipt += nk
              P.op("dve", lambda e, ob=ob: e.reciprocal(rec[:, 0:1], banks[ob][:, 128:129]),
                   reads=[t_bank[ob]], writes=[t_rec])
              P.op("dve", lambda e, ob=ob, j=j, h=h: e.tensor_scalar(
                  ab_sb[:, j, h * 128:(h + 1) * 128], banks[ob][:, :128], rec[:, 0:1], None, ALU.mult),
                  reads=[t_bank[ob], t_rec], writes=[t_ab])

    if dsa:
      ik_sb, t_ik = load("ik_sb", ikT2, [128, s], BF16)
      iw_sb, t_iw = load("iw_sb", iw, [128, nj, 16], F32)
      bk_sb, t_bk = load("bk_sb", bkT, [128, 2, s], BF16)
      dadm_sb, t_dadm = load("dadm_sb", dadm, [128, 8, 128], F32)
      bv_sb = P.sb([128, nb, 2, 129], BF16, "bv_sb")
      t_bv = Tk("bv")
      P.op("pool", lambda e: e.memset(bv_sb[:, :, :, 128:129], 1.0), writes=[t_bv])
      P.dma("sp", bv_sb[:, :, :, 0:128], bv, writes=[t_bv])
      scl = P.sb([128, nj, 16], F32, "scl")
      sgn = P.sb([128, nj, 16], F32, "sgn")
      t_scl, t_sgn = Tk("scl"), Tk("sgn")
      P.op("dve", lambda e: e.tensor_scalar(sgn[:], iw_sb[:], 0.0, 2.0, ALU.is_ge, ALU.mult),
           reads=[t_iw], writes=[t_sgn])
      P.op("dve", lambda e: e.tensor_scalar(sgn[:], sgn[:], -1.0, None, ALU.add), reads=[t_sgn], writes=[t_sgn])
      P.op("dve", lambda e: e.tensor_tensor(scl[:], iw_sb[:], sgn[:], ALU.mult), reads=[t_iw, t_sgn], writes=[t_scl])
      P.op("dve", lambda e: e.tensor_scalar(scl[:], scl[:], idx_scale, None, ALU.mult), reads=[t_scl], writes=[t_scl])
      score = [P.sb([128, s], F32, f"score{i}") for i in range(2)]
      t_score = [Tk(f"score{i}") for i in range(2)]
      mb = P.sb([128, s], BF16, "mb")
      t_mb = Tk("mb")
      junk = P.sb([128, s], BF16, "junk")
      t_junk = Tk("junk")
      iqs = [P.sb([128, 8, 128], BF16, f"iqs{i}") for i in range(2)]
      t_iqs = [Tk(f"iqs{i}") for i in range(2)]
      bqs = [P.sb([128, 8, 128], BF16, f"bqs{i}") for i in range(2)]
      t_bqs = [Tk(f"bqs{i}") for i in range(2)]
      dg0 = P.sb([128, 16, 128], BF16, "dg0")
      t_dg0 = Tk("dg0")
      dg = [dg0, dg0]
      t_dg = [t_dg0, t_dg0]
      rl = [P.sb([128, 512], BF16, f"rl{i}") for i in range(2)]
      t_rl = [Tk(f"rl{i}") for i in range(2)]
      pT4 = [P.sb([128, 512], BF16, f"pT4{i}") for i in range(3)]
      t_pT4 = [Tk(f"pT4{i}") for i in range(3)]
      outt = [P.sb([128, 1024], BF16, f"outt{i}") for i in range(2)]
      t_outt = [Tk(f"outt{i}") for i in range(2)]
      bisv = [P.sb([128, 8], F32, f"bis{i}") for i in range(2)]
      t_bisv = [Tk(f"bis{i}") for i in range(2)]
      rc = P.sb([128, 4], F32, "rc")
      t_rc = Tk("rc")
      st8 = dict(iz=0, iatt=0)

      def indexer(j):
          ji = j % 2
          sc, tsc = score[ji], t_score[ji]
          nk = NCORES * (j + 1)
          ngrp = nk * 128 // 512
          P.dma("sp", iqs[ji][:], iqT[:, :, j * 128:(j + 1) * 128], writes=[t_iqs[ji]])
          P.dma("sp", bqs[ji][:], bqT[:, j, :, :], writes=[t_bqs[ji]])
          for h in range(16):
              P.op("dve", lambda e, h=h: e.tensor_scalar(dg[ji][:, h, :], id_sb[:, :128], sgn[:, j, h:h + 1], None, ALU.mult),
                   reads=[t_id, t_sgn], writes=[t_dg[ji]])
          for kg in range(ngrp):
              def zmm(h, kg=kg):
                  zb = (st8["iz"] + h) % 2
                  pr = (h % 2) * 64
                  P.op("pe", lambda e, zb=zb, pr=pr, h=h, kg=kg: e.matmul(
                      banks[zb][:, :512], iqs[ji][pr:pr + 64, h // 2, :],
                      ik_sb[pr:pr + 64, kg * 512:(kg + 1) * 512], start=True, stop=True),
                      reads=[t_iqs[ji], t_ik], writes=[t_bank[zb]])
              zmm(0)
              for h in range(16):
                  if h + 1 < 16:
                      zmm(h + 1)
                  zb = (st8["iz"] + h) % 2
                  if h % 4 == 3:
                      P.op("dve", lambda e, zb=zb, h=h: e.tensor_scalar(
                          rl[zb][:], banks[zb][:, :512], 0.0, scl[:, j, h:h + 1], ALU.max, ALU.mult),
                          reads=[t_bank[zb], t_scl], writes=[t_rl[zb]])
                  else:
                      P.op("act", lambda e, zb=zb, h=h: e.activation(
                          out=rl[zb][:], in_=banks[zb][:, :512], func=AF.Relu, scale=scl[:, j, h:h + 1]),
                          reads=[t_bank[zb], t_scl], writes=[t_rl[zb]])
                  P.op("pe", lambda e, zb=zb, h=h: e.matmul(
                      banks[6][:, :512], dg[ji][:, h, :], rl[zb][:], start=(h == 0), stop=(h == 15)),
                      reads=[t_dg[ji], t_rl[zb]], writes=[t_bank[6]])
              st8["iz"] += 16
              lastg = kg - (ngrp - 2)
              if lastg >= 0:
                  init = dadm_sb[:, lastg * 4:(lastg + 1) * 4, :].rearrange("p a k -> p (a k)")
                  P.op("dve", lambda e, kg=kg, init=init: e.tensor_tensor(
                      sc[:, kg * 512:(kg + 1) * 512], banks[6][:, :512], init, ALU.add),
                      reads=[t_bank[6], t_dadm], writes=[tsc])
              else:
                  P.op("dve", lambda e, kg=kg: e.tensor_copy(sc[:, kg * 512:(kg + 1) * 512], banks[6][:, :512]),
                       reads=[t_bank[6]], writes=[tsc])
              yield

      def bisection(j):
          ji = j % 2
          sc, tsc = score[ji], t_score[ji]
          bv_, tb = bisv[ji], t_bisv[ji]
          LO, MID, CNT, FL = [bv_[:, i:i + 1] for i in range(4)]
          nkeys = NCORES * (j + 1) * 128
          P.op("dve", lambda e: e.memset(LO, -BIS_R), writes=[tb])
          P.op("dve", lambda e: e.memset(MID, 0.0), writes=[tb])
          for it in range(nbis):
              hw_ = BIS_R / (2.0 ** it)
              P.op("dve", lambda e: e.tensor_scalar(
                  junk[:, :nkeys], sc[:, :nkeys], MID, None, ALU.is_ge, ALU.add, accum_out=CNT),
                  reads=[tsc, tb], writes=[tb, t_junk])
              P.op("dve", lambda e, hw_=hw_: e.tensor_scalar(FL, CNT, nsel - 0.5, hw_, ALU.is_ge, ALU.mult),
                   reads=[tb], writes=[tb])
              P.op("dve", lambda e: e.tensor_tensor(LO, LO, FL, ALU.add), reads=[tb], writes=[tb])
              P.op("dve", lambda e, hw_=hw_: e.tensor_scalar(MID, LO, hw_ * 0.5, None, ALU.add), reads=[tb], writes=[tb])
              yield
          P.op("dve", lambda e: e.tensor_scalar(mb[:, :nkeys], sc[:, :nkeys], LO, NEG, ALU.is_lt, ALU.mult),
               reads=[tsc, tb], writes=[t_mb])

      def attention(j):
          ji = j % 2
          nk = NCORES * (j + 1)
          oi = j % 2
          SB = (4, 5, 7)
          LA = 2
          for g in range(2):
              obs = (2, 3)

              def qk2(kb, g=g):
                  sbk = SB[(st8["iatt"] + kb) % 3]
                  P.op("pe", lambda e, sbk=sbk, g=g, kb=kb: e.matmul(
                      banks[sbk][:, :512], bk_sb[:, g, kb * 128:(kb + 1) * 128],
                      bqs[ji][:, g * 4:(g + 1) * 4, :].rearrange("p a q -> p (a q)"), start=True, stop=False),
                      reads=[t_bk, t_bqs[ji]], writes=[t_bank[sbk]])
                  P.op("pe", lambda e, sbk=sbk, kb=kb: e.matmul(
                      banks[sbk][:, :512], mb[:, kb * 128:(kb + 1) * 128], id_sb[:, :512], start=False, stop=True),
                      reads=[t_mb, t_id], writes=[t_bank[sbk]])
              for kb in range(min(LA, nk)):
                  qk2(kb)
              for kb in range(nk):
                  if kb + LA < nk:
                      qk2(kb + LA)
                  sbk = SB[(st8["iatt"] + kb) % 3]
                  pi = (st8["iatt"] + kb) % 3
                  P.op("act", lambda e, pi=pi, sbk=sbk: e.activation(
                      out=pT4[pi][:], in_=banks[sbk][:, :512], func=AF.Exp, scale=scale),
                      reads=[t_bank[sbk]], writes=[t_pT4[pi]])
                  for hh in range(4):
                      ob = obs[hh // 2]
                      c0 = (hh % 2) * 129
                      P.op("pe", lambda e, ob=ob, c0=c0, pi=pi, hh=hh, kb=kb, g=g: e.matmul(
                          banks[ob][:, c0:c0 + 129], pT4[pi][:, hh * 128:(hh + 1) * 128], bv_sb[:, kb, g, :],
                          start=(kb == 0 and hh % 2 == 0), stop=(kb == nk - 1), skip_group_check=True),
                          reads=[t_pT4[pi], t_bv], writes=[t_bank[ob]])
              st8["iatt"] += nk
              for hh in range(4):
                  ob = obs[hh // 2]
                  c0 = (hh % 2) * 129
                  head = g * 4 + hh
                  P.op("act", lambda e, ob=ob, c0=c0: e.activation(out=rc[:, 0:1], in_=banks[ob][:, c0 + 128:c0 + 129], func=AF.Ln),
                       reads=[t_bank[ob]], writes=[t_rc])
                  P.op("act", lambda e: e.activation(out=rc[:, 1:2], in_=rc[:, 0:1], func=AF.Exp, scale=-1.0),
                       reads=[t_rc], writes=[t_rc])
                  P.op("act", lambda e, ob=ob, c0=c0, head=head: e.activation(
                      out=outt[oi][:, head * 128:(head + 1) * 128], in_=banks[ob][:, c0:c0 + 128], func=AF.Copy, scale=rc[:, 1:2]),
                      reads=[t_bank[ob], t_rc], writes=[t_outt[oi]])
          P.dma("sp", ab[j * 128:(j + 1) * 128, :], outt[oi][:], reads=[t_outt[oi]], writes=[Tk()], chan=t_outt[oi])

      order = list(range(nj))[::-1]
      for _ in indexer(order[0]):
          pass
      for oi_, j in enumerate(order):
          bs = bisection(j)
          if oi_ + 1 < nj:
              jn = order[oi_ + 1]
              ng = NCORES * (jn + 1) * 128 // 512
              per = -(-nbis // ng)
              for _ in indexer(jn):
                  for _i in range(per):
                      next(bs, None)
          for _ in bs:
              pass
          attention(j)
    if fox:
        for j in range(nj):
            P.dma("sp", ab[j * 128:(j + 1) * 128, :], ab_sb[:, j, :], reads=[t_ab], writes=[Tk()], chan=t_ab)
    P.emit()
    return nc


def bf(a):
    return np.ascontiguousarray(a).astype(NPBF)


def prep_B(c, t, s=S):
    nb = s // 128
    nq = s // NCORES
    nj = nq // 128
    tok = np.concatenate([np.arange((NCORES * j + c) * 128, (NCORES * j + c + 1) * 128) for j in range(nj)])
    m = {}
    m["aqT"] = np.ascontiguousarray(t["aq"][tok].reshape(nq, 8, 128).transpose(2, 1, 0))
    m["akT"] = np.ascontiguousarray(t["ak"].reshape(s, 8, 128).transpose(1, 2, 0))
    m["av"] = np.ascontiguousarray(t["av"].reshape(nb, 128, 8, 128).transpose(2, 1, 0, 3))
    m["af"] = np.ascontiguousarray(t["af"].reshape(nb, 128, 8).transpose(1, 0, 2))
    m["bfg"] = np.ascontiguousarray(np.broadcast_to(t["b_forget"].reshape(1, 8), (128, 8))).astype(np.float32)
    kk = np.arange(8)[None, :, None]
    k = np.arange(128)[:, None, None]
    q = np.arange(128)[None, None, :]
    dfox = np.where(kk < c, 0.0, np.where(kk > c, NEG, np.where(k <= q, 0.0, NEG)))
    m["dfox"] = bf(np.broadcast_to(dfox, (128, 8, 128)))
    qq = np.arange(128)[:, None, None]
    k2 = np.arange(128)[None, None, :]
    dadm = np.where(kk < c, 0.0, np.where(kk > c, -1e30, np.where(k2 // 64 <= qq // 64, 0.0, -1e30)))
    m["dadm"] = np.ascontiguousarray(np.broadcast_to(dadm, (128, 8, 128))).astype(np.float32)
    ind = (np.arange(nb)[None, :] < (NCORES * np.arange(nj)[:, None] + c)).astype(np.float32)
    m["ind"] = np.ascontiguousarray(np.broadcast_to(ind[None, :, :, None], (128, nj, nb, 8))).astype(np.float32)
    m["iqT"] = np.ascontiguousarray(t["iq"][tok].reshape(nq, 8, 128).transpose(2, 1, 0))
    ikT = t["ik"].T
    m["ikT2"] = np.ascontiguousarray(np.concatenate([ikT, ikT], 0))
    m["iw"] = np.ascontiguousarray(t["iw"][tok].reshape(nj, 128, 16).transpose(1, 0, 2))
    m["bqT"] = np.ascontiguousarray(t["bq"][tok].reshape(nj, 128, 8, 128).transpose(3, 0, 2, 1))
    m["bkT"] = np.ascontiguousarray(t["bk"].reshape(s, 2, 128).transpose(2, 1, 0))
    m["bv"] = np.ascontiguousarray(t["bv"].reshape(nb, 128, 2, 128).transpose(1, 0, 2, 3))
    m["ident4"] = bf(np.tile(np.eye(128, dtype=np.float32), (1, 4)))
    m["tri"] = np.triu(np.ones((128, 128), np.float32))
    return m


def emit_ln(P, x, t_x, g_sb, b_sb, t_gb, st, mv, t_st, eps=1e-5):
    for k in range(4):
        P.op("dve", lambda e, k=k: e.bn_stats(st[:, k, :], x[:, k * 512:(k + 1) * 512]), reads=[t_x], writes=[t_st])
    P.op("dve", lambda e: e.bn_aggr(mv[:, 0:2], st[:].rearrange("p a b -> p (a b)")), reads=[t_st], writes=[t_st])
    P.op("act", lambda e: e.activation(out=mv[:, 2:3], in_=mv[:, 1:2], func=AF.Sqrt, bias=mv[:, 4:5], scale=1.0),
         reads=[t_st], writes=[t_st])
    P.op("dve", lambda e: e.reciprocal(mv[:, 3:4], mv[:, 2:3]), reads=[t_st], writes=[t_st])
    P.op("dve", lambda e: e.tensor_scalar(x[:], x[:], mv[:, 0:1], mv[:, 3:4], ALU.subtract, ALU.mult),
         reads=[t_x, t_st], writes=[t_x])
    P.op("dve", lambda e: e.tensor_tensor(x[:], x[:], g_sb[:], ALU.mult), reads=[t_x, t_gb], writes=[t_x])
    P.op("dve", lambda e: e.tensor_tensor(x[:], x[:], b_sb[:], ALU.add), reads=[t_x, t_gb], writes=[t_x])


def build_C(nq=NQ, alpha=2.0 ** 0.25):
    nblk = nq // 128
    ntg = max(nq // 512, 1)
    tgw = min(512, nq)
    nc = bass.Bass("TRN2", target_bir_lowering=False)

    def din(name, shape, dt):
        return nc.dram_tensor(name, list(shape), dt, kind="ExternalInput").ap()
    abT = din("abT", [128, 16, nq], BF16)
    gaT = din("gaT", [16, 128, nq], F32)
    gbT = din("gbT", [16, 128, nq], F32)
    wa = din("wa", [1024, 2048], F32)
    wb = din("wb", [1024, 2048], F32)
    wo = din("wo", [2048, 2048], F32)
    xq = din("xq", [nq, 2048], F32)
    lng = din("lng", [128, 2048], F32)
    lnb = din("lnb", [128, 2048], F32)
    wr = din("wr", [2048, 32], F32)
    br = din("br", [128, 32], F32)
    ident = din("ident", [128, 128], F32)
    h_out = nc.dram_tensor("h", [nq, 2048], F32, kind="ExternalOutput").ap()
    g_out = nc.dram_tensor("G", [nq, 32], F32, kind="ExternalOutput").ap()
    P = Prog(nc)
    banks = [P.ps([128, 512], F32, f"bank{i}") for i in range(8)]
    t_bank = [Tk(f"bank{i}", excl=True) for i in range(8)]

    def load(name, src, shape, dt, eng="sp"):
        t = P.sb(shape, dt, name)
        tk = Tk(name)
        P.dma(eng, t[:], src, writes=[tk])
        return t, tk
    ab_sb, t_abT = load("abT_sb", abT, [128, 16, nq], BF16)
    W_sb = P.sb([128, 16, 2048], BF16, "W_sb")
    t_wa, t_wb = Tk("wa"), Tk("wb")
    P.dma("pool", W_sb[:, 0:8, :], wa.rearrange("(kt p) n -> p kt n", p=128), writes=[t_wa])
    P.dma("pool", W_sb[:, 8:16, :], wb.rearrange("(kt p) n -> p kt n", p=128), writes=[t_wb])
    wa_sb = W_sb[:, 0:8, :]
    wb_sb = W_sb[:, 8:16, :]
    mT = P.sb([128, 16, nq], BF16, "mT")
    t_mT = Tk("mT")
    gsb = [P.sb([128, 2, nq], F32, f"gsb{i}") for i in range(2)]
    t_g = [Tk(f"g{i}") for i in range(2)]
    m12 = [[P.sb([128, 512], F32, f"m12{p_}_{i}") for i in range(2)] for p_ in range(2)]
    t_m12 = [Tk("m12a"), Tk("m12b")]
    itc = [0]
    for c in range(16):
        gi = c % 2
        P.dma("sp", gsb[gi][:, 0, :], gaT[c], writes=[t_g[gi]])
        P.dma("sp", gsb[gi][:, 1, :], gbT[c], writes=[t_g[gi]])
        P.op("act", lambda e, gi=gi: e.activation(out=gsb[gi][:], in_=gsb[gi][:], func=AF.Sigmoid),
             reads=[t_g[gi]], writes=[t_g[gi]])
        for tg in range(ntg):
            ts_ = slice(tg * tgw, (tg + 1) * tgw)
            pp = itc[0] % 2
            itc[0] += 1
            ba, bb = (0, 1) if pp == 0 else (2, 3)
            ma, mb_ = m12[pp]
            for kt in range(8):
                P.op("pe", lambda e, kt=kt, c=c, ts_=ts_, ba=ba: e.matmul(
                    banks[ba][:, :tgw], wa_sb[:, kt, c * 128:(c + 1) * 128], ab_sb[:, kt, ts_],
                    start=(kt == 0), stop=(kt == 7)), reads=[t_wa, t_abT], writes=[t_bank[ba]])
            for kt in range(8):
                P.op("pe", lambda e, kt=kt, c=c, ts_=ts_, bb=bb: e.matmul(
                    banks[bb][:, :tgw], wb_sb[:, kt, c * 128:(c + 1) * 128], ab_sb[:, 8 + kt, ts_],
                    start=(kt == 0), stop=(kt == 7)), reads=[t_wb, t_abT], writes=[t_bank[bb]])
            P.op("dve", lambda e, gi=gi, ts_=ts_, ba=ba, ma=ma: e.tensor_tensor(ma[:, :tgw], banks[ba][:, :tgw], gsb[gi][:, 0, ts_], ALU.mult),
                 reads=[t_bank[ba], t_g[gi]], writes=[t_m12[pp]])
            P.op("dve", lambda e, gi=gi, ts_=ts_, bb=bb, mb_=mb_: e.tensor_tensor(mb_[:, :tgw], banks[bb][:, :tgw], gsb[gi][:, 1, ts_], ALU.mult),
                 reads=[t_bank[bb], t_g[gi]], writes=[t_m12[pp]])
            P.op("dve", lambda e, c=c, ts_=ts_, ma=ma, mb_=mb_: e.tensor_tensor(mT[:, c, ts_], ma[:, :tgw], mb_[:, :tgw], ALU.add),
                 reads=[t_m12[pp]], writes=[t_mT])
    wo_sb = W_sb
    wov = wo.rearrange("(kt p) n -> p kt n", p=128)
    P.dma("pool", W_sb[:, 0:8, :], wov[:, 0:8, :], writes=[t_wa])
    P.dma("pool", W_sb[:, 8:16, :], wov[:, 8:16, :], writes=[t_wb])
    lng_sb, t_lng = load("lng_sb", lng, [128, 2048], F32)
    lnb_sb, t_lnb = load("lnb_sb", lnb, [128, 2048], F32)
    t_gb = Tk("gb")
    P.op("pool", lambda e: e.engine_nop(), reads=[t_lng, t_lnb], writes=[t_gb])
    wr_sb, t_wr = load("wr_sb", wr.rearrange("(kt p) n -> p kt n", p=128), [128, 16, 32], F32)
    br_sb, t_br = load("br_sb", br, [128, 32], F32)
    id_sb, t_id = load("id_sb", ident, [128, 128], F32)
    xs0 = P.sb([128, 2048], F32, "xs0")
    t_xs0 = Tk("xs0")
    xs = [xs0, xs0]
    t_xs = [t_xs0, t_xs0]
    hs = [P.sb([128, 2048], F32, f"hs{i}") for i in range(2)]
    t_hs = [Tk(f"hs{i}") for i in range(2)]
    st = P.sb([128, 4, 6], F32, "st")
    mv = P.sb([128, 8], F32, "mv")
    t_st = Tk("st")
    P.op("dve", lambda e: e.memset(mv[:, 4:5], 1e-5), writes=[t_st])
    hT = P.sb([128, 16, 128], F32, "hT")
    t_hT = Tk("hT")
    rt = P.sb([128, 4, 32], F32, "rt")
    m8 = P.sb([128, 16], F32, "m8")
    t_rt = Tk("rt")
    for b in range(nblk):
        i2 = b % 2
        P.dma("sp", xs[i2][:], xq[b * 128:(b + 1) * 128, :], writes=[t_xs[i2]])
        for n in range(4):
            for kt in range(16):
                P.op("pe", lambda e, n=n, kt=kt, b=b: e.matmul(
                    banks[4 + n][:, :512], mT[:, kt, b * 128:(b + 1) * 128], wo_sb[:, kt, n * 512:(n + 1) * 512],
                    start=(kt == 0), stop=(kt == 15)), reads=[t_mT, t_wa, t_wb], writes=[t_bank[4 + n]])
            P.op("dve", lambda e, n=n, i2=i2: e.scalar_tensor_tensor(
                out=hs[i2][:, n * 512:(n + 1) * 512], in0=xs[i2][:, n * 512:(n + 1) * 512], scalar=float(alpha),
                in1=banks[4 + n][:, :512], op0=ALU.mult, op1=ALU.add),
                reads=[t_xs[i2], t_bank[4 + n]], writes=[t_hs[i2]])
        emit_ln(P, hs[i2], t_hs[i2], lng_sb, lnb_sb, t_gb, st, mv, t_st)
        P.dma("sp", h_out[b * 128:(b + 1) * 128, :], hs[i2][:], reads=[t_hs[i2]], writes=[Tk()], chan=t_hs[i2])
        for kt in range(16):
            bk = kt % 2
            P.op("pe", lambda e, kt=kt, bk=bk, i2=i2: e.transpose(banks[bk][:, :128], hs[i2][:, kt * 128:(kt + 1) * 128], id_sb[:]),
                 reads=[t_hs[i2], t_id], writes=[t_bank[bk]])
            P.op("act", lambda e, kt=kt, bk=bk: e.copy(hT[:, kt, :], banks[bk][:, :128]), reads=[t_bank[bk]], writes=[t_hT])
        for kt in range(16):
            P.op("pe", lambda e, kt=kt: e.matmul(banks[2][:, :32], hT[:, kt, :], wr_sb[:, kt, :], start=(kt == 0), stop=(kt == 15)),
                 reads=[t_hT, t_wr], writes=[t_bank[2]])
        LG, EX, SEL, GG = rt[:, 0, :], rt[:, 1, :], rt[:, 2, :], rt[:, 3, :]
        P.op("dve", lambda e: e.tensor_tensor(LG, banks[2][:, :32], br_sb[:], ALU.add), reads=[t_bank[2], t_br], writes=[t_rt])
        P.op("dve", lambda e: e.max(m8[:, 0:8], LG), reads=[t_rt], writes=[t_rt])
        P.op("dve", lambda e: e.tensor_scalar(m8[:, 8:9], m8[:, 0:1], -1.0, None, ALU.mult), reads=[t_rt], writes=[t_rt])
        P.op("act", lambda e: e.activation(out=EX, in_=LG, func=AF.Exp, bias=m8[:, 8:9], scale=1.0), reads=[t_rt], writes=[t_rt])
        P.op("dve", lambda e: e.tensor_scalar(SEL, LG, m8[:, 3:4], None, ALU.is_ge), reads=[t_rt], writes=[t_rt])
        P.op("dve", lambda e: e.tensor_tensor(EX, EX, SEL, ALU.mult), reads=[t_rt], writes=[t_rt])
        P.op("dve", lambda e: e.tensor_reduce(m8[:, 9:10], EX, AX.X, ALU.add), reads=[t_rt], writes=[t_rt])
        P.op("dve", lambda e: e.reciprocal(m8[:, 10:11], m8[:, 9:10]), reads=[t_rt], writes=[t_rt])
        P.op("dve", lambda e: e.tensor_scalar(GG, EX, m8[:, 10:11], None, ALU.mult), reads=[t_rt], writes=[t_rt])
        P.dma("sp", g_out[b * 128:(b + 1) * 128, :], GG, reads=[t_rt], writes=[Tk()], chan=t_rt)
    P.emit()
    return nc


def build_D(cap, nexp=4):
    nc = bass.Bass("TRN2", target_bir_lowering=False)

    def din(name, shape, dt):
        return nc.dram_tensor(name, list(shape), dt, kind="ExternalInput").ap()
    xeT = din("xeT", [nexp, 2048, cap], F32)
    wgu = din("wgu", [nexp, 2048, 4096], F32)
    bgu = din("bgu", [nexp, 128, 32], F32)
    wd = din("wd", [nexp, 2048, 2048], F32)
    bd = din("bd", [nexp, 128, 2048], F32)
    y = nc.dram_tensor("y", [nexp, cap, 2048], F32, kind="ExternalOutput").ap()
    P = Prog(nc)
    banks = [P.ps([128, 512], F32, f"bank{i}") for i in range(8)]
    t_bank = [Tk(f"bank{i}", excl=True) for i in range(8)]
    xe = P.sb([128, 16, cap], BF16, "xe")
    t_xe = Tk("xe")
    actT = P.sb([128, 16, cap], BF16, "actT")
    t_act = Tk("actT")
    NW = 3
    wt = [P.sb([128, 16, 512], BF16, f"wt{i}") for i in range(NW)]
    t_wt = [Tk(f"wt{i}") for i in range(NW)]
    bg_sb = P.sb([128, 32], F32, "bg_sb")
    t_bg = Tk("bg")
    bd_sb = P.sb([128, 2048], F32, "bd_sb")
    t_bd = Tk("bd")
    ep = [[P.sb([128, 512], F32, f"ep{p_}_{i}") for i in range(4)] for p_ in range(2)]
    t_ep = [Tk("ep0"), Tk("ep1")]
    itd = [0]
    pend = [None]
    yo = [P.sb([128, 512], F32, f"yo{i}") for i in range(2)]
    t_yo = [Tk(f"yo{i}") for i in range(2)]
    ncg = -(-cap // 512)
    cgw = -(-(cap // ncg) // 64) * 64
    cgs = [(c0, min(cgw, cap - c0)) for c0 in range(0, cap, cgw)]
    iw_ = 0
    iy = 0
    for ex in range(nexp):
        for h in range(2):
            P.dma("pool", xe[:, h * 8:(h + 1) * 8, :], xeT[ex].rearrange("(kt p) c -> p kt c", p=128)[:, h * 8:(h + 1) * 8, :],
                  writes=[t_xe], chan=t_xe)
        P.dma("sp", bg_sb[:], bgu[ex], writes=[t_bg])
        P.dma("sp", bd_sb[:], bd[ex], writes=[t_bd])
        wv = wgu[ex].rearrange("(kt p) c -> p kt c", p=128)
        for q in range(4):
            wi = []
            for half in range(2):
                w_i = iw_ % NW
                iw_ += 1
                c0 = half * 2048 + q * 512
                for hh in range(2):
                    P.dma("pool", wt[w_i][:, hh * 8:(hh + 1) * 8, :], wv[:, hh * 8:(hh + 1) * 8, c0:c0 + 512],
                          writes=[t_wt[w_i]], chan=t_wt[w_i])
                wi.append(w_i)
            for f in range(4):
                ffc = q * 4 + f
                for (c0, cw) in cgs:
                    pp = itd[0] % 2
                    itd[0] += 1
                    bg, bu = (0, 1) if pp == 0 else (4, 5)
                    e0, e1, e2, e3 = ep[pp]
                    for half in range(2):
                        bk = bg if half == 0 else bu
                        for kt in range(16):
                            P.op("pe", lambda e, bk=bk, wi_=wi[half], kt=kt, f=f, c0=c0, cw=cw: e.matmul(
                                banks[bk][:, :cw], wt[wi_][:, kt, f * 128:(f + 1) * 128], xe[:, kt, c0:c0 + cw],
                                start=(kt == 0), stop=(kt == 15)), reads=[t_wt[wi[half]], t_xe], writes=[t_bank[bk]])
                    P.op("dve", lambda e, ffc=ffc, cw=cw, bg=bg, e0=e0: e.tensor_scalar(e0[:, :cw], banks[bg][:, :cw], bg_sb[:, ffc:ffc + 1], 7.0, ALU.add, ALU.min),
                         reads=[t_bank[bg], t_bg], writes=[t_ep[pp]])
                    P.op("dve", lambda e, ffc=ffc, cw=cw, bu=bu, e1=e1: e.tensor_scalar(e1[:, :cw], banks[bu][:, :cw], bg_sb[:, 16 + ffc:17 + ffc], 7.0, ALU.add, ALU.min),
                         reads=[t_bank[bu], t_bg], writes=[t_ep[pp]])
                    P.op("dve", lambda e, cw=cw, e1=e1: e.tensor_scalar(e1[:, :cw], e1[:, :cw], -7.0, 1.0, ALU.max, ALU.add),
                         reads=[t_ep[pp]], writes=[t_ep[pp]])
                    P.op("act", lambda e, cw=cw, e0=e0, e2=e2: e.activation(out=e2[:, :cw], in_=e0[:, :cw], func=AF.Sigmoid, scale=1.702),
                         reads=[t_ep[pp]], writes=[t_ep[pp]])
                    if pend[0] is not None:
                        pend[0]()

                    def part2(pp=pp, cw=cw, ffc=ffc, c0=c0, e0=e0, e1=e1, e2=e2, e3=e3):
                        P.op("dve", lambda e: e.tensor_tensor(e3[:, :cw], e0[:, :cw], e2[:, :cw], ALU.mult),
                             reads=[t_ep[pp]], writes=[t_ep[pp]])
                        P.op("dve", lambda e: e.tensor_tensor(actT[:, ffc, c0:c0 + cw], e3[:, :cw], e1[:, :cw], ALU.mult),
                             reads=[t_ep[pp]], writes=[t_act])
                    pend[0] = part2
        if pend[0] is not None:
            pend[0]()
            pend[0] = None
        wdv = wd[ex].rearrange("(kt p) c -> p kt c", p=128)
        for n in range(4):
            w_i = iw_ % NW
            iw_ += 1
            for hh in range(2):
                P.dma("pool", wt[w_i][:, hh * 8:(hh + 1) * 8, :], wdv[:, hh * 8:(hh + 1) * 8, n * 512:(n + 1) * 512],
                      writes=[t_wt[w_i]], chan=t_wt[w_i])
            for sb_ in range(cap // 128):
                bk = 2 + iy % 2
                yi = iy % 2
                iy += 1
                for kt in range(16):
                    P.op("pe", lambda e, bk=bk, kt=kt, sb_=sb_, w_i=w_i: e.matmul(
                        banks[bk][:, :512], actT[:, kt, sb_ * 128:(sb_ + 1) * 128], wt[w_i][:, kt, :],
                        start=(kt == 0), stop=(kt == 15)), reads=[t_act, t_wt[w_i]], writes=[t_bank[bk]])
                P.op("dve", lambda e, bk=bk, yi=yi, n=n: e.tensor_tensor(yo[yi][:], banks[bk][:, :512], bd_sb[:, n * 512:(n + 1) * 512], ALU.add),
                     reads=[t_bank[bk], t_bd], writes=[t_yo[yi]])
                P.dma("sp", y[ex, sb_ * 128:(sb_ + 1) * 128, n * 512:(n + 1) * 512], yo[yi][:], reads=[t_yo[yi]], writes=[Tk()], chan=t_yo[yi])
    P.emit()
    return nc


def build_E(nq=NQ, alpha=2.0 ** 0.25):
    nblk = nq // 128
    nc = bass.Bass("TRN2", target_bir_lowering=False)

    def din(name, shape, dt):
        return nc.dram_tensor(name, list(shape), dt, kind="ExternalInput").ap()
    y4 = din("y4", [nq, 4, 2048], F32)
    g4 = din("g4", [nq, 4], F32)
    h = din("h", [nq, 2048], F32)
    lng = din("lng", [128, 2048], F32)
    lnb = din("lnb", [128, 2048], F32)
    out = nc.dram_tensor("out", [nq, 2048], F32, kind="ExternalOutput").ap()
    P = Prog(nc)
    lng_sb = P.sb([128, 2048], F32, "lng_sb")
    lnb_sb = P.sb([128, 2048], F32, "lnb_sb")
    t_gb = Tk("gb")
    P.dma("sp", lng_sb[:], lng, writes=[t_gb], chan=t_gb)
    P.dma("sp", lnb_sb[:], lnb, writes=[t_gb], chan=t_gb)
    ys = [P.sb([128, 4, 2048], F32, f"ys{i}") for i in range(2)]
    t_ys = [Tk(f"ys{i}") for i in range(2)]
    hs = [P.sb([128, 2048], F32, f"hs{i}") for i in range(2)]
    t_hs = [Tk(f"hs{i}") for i in range(2)]
    gs = [P.sb([128, 4], F32, f"gs{i}") for i in range(2)]
    t_gs = [Tk(f"gs{i}") for i in range(2)]
    st = P.sb([128, 4, 6], F32, "st")
    mv = P.sb([128, 8], F32, "mv")
    t_st = Tk("st")
    P.op("dve", lambda e: e.memset(mv[:, 4:5], 1e-5), writes=[t_st])
    for b in range(nblk):
        i = b % 2
        P.dma("sp", ys[i][:], y4[b * 128:(b + 1) * 128], writes=[t_ys[i]])
        P.dma("sp", hs[i][:], h[b * 128:(b + 1) * 128, :], writes=[t_hs[i]])
        P.dma("sp", gs[i][:], g4[b * 128:(b + 1) * 128, :], writes=[t_gs[i]])
        P.op("act", lambda e, i=i: e.mul(hs[i][:], hs[i][:], float(alpha)), reads=[t_hs[i]], writes=[t_hs[i]])
        for k in range(4):
            P.op("dve", lambda e, i=i, k=k: e.scalar_tensor_tensor(
                out=hs[i][:], in0=ys[i][:, k, :], scalar=gs[i][:, k:k + 1], in1=hs[i][:], op0=ALU.mult, op1=ALU.add),
                reads=[t_ys[i], t_gs[i], t_hs[i]], writes=[t_hs[i]])
        emit_ln(P, hs[i], t_hs[i], lng_sb, lnb_sb, t_gb, st, mv, t_st)
        P.dma("sp", out[b * 128:(b + 1) * 128, :], hs[i][:], reads=[t_hs[i]], writes=[Tk()], chan=t_hs[i])
    P.emit()
    return nc


FOX_KEYS = ("aqT", "akT", "av", "af", "bfg", "dfox", "ind", "tri", "ident4")
DSA_KEYS = ("iqT", "ikT2", "iw", "bqT", "bkT", "bv", "dadm", "ident4")


def scatter_rows(parts, width, dtype):
    full = np.empty((S, width), dtype)
    for c in range(NCORES):
        full[own_tokens(c)] = parts[c]
    return full


def kernel(x, w_in, b_forget, w_branch_a, w_branch_b, w_out, ln1_g, ln1_b, w_router, b_router,
           w_gate_up, b_gate_up, w_down, b_down, ln2_g, ln2_b):
    x2 = np.asarray(x, np.float32)[0]
    resA = launch_A(x2, np.asarray(w_in, np.float32)[0])
    pbf = scatter_rows([r[0] for r in resA], NBF, NPBF)
    pf = scatter_rows([r[1] for r in resA], NF, np.float32)
    del resA
    no = _new_offsets()
    t = {k: pbf[:, no[k]:no[k] + SZ[k]] for k in ORD_BF}
    t["af"] = pf[:, no["af"]:no["af"] + 8]
    t["iw"] = pf[:, no["iw"]:no["iw"] + 16]
    t["b_forget"] = np.asarray(b_forget, np.float32)[0]
    ga = pf[:, no["ga"]:no["ga"] + 2048]
    gb = pf[:, no["gb"]:no["gb"] + 2048]
    maps = [prep_B(c, t) for c in range(NCORES)]
    resF = run_spmd(build_B(part="fox"), [{k: m[k] for k in FOX_KEYS} for m in maps])
    a_parts = [r["ab"] for r in resF]
    resD = run_spmd(build_B(part="dsa"), [{k: m[k] for k in DSA_KEYS} for m in maps])
    b_parts = [r["ab"] for r in resD]
    del maps, resF, resD
    eye = np.eye(128, dtype=np.float32)
    lng1 = np.ascontiguousarray(np.broadcast_to(np.asarray(ln1_g, np.float32)[0], (128, 2048)))
    lnb1 = np.ascontiguousarray(np.broadcast_to(np.asarray(ln1_b, np.float32)[0], (128, 2048)))
    mapsC = []
    for c in range(NCORES):
        tok = own_tokens(c)
        abc = np.concatenate([a_parts[c], b_parts[c]], 1)
        mapsC.append(dict(
            abT=np.ascontiguousarray(abc.reshape(NQ, 16, 128).transpose(2, 1, 0)),
            gaT=np.ascontiguousarray(ga[tok].T.reshape(16, 128, NQ)),
            gbT=np.ascontiguousarray(gb[tok].T.reshape(16, 128, NQ)),
            wa=np.asarray(w_branch_a, np.float32)[0], wb=np.asarray(w_branch_b, np.float32)[0],
            wo=np.asarray(w_out, np.float32)[0], xq=np.ascontiguousarray(x2[tok]), lng=lng1, lnb=lnb1,
            wr=np.asarray(w_router, np.float32)[0],
            br=np.ascontiguousarray(np.broadcast_to(np.asarray(b_router, np.float32)[0], (128, 32))), ident=eye))
    resC = run_spmd(build_C(), mapsC)
    h_full = scatter_rows([r["h"] for r in resC], 2048, np.float32)
    G_full = scatter_rows([r["G"] for r in resC], 32, np.float32)
    del mapsC, resC
    sel = G_full > 0
    pos = np.cumsum(sel, 0) - 1
    counts = sel.sum(0)
    cap = int(max(128, -(-int(counts.max()) // 128) * 128))
    wgu = np.asarray(w_gate_up)[0]
    wdn = np.asarray(w_down)[0]
    bgu = np.asarray(b_gate_up, np.float32)[0]
    bdn = np.asarray(b_down, np.float32)[0]
    mapsD = []
    for c in range(NCORES):
        xeT = np.zeros((4, 2048, cap), np.float32)
        for i in range(4):
            e = 4 * c + i
            idx = np.nonzero(sel[:, e])[0]
            xeT[i, :, :len(idx)] = h_full[idx].T
        mapsD.append(dict(
            xeT=xeT, wgu=np.ascontiguousarray(wgu[4 * c:4 * c + 4]),
            bgu=np.ascontiguousarray(bgu[4 * c:4 * c + 4].reshape(4, 32, 128).transpose(0, 2, 1)),
            wd=np.ascontiguousarray(wdn[4 * c:4 * c + 4]),
            bd=np.ascontiguousarray(np.broadcast_to(bdn[4 * c:4 * c + 4][:, None, :], (4, 128, 2048)))))
    resD2 = run_spmd(build_D(cap), mapsD)
    Y = np.concatenate([r["y"] for r in resD2], 0)
    del mapsD, resD2
    ek = np.argsort(~sel, axis=1, kind="stable")[:, :4]
    ar = np.arange(S)
    lng2 = np.ascontiguousarray(np.broadcast_to(np.asarray(ln2_g, np.float32)[0], (128, 2048)))
    lnb2 = np.ascontiguousarray(np.broadcast_to(np.asarray(ln2_b, np.float32)[0], (128, 2048)))
    mapsE = []
    for c in range(NCORES):
        tok = own_tokens(c)
        y4 = np.empty((NQ, 4, 2048), np.float32)
        g4 = np.empty((NQ, 4), np.float32)
        for k in range(4):
            e_k = ek[tok, k]
            valid = sel[tok, e_k]
            p_k = np.where(valid, pos[tok, e_k], 0)
            y4[:, k] = Y[e_k, p_k]
            g4[:, k] = G_full[tok, e_k]
        mapsE.append(dict(y4=y4, g4=g4, h=np.ascontiguousarray(h_full[tok]), lng=lng2, lnb=lnb2))
    resE = run_spmd(build_E(), mapsE)
    out = scatter_rows([r["out"] for r in resE], 2048, np.float32)
    return out[None]
```

```python
import contextlib
import numpy as np
import ml_dtypes
import concourse.bass as bass
import concourse.mybir as mybir
from concourse.bass_utils import run_bass_kernel_spmd

F32 = mybir.dt.float32
BF16 = mybir.dt.bfloat16
AF = mybir.ActivationFunctionType
ALU = mybir.AluOpType
AX = mybir.AxisListType
NPBF = ml_dtypes.bfloat16

NCORES = 8
S = 8192
D = 2048
NQ = S // NCORES
NB = S // 128
NJ = NQ // 128
KT = D // 128
NEG = -60000.0
EPOCH = 30000


class Tk:
    __slots__ = ("name", "w", "rs", "excl")

    def __init__(self, name="", excl=False):
        self.name = name
        self.w = None
        self.rs = []
        self.excl = excl


class Ctx:
    def __init__(self, nc):
        self.nc = nc
        self.esems = {e: [] for e in Prog.ENG}
        self.ecount = {e: 0 for e in Prog.ENG}
        self.nsem = 0

    def esem(self, e, idx):
        k = idx // EPOCH
        while len(self.esems[e]) <= k:
            self.esems[e].append(self.nc.alloc_semaphore(name=f"s_{e}{len(self.esems[e])}"))
            self.nsem += 1
        return self.esems[e][k], idx % EPOCH + 1

    def csem(self):
        self.nsem += 1
        return self.nc.alloc_semaphore(name=f"c{self.nsem}")


class Prog:
    ENG = ("pe", "act", "dve", "pool", "sp")

    def __init__(self, nc, ctx=None):
        self.nc = nc
        self.ctx = ctx or Ctx(nc)
        self.ops = []
        self.stack = contextlib.ExitStack()
        self.chan_count = {}
        self.nt = 0

    def sb(self, shape, dt, name=None):
        self.nt += 1
        return self.stack.enter_context(self.nc.sbuf_tensor(name or f"t{self.nt}", list(shape), dt))

    def ps(self, shape, dt, name=None):
        self.nt += 1
        return self.stack.enter_context(self.nc.psum_tensor(name or f"p{self.nt}", list(shape), dt))

    def _rec(self, eng, fn, reads, writes, dma=False, chan=None, inc=16):
        writes = writes + [r for r in reads if r.excl and r not in writes]
        reads = [r for r in reads if not r.excl]
        deps = set()
        for r in reads:
            if r.w is not None:
                deps.add(r.w)
        for w in writes:
            if w.w is not None:
                deps.add(w.w)
            deps.update(w.rs)
        i = len(self.ops)
        deps.discard(i)
        if dma:
            if chan is None:
                chan = writes[0]
            ckey = (id(chan), eng, inc)
            n = self.chan_count.get(ckey, 0) + inc
            self.chan_count[ckey] = n
            tokv = n
        else:
            tokv = None
        self.ops.append(dict(eng=eng, fn=fn, deps=deps, dma=dma, chan=ckey if dma else None, tokv=tokv, inc=inc))
        for r in reads:
            r.rs.append(i)
        for w in writes:
            w.w = i
            w.rs = []
        return i

    def op(self, eng, fn, reads=(), writes=()):
        return self._rec(eng, fn, list(reads), list(writes))

    def dma(self, eng, out, in_, reads=(), writes=(), chan=None):
        return self._rec(eng, lambda e: e.dma_start(out=out, in_=in_), list(reads), list(writes), dma=True, chan=chan)

    def dma_fn(self, eng, fn, reads=(), writes=(), chan=None, inc=16):
        return self._rec(eng, fn, list(reads), list(writes), dma=True, chan=chan, inc=inc)

    def emit(self):
        nc = self.nc
        ctx = self.ctx
        ops = self.ops
        n = len(ops)
        per_eng = {e: [] for e in self.ENG}
        for i, o in enumerate(ops):
            per_eng[o["eng"]].append(i)
        need = [False] * n
        for i, o in enumerate(ops):
            for d in o["deps"]:
                od = ops[d]
                if od["dma"]:
                    continue
                if od["eng"] == o["eng"] and o["eng"] == "pe" and not o["dma"]:
                    continue
                need[d] = True
        for e in self.ENG:
            comp = [i for i in per_eng[e] if not ops[i]["dma"]]
            if comp:
                need[comp[-1]] = True
        last_chan = {}
        for i, o in enumerate(ops):
            if o["dma"]:
                last_chan[o["chan"]] = i
        sig = [None] * n
        base = dict(ctx.ecount)
        cnt = dict(ctx.ecount)
        for i, o in enumerate(ops):
            if not o["dma"] and need[i]:
                sig[i] = cnt[o["eng"]]
                cnt[o["eng"]] += 1
        csems = {ck: ctx.csem() for ck in self.chan_count}

        def run_engine(ename, e):
            known_e = {x: base[x] - 1 for x in self.ENG}
            known_c = {}
            for i in per_eng[ename]:
                o = ops[i]
                waits_e = {}
                waits_c = {}
                for d in o["deps"]:
                    od = ops[d]
                    if od["dma"]:
                        if od["tokv"] > known_c.get(od["chan"], 0):
                            waits_c[od["chan"]] = max(waits_c.get(od["chan"], 0), od["tokv"])
                    else:
                        if od["eng"] == ename and ename == "pe" and not o["dma"]:
                            continue
                        if sig[d] is not None and sig[d] > known_e[od["eng"]]:
                            waits_e[od["eng"]] = max(waits_e.get(od["eng"], -1), sig[d])
                for en, idx in waits_e.items():
                    sm, v = ctx.esem(en, idx)
                    e.wait_ge(sm, v)
                    known_e[en] = idx
                for ck, v in waits_c.items():
                    e.wait_ge(csems[ck], v)
                    known_c[ck] = v
                ins = o["fn"](e)
                if o["dma"]:
                    ins.then_inc(csems[o["chan"]], o["inc"])
                elif sig[i] is not None:
                    sm, v = ctx.esem(ename, sig[i])
                    ins.then_inc(sm, 1)
            for en in self.ENG:
                if cnt[en] > base[en] and cnt[en] - 1 > known_e[en]:
                    sm, v = ctx.esem(en, cnt[en] - 1)
                    e.wait_ge(sm, v)
            for ck, i in last_chan.items():
                v = ops[i]["tokv"]
                if v > known_c.get(ck, 0):
                    e.wait_ge(csems[ck], v)

        with nc.Block() as block:
            @block.tensor
            def _(e):
                run_engine("pe", e)

            @block.scalar
            def _(e):
                run_engine("act", e)

            @block.vector
            def _(e):
                run_engine("dve", e)

            @block.gpsimd
            def _(e):
                run_engine("pool", e)

            @block.sync
            def _(e):
                run_engine("sp", e)
        ctx.ecount = cnt
        self.stack.close()


def run_spmd(nc, in_maps):
    res = run_bass_kernel_spmd(nc, in_maps, core_ids=list(range(NCORES)))
    if getattr(res, "exec_time_ns", None):
        print(f"[launch] exec_time_ns={res.exec_time_ns}", flush=True)
    return res.results


SZ = dict(aq=1024, ak=1024, av=1024, af=8, bq=1024, bk=256, bv=256, iq=1024, ik=64, iw=16, ga=2048, gb=2048)
ORIG = ["aq", "ak", "av", "af", "bq", "bk", "bv", "iq", "ik", "iw", "ga", "gb"]
ORD_BF = ["bq", "bk", "iq", "ik", "aq", "ak", "av", "bv"]
ORD_F = ["af", "iw", "ga", "gb"]
NBF = sum(SZ[k] for k in ORD_BF)
NF = sum(SZ[k] for k in ORD_F)


def _orig_offsets():
    o, off = {}, 0
    for k in ORIG:
        o[k] = off
        off += SZ[k]
    return o


def _new_offsets():
    o, off = {}, 0
    for k in ORD_BF:
        o[k] = off
        off += SZ[k]
    off = 0
    for k in ORD_F:
        o[k] = off
        off += SZ[k]
    return o


def a_chunks():
    ch = []
    col = 0
    outc = 0
    for k in ORD_BF:
        kind = "rope128" if k in ("bq", "bk") else ("rope64" if k in ("iq", "ik") else "plain")
        n = SZ[k]
        o = 0
        while o < n:
            w = min(512, n - o)
            ch.append((col + o, w, kind, "bf", outc + o))
            o += w
        col += n
        outc += n
    ch.append((col, 24, "plain", "f", 0))
    col += 24
    outc = 24
    for k in ("ga", "gb"):
        for o in range(0, SZ[k], 512):
            ch.append((col + o, 512, "plain", "f", outc + o))
        col += SZ[k]
        outc += SZ[k]
    return ch


def build_A(nq=NQ, chunks=None):
    chunks = chunks or a_chunks()
    ncol = max(c[0] + c[1] for c in chunks)
    nbf = max([c[4] + c[1] for c in chunks if c[3] == "bf"] + [2])
    nf = max([c[4] + c[1] for c in chunks if c[3] == "f"] + [2])
    nblk = nq // 128
    nc = bass.Bass("TRN2", target_bir_lowering=False)
    xT = nc.dram_tensor("xT", [D, nq], F32, kind="ExternalInput").ap()
    w = nc.dram_tensor("w", [D, ncol], F32, kind="ExternalInput").ap()
    cs128 = nc.dram_tensor("cs128", [nq, 2, 4, 16], F32, kind="ExternalInput").ap()
    cs64 = nc.dram_tensor("cs64", [nq, 2, 8, 8], F32, kind="ExternalInput").ap()
    pbf = nc.dram_tensor("pbf", [nq, nbf], BF16, kind="ExternalOutput").ap()
    pf = nc.dram_tensor("pf", [nq, nf], F32, kind="ExternalOutput").ap()
    P = Prog(nc)
    xb = P.sb([128, KT, nq], BF16, "xb")
    t_xb = Tk("xb")
    xTv = xT.rearrange("(kt p) s -> p kt s", p=128)
    for h in range(2):
        P.dma("pool", xb[:, h * 8:(h + 1) * 8, :], xTv[:, h * 8:(h + 1) * 8, :], writes=[t_xb], chan=t_xb)
    c128 = P.sb([128, nblk, 2, 4, 16], F32, "c128")
    c64 = P.sb([128, nblk, 2, 8, 8], F32, "c64")
    t_cs = Tk("cs")
    P.dma("sp", c128[:], cs128.rearrange("(b p) a h r -> p b a h r", p=128), writes=[t_cs], chan=t_cs)
    P.dma("sp", c64[:], cs64.rearrange("(b p) a h r -> p b a h r", p=128), writes=[t_cs], chan=t_cs)
    NW = 3
    wb = [P.sb([128, KT, 512], BF16, f"wb{i}") for i in range(NW)]
    t_wb = [Tk(f"wb{i}") for i in range(NW)]
    NPS = 4
    psm = [P.ps([128, 512], F32, f"psA{i}") for i in range(NPS)]
    t_ps = [Tk(f"ps{i}", excl=True) for i in range(NPS)]
    NO = 4
    obf = [P.sb([128, 512], BF16, f"obf{i}") for i in range(NO)]
    of = [P.sb([128, 512], F32, f"of{i}") for i in range(NO)]
    t_o = [Tk(f"o{i}") for i in range(NO)]
    tmp = [P.sb([128, 8, 16], F32, f"rtmp{i}") for i in range(4)]
    t_tmp = Tk("rtmp")
    wv = w.rearrange("(kt p) c -> p kt c", p=128)
    it = 0
    for ci, (c0, cw, kind, grp, oc) in enumerate(chunks):
        wi = ci % NW
        for h in range(2):
            P.dma("pool", wb[wi][:, h * 8:(h + 1) * 8, :cw], wv[:, h * 8:(h + 1) * 8, c0:c0 + cw],
                  writes=[t_wb[wi]], chan=t_wb[wi])
        for b in range(nblk):
            pi = it % NPS
            oi = it % NO
            it += 1
            ps = psm[pi]
            for kt in range(KT):
                P.op("pe", lambda e, ps=ps, kt=kt, b=b, wi=wi, cw=cw: e.matmul(
                    ps[:, :cw], xb[:, kt, b * 128:(b + 1) * 128], wb[wi][:, kt, :cw],
                    start=(kt == 0), stop=(kt == KT - 1)),
                    reads=[t_xb, t_wb[wi]], writes=[t_ps[pi]])
            ot = obf[oi] if grp == "bf" else of[oi]
            if kind == "plain":
                eng = "act" if (it % 2 == 0) else "dve"
                if eng == "act":
                    P.op("act", lambda e, ot=ot, ps=ps, cw=cw: e.copy(ot[:, :cw], ps[:, :cw]),
                         reads=[t_ps[pi]], writes=[t_o[oi]])
                else:
                    P.op("dve", lambda e, ot=ot, ps=ps, cw=cw: e.tensor_copy(ot[:, :cw], ps[:, :cw]),
                         reads=[t_ps[pi]], writes=[t_o[oi]])
            else:
                hd, r = (128, 16) if kind == "rope128" else (64, 8)
                nh = cw // hd
                cst = c128 if kind == "rope128" else c64
                pv = ps[:, :cw].rearrange("p (h d) -> p h d", d=hd)
                ov = ot[:, :cw].rearrange("p (h d) -> p h d", d=hd)
                x1, x2 = pv[:, :, 0:r], pv[:, :, r:2 * r]
                cc, ss = cst[:, b, 0, :nh, :], cst[:, b, 1, :nh, :]
                tv = [t[:, :nh, :r] for t in tmp]
                P.op("act", lambda e, ov=ov, pv=pv, r=r: e.copy(ov[:, :, 2 * r:], pv[:, :, 2 * r:]),
                     reads=[t_ps[pi]], writes=[t_o[oi]])
                P.op("dve", lambda e, a=tv[0], x=x1, c=cc: e.tensor_tensor(a, x, c, ALU.mult),
                     reads=[t_ps[pi], t_cs], writes=[t_tmp])
                P.op("dve", lambda e, a=tv[1], x=x2, c=ss: e.tensor_tensor(a, x, c, ALU.mult),
                     reads=[t_ps[pi], t_cs], writes=[t_tmp])
                P.op("dve", lambda e, a=tv[2], x=x2, c=cc: e.tensor_tensor(a, x, c, ALU.mult),
                     reads=[t_ps[pi], t_cs], writes=[t_tmp])
                P.op("dve", lambda e, a=tv[3], x=x1, c=ss: e.tensor_tensor(a, x, c, ALU.mult),
                     reads=[t_ps[pi], t_cs], writes=[t_tmp])
                P.op("dve", lambda e, o=ov[:, :, 0:r], a=tv[0], b_=tv[1]: e.tensor_tensor(o, a, b_, ALU.subtract),
                     reads=[t_tmp], writes=[t_o[oi]])
                P.op("dve", lambda e, o=ov[:, :, r:2 * r], a=tv[2], b_=tv[3]: e.tensor_tensor(o, a, b_, ALU.add),
                     reads=[t_tmp], writes=[t_o[oi]])
            dst = pbf if grp == "bf" else pf
            P.dma("sp", dst[b * 128:(b + 1) * 128, oc:oc + cw], ot[:, :cw], reads=[t_o[oi]], writes=[Tk()], chan=t_o[oi])
    P.emit()
    return nc


def rope_tables(pos, rot_dim, theta=500000.0):
    inv = np.power(np.float32(theta), -np.arange(0, rot_dim, 2, dtype=np.float32) / np.float32(rot_dim)).astype(np.float32)
    ang = pos.astype(np.float32)[:, None] * inv[None, :]
    return np.cos(ang).astype(np.float32), np.sin(ang).astype(np.float32)


def own_tokens(c):
    return np.concatenate([np.arange((8 * j + c) * 128, (8 * j + c + 1) * 128) for j in range(NJ)])


def perm_w_in(w_in):
    oo = _orig_offsets()
    cols = []
    for k in ORD_BF + ORD_F:
        cols.append(np.arange(oo[k], oo[k] + SZ[k]))
    return np.ascontiguousarray(w_in[:, np.concatenate(cols)])


def launch_A(x, w_in):
    wp = perm_w_in(w_in)
    nc = build_A()
    in_maps = []
    for c in range(NCORES):
        tok = own_tokens(c)
        cos, sin = rope_tables(tok, 32)
        cs128 = np.stack([np.repeat(cos[:, None, :], 4, 1), np.repeat(sin[:, None, :], 4, 1)], 1)
        cos, sin = rope_tables(tok, 16)
        cs64 = np.stack([np.repeat(cos[:, None, :], 8, 1), np.repeat(sin[:, None, :], 8, 1)], 1)
        in_maps.append(dict(xT=np.ascontiguousarray(x[tok].T), w=wp,
                            cs128=np.ascontiguousarray(cs128, dtype=np.float32),
                            cs64=np.ascontiguousarray(cs64, dtype=np.float32)))
    res = run_spmd(nc, in_maps)
    return [(r["pbf"], r["pf"]) for r in res]


BIS_R = 16.0


def build_B(s=S, nsel=256, nheads=8, nbis=26, part="fox"):
    fox = part == "fox"
    dsa = part == "dsa"
    nb = s // 128
    nq = s // NCORES
    nj = nq // 128
    scale = 128 ** -0.5
    idx_scale = (16 ** -0.5) * (64 ** -0.5)
    nc = bass.Bass("TRN2", target_bir_lowering=False)

    def din(name, shape, dt):
        return nc.dram_tensor(name, list(shape), dt, kind="ExternalInput").ap()
    if fox:
        aqT = din("aqT", [128, 8, nq], BF16)
        akT = din("akT", [8, 128, s], BF16)
        av = din("av", [8, 128, nb, 128], BF16)
        af = din("af", [128, nb, 8], F32)
        bfg = din("bfg", [128, 8], F32)
        dfox = din("dfox", [128, 8, 128], BF16)
        ind = din("ind", [128, nj, nb, 8], F32)
        tri = din("tri", [128, 128], F32)
    if dsa:
        iqT = din("iqT", [128, 8, nq], BF16)
        ikT2 = din("ikT2", [128, s], BF16)
        iw = din("iw", [128, nj, 16], F32)
        bqT = din("bqT", [128, nj, 8, 128], BF16)
        bkT = din("bkT", [128, 2, s], BF16)
        bv = din("bv", [128, nb, 2, 128], BF16)
        dadm = din("dadm", [128, 8, 128], F32)
    ident4 = din("ident4", [128, 512], BF16)
    ab = nc.dram_tensor("ab", [nq, 1024], BF16, kind="ExternalOutput").ap()

    P = Prog(nc)
    banks = [P.ps([128, 512], F32, f"bank{i}") for i in range(8)]
    t_bank = [Tk(f"bank{i}", excl=True) for i in range(8)]

    def load(name, src, shape, dt, eng="sp"):
        t = P.sb(shape, dt, name)
        tk = Tk(name)
        P.dma(eng, t[:], src, writes=[tk])
        return t, tk
    id_sb, t_id = load("id_sb", ident4, [128, 512], BF16)
    if fox:
        aq_sb, t_aq = load("aq_sb", aqT, [128, 8, nq], BF16)
        af_sb, t_af = load("af_sb", af, [128, nb, 8], F32)
        bfg_sb, t_bfg = load("bfg_sb", bfg, [128, 8], F32)
        dfox_sb, t_dfox = load("dfox_sb", dfox, [128, 8, 128], BF16)
        ind_sb, t_ind = load("ind_sb", ind, [128, nj, nb, 8], F32)
        tri_sb, t_tri = load("tri_sb", tri, [128, 128], F32)
    ones_sb = P.sb([128, 128], F32, "ones_sb")
    t_ones = Tk("ones")
    P.op("pool", lambda e: e.memset(ones_sb[:], 1.0), writes=[t_ones])
    if fox:
        ab_sb = P.sb([128, nj, 1024], BF16, "ab_sb")
    t_ab = Tk("ab")

    rec = P.sb([128, 8], F32, "rec")
    t_rec = Tk("rec")
    if fox:
      L = P.sb([128, nb, 8], F32, "L")
      t_L = Tk("L")
      for b in range(nb):
          P.op("dve", lambda e, b=b: e.tensor_tensor(L[:, b, :], af_sb[:, b, :], bfg_sb[:], ALU.add),
               reads=[t_af, t_bfg], writes=[t_L])
      Lf = L[:].rearrange("p b h -> p (b h)")
      P.op("act", lambda e: e.activation(out=Lf, in_=Lf, func=AF.Exp, scale=-1.0), reads=[t_L], writes=[t_L])
      P.op("act", lambda e: e.activation(out=Lf, in_=Lf, func=AF.Ln, bias=1.0), reads=[t_L], writes=[t_L])
      CP = P.sb([128, nb, 8], F32, "CP")
      TOT = P.sb([128, nb, 8], F32, "TOT")
      PRE = P.sb([128, nb, 8], F32, "PRE")
      t_CP, t_TOT, t_PRE = Tk("CP"), Tk("TOT"), Tk("PRE")
      ncol = nb * 8
      for c0 in range(0, ncol, 512):
          cw = min(512, ncol - c0)
          P.op("pe", lambda e, c0=c0, cw=cw: e.matmul(banks[0][:, :cw], tri_sb[:], Lf[:, c0:c0 + cw], start=True, stop=True),
               reads=[t_tri, t_L], writes=[t_bank[0]])
          P.op("dve", lambda e, c0=c0, cw=cw: e.tensor_copy(CP[:].rearrange("p b h -> p (b h)")[:, c0:c0 + cw], banks[0][:, :cw]),
               reads=[t_bank[0]], writes=[t_CP])
          P.op("pe", lambda e, c0=c0, cw=cw: e.matmul(banks[1][:, :cw], ones_sb[:], Lf[:, c0:c0 + cw], start=True, stop=True),
               reads=[t_ones, t_L], writes=[t_bank[1]])
          P.op("dve", lambda e, c0=c0, cw=cw: e.tensor_copy(TOT[:].rearrange("p b h -> p (b h)")[:, c0:c0 + cw], banks[1][:, :cw]),
               reads=[t_bank[1]], writes=[t_TOT])
      P.op("dve", lambda e: e.memset(PRE[:, 0, :], 0.0), writes=[t_PRE])
      for b in range(1, nb):
          P.op("dve", lambda e, b=b: e.tensor_tensor(PRE[:, b, :], PRE[:, b - 1, :], TOT[:, b - 1, :], ALU.add),
               reads=[t_TOT, t_PRE], writes=[t_PRE])
      P.op("dve", lambda e: e.tensor_tensor(CP[:], CP[:], PRE[:], ALU.add), reads=[t_CP, t_PRE], writes=[t_CP])
      cref = P.sb([128, nj, 8], F32, "cref")
      t_cref = Tk("cref")
      tmpi = P.sb([128, nb, 8], F32, "tmpi")
      t_tmpi = Tk("tmpi")
      for j in range(nj):
          P.op("dve", lambda e, j=j: e.tensor_tensor(tmpi[:], TOT[:], ind_sb[:, j, :, :], ALU.mult),
               reads=[t_TOT, t_ind], writes=[t_tmpi])
          P.op("dve", lambda e, j=j: e.tensor_reduce(cref[:, j, :], tmpi[:].rearrange("p b h -> p h b"), AX.X, ALU.add),
               reads=[t_tmpi], writes=[t_cref])
      biasJ = P.sb([128, nj, nb, 8], F32, "biasJ")
      t_bias = Tk("biasJ")
      for j in range(nj):
          nk = NCORES * (j + 1)
          for h in range(8):
              P.op("dve", lambda e, j=j, h=h, nk=nk: e.tensor_scalar(
                  biasJ[:, j, :nk, h], CP[:, :nk, h], cref[:, j, h:h + 1], None, ALU.subtract),
                  reads=[t_CP, t_cref], writes=[t_bias])

      kT = [P.sb([128, s], BF16, f"kT{i}") for i in range(2)]
      t_kT = [Tk(f"kT{i}") for i in range(2)]
      vt = [P.sb([128, nb, 129], BF16, f"vt{i}") for i in range(2)]
      t_vt = [Tk(f"vt{i}") for i in range(2)]
      for i in range(2):
          P.op("pool", lambda e, i=i: e.memset(vt[i][:, :, 128:129], 1.0), writes=[t_vt[i]])
      pT = [P.sb([128, 128], BF16, f"pT{i}") for i in range(4)]
      t_pT = [Tk(f"pT{i}") for i in range(4)]
      ipt = 0
      for h in range(nheads):
          hi = h % 2
          P.dma("sp", kT[hi][:], akT[h], writes=[t_kT[hi]])
          P.dma("sp", vt[hi][:, :, 0:128], av[h], writes=[t_vt[hi]])
          for j in range(nj):
              nk = NCORES * (j + 1)
              ob = 2 + (j % 2)
              SB = (0, 1, 4, 5)
              LA = 2

              def qk(kb, j=j, h=h, hi=hi, nk=nk):
                  sbk = SB[(tile0 + kb) % 4]
                  diag = kb >= nk - NCORES
                  P.op("pe", lambda e, sbk=sbk, diag=diag: e.matmul(
                      banks[sbk][:, :128], kT[hi][:, kb * 128:(kb + 1) * 128], aq_sb[:, h, j * 128:(j + 1) * 128],
                      start=True, stop=not diag), reads=[t_kT[hi], t_aq], writes=[t_bank[sbk]])
                  if diag:
                      P.op("pe", lambda e, sbk=sbk: e.matmul(
                          banks[sbk][:, :128], id_sb[:, :128], dfox_sb[:, kb - (nk - NCORES), :],
                          start=False, stop=True), reads=[t_id, t_dfox], writes=[t_bank[sbk]])
              tile0 = ipt
              for kb in range(min(LA, nk)):
                  qk(kb)
              for kb in range(nk):
                  if kb + LA < nk:
                      qk(kb + LA)
                  sbk = SB[(tile0 + kb) % 4]
                  pi = (tile0 + kb) % 4
                  P.op("act", lambda e, pi=pi, sbk=sbk, j=j, kb=kb, h=h: e.activation(
                      out=pT[pi][:, :128], in_=banks[sbk][:, :128], func=AF.Exp,
                      bias=biasJ[:, j, kb, h:h + 1], scale=scale),
                      reads=[t_bank[sbk], t_bias], writes=[t_pT[pi]])
                  P.op("pe", lambda e, ob=ob, pi=pi, hi=hi, kb=kb, nk=nk: e.matmul(
                      banks[ob][:, :129], pT[pi][:, :128], vt[hi][:, kb, :],
                      start=(kb == 0), stop=(kb == nk - 1)), reads=[t_pT[pi], t_vt[hi]], writes=[t_bank[ob]])
              ipt += nk
              P.op("dve", lambda e, ob=ob: e.reciprocal(rec[:, 0:1], banks[ob][:, 128:129]),
                   reads=[t_bank[ob]], writes=[t_rec])
              P.op("dve", lambda e, ob=ob, j=j, h=h: e.tensor_scalar(
                  ab_sb[:, j, h * 128:(h + 1) * 128], banks[ob][:, :128], rec[:, 0:1], None, ALU.mult),
                  reads=[t_bank[ob], t_rec], writes=[t_ab])

    if dsa:
      ik_sb, t_ik = load("ik_sb", ikT2, [128, s], BF16)
      iw_sb, t_iw = load("iw_sb", iw, [128, nj, 16], F32)
      bk_sb, t_bk = load("bk_sb", bkT, [128, 2, s], BF16)
      dadm_sb, t_dadm = load("dadm_sb", dadm, [128, 8, 128], F32)
      bv_sb = P.sb([128, nb, 2, 129], BF16, "bv_sb")
      t_bv = Tk("bv")
      P.op("pool", lambda e: e.memset(bv_sb[:, :, :, 128:129], 1.0), writes=[t_bv])
      P.dma("sp", bv_sb[:, :, :, 0:128], bv, writes=[t_bv])
      scl = P.sb([128, nj, 16], F32, "scl")
      sgn = P.sb([128, nj, 16], F32, "sgn")
      t_scl, t_sgn = Tk("scl"), Tk("sgn")
      P.op("dve", lambda e: e.tensor_scalar(sgn[:], iw_sb[:], 0.0, 2.0, ALU.is_ge, ALU.mult),
           reads=[t_iw], writes=[t_sgn])
      P.op("dve", lambda e: e.tensor_scalar(sgn[:], sgn[:], -1.0, None, ALU.add), reads=[t_sgn], writes=[t_sgn])
      P.op("dve", lambda e: e.tensor_tensor(scl[:], iw_sb[:], sgn[:], ALU.mult), reads=[t_iw, t_sgn], writes=[t_scl])
      P.op("dve", lambda e: e.tensor_scalar(scl[:], scl[:], idx_scale, None, ALU.mult), reads=[t_scl], writes=[t_scl])
      wsc = P.sb([128, nj, 16], F32, "wsc")
      P.op("dve", lambda e: e.tensor_scalar(wsc[:], iw_sb[:], idx_scale, None, ALU.mult), reads=[t_iw, t_scl], writes=[t_scl])
      score = [P.sb([128, s], F32, f"score{i}") for i in range(2)]
      t_score = [Tk(f"score{i}") for i in range(2)]
      mb = P.sb([128, s], BF16, "mb")
      t_mb = Tk("mb")
      junk = P.sb([128, s], BF16, "junk")
      t_junk = Tk("junk")
      iqs = [P.sb([128, 8, 128], BF16, f"iqs{i}") for i in range(2)]
      t_iqs = [Tk(f"iqs{i}") for i in range(2)]
      bqs = [P.sb([128, 8, 128], BF16, f"bqs{i}") for i in range(2)]
      t_bqs = [Tk(f"bqs{i}") for i in range(2)]
      dg0 = P.sb([128, 16, 128], BF16, "dg0")
      t_dg0 = Tk("dg0")
      dg = [dg0, dg0]
      t_dg = [t_dg0, t_dg0]
      rl = [P.sb([128, 512], BF16, f"rl{i}") for i in range(2)]
      t_rl = [Tk(f"rl{i}") for i in range(2)]
      pT4 = [P.sb([128, 512], BF16, f"pT4{i}") for i in range(3)]
      t_pT4 = [Tk(f"pT4{i}") for i in range(3)]
      outt = [P.sb([128, 1024], BF16, f"outt{i}") for i in range(2)]
      t_outt = [Tk(f"outt{i}") for i in range(2)]
      bisv = [P.sb([128, 8], F32, f"bis{i}") for i in range(2)]
      t_bisv = [Tk(f"bis{i}") for i in range(2)]
      rc = P.sb([128, 4], F32, "rc")
      t_rc = Tk("rc")
      st8 = dict(iz=0, iatt=0)

      def indexer(j):
          ji = j % 2
          sc, tsc = score[ji], t_score[ji]
          nk = NCORES * (j + 1)
          ngrp = nk * 128 // 512
          P.dma("sp", iqs[ji][:], iqT[:, :, j * 128:(j + 1) * 128], writes=[t_iqs[ji]])
          P.dma("sp", bqs[ji][:], bqT[:, j, :, :], writes=[t_bqs[ji]])
          for h in range(16):
              P.op("dve", lambda e, h=h: e.tensor_scalar(dg[ji][:, h, :], id_sb[:, :128], wsc[:, j, h:h + 1], None, ALU.mult),
                   reads=[t_id, t_scl], writes=[t_dg[ji]])
          for kg in range(ngrp):
              def zmm(h, kg=kg):
                  zb = (st8["iz"] + h) % 2
                  pr = (h % 2) * 64
                  P.op("pe", lambda e, zb=zb, pr=pr, h=h, kg=kg: e.matmul(
                      banks[zb][:, :512], iqs[ji][pr:pr + 64, h // 2, :],
                      ik_sb[pr:pr + 64, kg * 512:(kg + 1) * 512], start=True, stop=True),
                      reads=[t_iqs[ji], t_ik], writes=[t_bank[zb]])
              zmm(0)
              for h in range(16):
                  if h + 1 < 16:
                      zmm(h + 1)
                  zb = (st8["iz"] + h) % 2
                  P.op("act", lambda e, zb=zb, h=h: e.activation(
                      out=rl[zb][:], in_=banks[zb][:, :512], func=AF.Relu),
                      reads=[t_bank[zb]], writes=[t_rl[zb]])
                  P.op("pe", lambda e, zb=zb, h=h: e.matmul(
                      banks[6][:, :512], dg[ji][:, h, :], rl[zb][:], start=(h == 0), stop=(h == 15)),
                      reads=[t_dg[ji], t_rl[zb]], writes=[t_bank[6]])
              st8["iz"] += 16
              lastg = kg - (ngrp - 2)
              if lastg >= 0:
                  init = dadm_sb[:, lastg * 4:(lastg + 1) * 4, :].rearrange("p a k -> p (a k)")
                  P.op("dve", lambda e, kg=kg, init=init: e.tensor_tensor(
                      sc[:, kg * 512:(kg + 1) * 512], banks[6][:, :512], init, ALU.add),
                      reads=[t_bank[6], t_dadm], writes=[tsc])
              else:
                  P.op("dve", lambda e, kg=kg: e.tensor_copy(sc[:, kg * 512:(kg + 1) * 512], banks[6][:, :512]),
                       reads=[t_bank[6]], writes=[tsc])
              yield

      def bisection(j):
          ji = j % 2
          sc, tsc = score[ji], t_score[ji]
          bv_, tb = bisv[ji], t_bisv[ji]
          LO, MID, CNT, FL = [bv_[:, i:i + 1] for i in range(4)]
          nkeys = NCORES * (j + 1) * 128
          P.op("dve", lambda e: e.memset(LO, -BIS_R), writes=[tb])
          P.op("dve", lambda e: e.memset(MID, 0.0), writes=[tb])
          for it in range(nbis):
              hw_ = BIS_R / (2.0 ** it)
              P.op("dve", lambda e: e.tensor_scalar(
                  junk[:, :nkeys], sc[:, :nkeys], MID, None, ALU.is_ge, ALU.add, accum_out=CNT),
                  reads=[tsc, tb], writes=[tb, t_junk])
              P.op("dve", lambda e, hw_=hw_: e.tensor_scalar(FL, CNT, nsel - 0.5, hw_, ALU.is_ge, ALU.mult),
                   reads=[tb], writes=[tb])
              P.op("dve", lambda e: e.tensor_tensor(LO, LO, FL, ALU.add), reads=[tb], writes=[tb])
              P.op("dve", lambda e, hw_=hw_: e.tensor_scalar(MID, LO, hw_ * 0.5, None, ALU.add), reads=[tb], writes=[tb])
              yield
          P.op("dve", lambda e: e.tensor_scalar(mb[:, :nkeys], sc[:, :nkeys], LO, NEG, ALU.is_lt, ALU.mult),
               reads=[tsc, tb], writes=[t_mb])

      def attention(j):
          ji = j % 2
          nk = NCORES * (j + 1)
          oi = j % 2
          SB = (4, 5, 7)
          LA = 2
          for g in range(2):
              obs = (2, 3)

              def qk2(kb, g=g):
                  sbk = SB[(st8["iatt"] + kb) % 3]
                  P.op("pe", lambda e, sbk=sbk, g=g, kb=kb: e.matmul(
                      banks[sbk][:, :512], bk_sb[:, g, kb * 128:(kb + 1) * 128],
                      bqs[ji][:, g * 4:(g + 1) * 4, :].rearrange("p a q -> p (a q)"), start=True, stop=False),
                      reads=[t_bk, t_bqs[ji]], writes=[t_bank[sbk]])
                  P.op("pe", lambda e, sbk=sbk, kb=kb: e.matmul(
                      banks[sbk][:, :512], mb[:, kb * 128:(kb + 1) * 128], id_sb[:, :512], start=False, stop=True),
                      reads=[t_mb, t_id], writes=[t_bank[sbk]])
              for kb in range(min(LA, nk)):
                  qk2(kb)
              for kb in range(nk):
                  if kb + LA < nk:
                      qk2(kb + LA)
                  sbk = SB[(st8["iatt"] + kb) % 3]
                  pi = (st8["iatt"] + kb) % 3
                  P.op("act", lambda e, pi=pi, sbk=sbk: e.activation(
                      out=pT4[pi][:], in_=banks[sbk][:, :512], func=AF.Exp, scale=scale),
                      reads=[t_bank[sbk]], writes=[t_pT4[pi]])
                  for hh in range(4):
                      ob = obs[hh // 2]
                      c0 = (hh % 2) * 129
                      P.op("pe", lambda e, ob=ob, c0=c0, pi=pi, hh=hh, kb=kb, g=g: e.matmul(
                          banks[ob][:, c0:c0 + 129], pT4[pi][:, hh * 128:(hh + 1) * 128], bv_sb[:, kb, g, :],
                          start=(kb == 0 and hh % 2 == 0), stop=(kb == nk - 1), skip_group_check=True),
                          reads=[t_pT4[pi], t_bv], writes=[t_bank[ob]])
              st8["iatt"] += nk
              for hh in range(4):
                  ob = obs[hh // 2]
                  c0 = (hh % 2) * 129
                  head = g * 4 + hh
                  P.op("act", lambda e, ob=ob, c0=c0: e.activation(out=rc[:, 0:1], in_=banks[ob][:, c0 + 128:c0 + 129], func=AF.Ln),
                       reads=[t_bank[ob]], writes=[t_rc])
                  P.op("act", lambda e: e.activation(out=rc[:, 1:2], in_=rc[:, 0:1], func=AF.Exp, scale=-1.0),
                       reads=[t_rc], writes=[t_rc])
                  P.op("act", lambda e, ob=ob, c0=c0, head=head: e.activation(
                      out=outt[oi][:, head * 128:(head + 1) * 128], in_=banks[ob][:, c0:c0 + 128], func=AF.Copy, scale=rc[:, 1:2]),
                      reads=[t_bank[ob], t_rc], writes=[t_outt[oi]])
          P.dma("sp", ab[j * 128:(j + 1) * 128, :], outt[oi][:], reads=[t_outt[oi]], writes=[Tk()], chan=t_outt[oi])

      order = list(range(nj))[::-1]
      for _ in indexer(order[0]):
          pass
      for oi_, j in enumerate(order):
          bs = bisection(j)
          if oi_ + 1 < nj:
              jn = order[oi_ + 1]
              ng = NCORES * (jn + 1) * 128 // 512
              per = -(-nbis // ng)
              for _ in indexer(jn):
                  for _i in range(per):
                      next(bs, None)
          for _ in bs:
              pass
          attention(j)
    if fox:
        for j in range(nj):
            P.dma("sp", ab[j * 128:(j + 1) * 128, :], ab_sb[:, j, :], reads=[t_ab], writes=[Tk()], chan=t_ab)
    P.emit()
    return nc


def bf(a):
    return np.ascontiguousarray(a).astype(NPBF)


def prep_B(c, t, s=S):
    nb = s // 128
    nq = s // NCORES
    nj = nq // 128
    tok = np.concatenate([np.arange((NCORES * j + c) * 128, (NCORES * j + c + 1) * 128) for j in range(nj)])
    m = {}
    m["aqT"] = np.ascontiguousarray(t["aq"][tok].reshape(nq, 8, 128).transpose(2, 1, 0))
    m["akT"] = np.ascontiguousarray(t["ak"].reshape(s, 8, 128).transpose(1, 2, 0))
    m["av"] = np.ascontiguousarray(t["av"].reshape(nb, 128, 8, 128).transpose(2, 1, 0, 3))
    m["af"] = np.ascontiguousarray(t["af"].reshape(nb, 128, 8).transpose(1, 0, 2))
    m["bfg"] = np.ascontiguousarray(np.broadcast_to(t["b_forget"].reshape(1, 8), (128, 8))).astype(np.float32)
    kk = np.arange(8)[None, :, None]
    k = np.arange(128)[:, None, None]
    q = np.arange(128)[None, None, :]
    dfox = np.where(kk < c, 0.0, np.where(kk > c, NEG, np.where(k <= q, 0.0, NEG)))
    m["dfox"] = bf(np.broadcast_to(dfox, (128, 8, 128)))
    qq = np.arange(128)[:, None, None]
    k2 = np.arange(128)[None, None, :]
    dadm = np.where(kk < c, 0.0, np.where(kk > c, -1e30, np.where(k2 // 64 <= qq // 64, 0.0, -1e30)))
    m["dadm"] = np.ascontiguousarray(np.broadcast_to(dadm, (128, 8, 128))).astype(np.float32)
    ind = (np.arange(nb)[None, :] < (NCORES * np.arange(nj)[:, None] + c)).astype(np.float32)
    m["ind"] = np.ascontiguousarray(np.broadcast_to(ind[None, :, :, None], (128, nj, nb, 8))).astype(np.float32)
    m["iqT"] = np.ascontiguousarray(t["iq"][tok].reshape(nq, 8, 128).transpose(2, 1, 0))
    ikT = t["ik"].T
    m["ikT2"] = np.ascontiguousarray(np.concatenate([ikT, ikT], 0))
    m["iw"] = np.ascontiguousarray(t["iw"][tok].reshape(nj, 128, 16).transpose(1, 0, 2))
    m["bqT"] = np.ascontiguousarray(t["bq"][tok].reshape(nj, 128, 8, 128).transpose(3, 0, 2, 1))
    m["bkT"] = np.ascontiguousarray(t["bk"].reshape(s, 2, 128).transpose(2, 1, 0))
    m["bv"] = np.ascontiguousarray(t["bv"].reshape(nb, 128, 2, 128).transpose(1, 0, 2, 3))
    m["ident4"] = bf(np.tile(np.eye(128, dtype=np.float32), (1, 4)))
    m["tri"] = np.triu(np.ones((128, 128), np.float32))
    return m


def emit_ln(P, x, t_x, g_sb, b_sb, t_gb, st, mv, t_st, eps=1e-5):
    for k in range(4):
        P.op("dve", lambda e, k=k: e.bn_stats(st[:, k, :], x[:, k * 512:(k + 1) * 512]), reads=[t_x], writes=[t_st])
    P.op("dve", lambda e: e.bn_aggr(mv[:, 0:2], st[:].rearrange("p a b -> p (a b)")), reads=[t_st], writes=[t_st])
    P.op("act", lambda e: e.activation(out=mv[:, 2:3], in_=mv[:, 1:2], func=AF.Sqrt, bias=mv[:, 4:5], scale=1.0),
         reads=[t_st], writes=[t_st])
    P.op("dve", lambda e: e.reciprocal(mv[:, 3:4], mv[:, 2:3]), reads=[t_st], writes=[t_st])
    P.op("dve", lambda e: e.tensor_scalar(x[:], x[:], mv[:, 0:1], mv[:, 3:4], ALU.subtract, ALU.mult),
         reads=[t_x, t_st], writes=[t_x])
    P.op("dve", lambda e: e.tensor_tensor(x[:], x[:], g_sb[:], ALU.mult), reads=[t_x, t_gb], writes=[t_x])
    P.op("dve", lambda e: e.tensor_tensor(x[:], x[:], b_sb[:], ALU.add), reads=[t_x, t_gb], writes=[t_x])


def build_C(nq=NQ, alpha=2.0 ** 0.25):
    nblk = nq // 128
    ntg = max(nq // 512, 1)
    tgw = min(512, nq)
    nc = bass.Bass("TRN2", target_bir_lowering=False)

    def din(name, shape, dt):
        return nc.dram_tensor(name, list(shape), dt, kind="ExternalInput").ap()
    abT = din("abT", [128, 16, nq], BF16)
    gaT = din("gaT", [16, 128, nq], F32)
    gbT = din("gbT", [16, 128, nq], F32)
    wa = din("wa", [1024, 2048], F32)
    wb = din("wb", [1024, 2048], F32)
    wo = din("wo", [2048, 2048], F32)
    xq = din("xq", [nq, 2048], F32)
    lng = din("lng", [128, 2048], F32)
    lnb = din("lnb", [128, 2048], F32)
    wr = din("wr", [2048, 32], F32)
    br = din("br", [128, 32], F32)
    ident = din("ident", [128, 128], F32)
    h_out = nc.dram_tensor("h", [nq, 2048], F32, kind="ExternalOutput").ap()
    g_out = nc.dram_tensor("G", [nq, 32], F32, kind="ExternalOutput").ap()
    P = Prog(nc)
    banks = [P.ps([128, 512], F32, f"bank{i}") for i in range(8)]
    t_bank = [Tk(f"bank{i}", excl=True) for i in range(8)]

    def load(name, src, shape, dt, eng="sp"):
        t = P.sb(shape, dt, name)
        tk = Tk(name)
        P.dma(eng, t[:], src, writes=[tk])
        return t, tk
    ab_sb, t_abT = load("abT_sb", abT, [128, 16, nq], BF16)
    W_sb = P.sb([128, 16, 2048], BF16, "W_sb")
    t_wa, t_wb = Tk("wa"), Tk("wb")
    P.dma("pool", W_sb[:, 0:8, :], wa.rearrange("(kt p) n -> p kt n", p=128), writes=[t_wa])
    P.dma("pool", W_sb[:, 8:16, :], wb.rearrange("(kt p) n -> p kt n", p=128), writes=[t_wb])
    wa_sb = W_sb[:, 0:8, :]
    wb_sb = W_sb[:, 8:16, :]
    mT = P.sb([128, 16, nq], BF16, "mT")
    t_mT = Tk("mT")
    gsb = [P.sb([128, 2, nq], F32, f"gsb{i}") for i in range(2)]
    t_g = [Tk(f"g{i}") for i in range(2)]
    m12 = [[P.sb([128, 512], F32, f"m12{p_}_{i}") for i in range(2)] for p_ in range(2)]
    t_m12 = [Tk("m12a"), Tk("m12b")]
    itc = [0]
    for c in range(16):
        gi = c % 2
        P.dma("sp", gsb[gi][:, 0, :], gaT[c], writes=[t_g[gi]])
        P.dma("sp", gsb[gi][:, 1, :], gbT[c], writes=[t_g[gi]])
        P.op("act", lambda e, gi=gi: e.activation(out=gsb[gi][:], in_=gsb[gi][:], func=AF.Sigmoid),
             reads=[t_g[gi]], writes=[t_g[gi]])
        for tg in range(ntg):
            ts_ = slice(tg * tgw, (tg + 1) * tgw)
            pp = itc[0] % 2
            itc[0] += 1
            ba, bb = (0, 1) if pp == 0 else (2, 3)
            ma, mb_ = m12[pp]
            for kt in range(8):
                P.op("pe", lambda e, kt=kt, c=c, ts_=ts_, ba=ba: e.matmul(
                    banks[ba][:, :tgw], wa_sb[:, kt, c * 128:(c + 1) * 128], ab_sb[:, kt, ts_],
                    start=(kt == 0), stop=(kt == 7)), reads=[t_wa, t_abT], writes=[t_bank[ba]])
            for kt in range(8):
                P.op("pe", lambda e, kt=kt, c=c, ts_=ts_, bb=bb: e.matmul(
                    banks[bb][:, :tgw], wb_sb[:, kt, c * 128:(c + 1) * 128], ab_sb[:, 8 + kt, ts_],
                    start=(kt == 0), stop=(kt == 7)), reads=[t_wb, t_abT], writes=[t_bank[bb]])
            P.op("dve", lambda e, gi=gi, ts_=ts_, ba=ba, ma=ma: e.tensor_tensor(ma[:, :tgw], banks[ba][:, :tgw], gsb[gi][:, 0, ts_], ALU.mult),
                 reads=[t_bank[ba], t_g[gi]], writes=[t_m12[pp]])
            P.op("dve", lambda e, gi=gi, ts_=ts_, bb=bb, mb_=mb_: e.tensor_tensor(mb_[:, :tgw], banks[bb][:, :tgw], gsb[gi][:, 1, ts_], ALU.mult),
                 reads=[t_bank[bb], t_g[gi]], writes=[t_m12[pp]])
            P.op("dve", lambda e, c=c, ts_=ts_, ma=ma, mb_=mb_: e.tensor_tensor(mT[:, c, ts_], ma[:, :tgw], mb_[:, :tgw], ALU.add),
                 reads=[t_m12[pp]], writes=[t_mT])
    wo_sb = W_sb
    wov = wo.rearrange("(kt p) n -> p kt n", p=128)
    P.dma("pool", W_sb[:, 0:8, :], wov[:, 0:8, :], writes=[t_wa])
    P.dma("pool", W_sb[:, 8:16, :], wov[:, 8:16, :], writes=[t_wb])
    lng_sb, t_lng = load("lng_sb", lng, [128, 2048], F32)
    lnb_sb, t_lnb = load("lnb_sb", lnb, [128, 2048], F32)
    t_gb = Tk("gb")
    P.op("pool", lambda e: e.engine_nop(), reads=[t_lng, t_lnb], writes=[t_gb])
    wr_sb, t_wr = load("wr_sb", wr.rearrange("(kt p) n -> p kt n", p=128), [128, 16, 32], F32)
    br_sb, t_br = load("br_sb", br, [128, 32], F32)
    id_sb, t_id = load("id_sb", ident, [128, 128], F32)
    xs0 = P.sb([128, 2048], F32, "xs0")
    t_xs0 = Tk("xs0")
    xs = [xs0, xs0]
    t_xs = [t_xs0, t_xs0]
    hs = [P.sb([128, 2048], F32, f"hs{i}") for i in range(2)]
    t_hs = [Tk(f"hs{i}") for i in range(2)]
    st = P.sb([128, 4, 6], F32, "st")
    mv = P.sb([128, 8], F32, "mv")
    t_st = Tk("st")
    P.op("dve", lambda e: e.memset(mv[:, 4:5], 1e-5), writes=[t_st])
    hT = P.sb([128, 16, 128], F32, "hT")
    t_hT = Tk("hT")
    rt = P.sb([128, 4, 32], F32, "rt")
    m8 = P.sb([128, 16], F32, "m8")
    t_rt = Tk("rt")
    for b in range(nblk):
        i2 = b % 2
        P.dma("sp", xs[i2][:], xq[b * 128:(b + 1) * 128, :], writes=[t_xs[i2]])
        for n in range(4):
            for kt in range(16):
                P.op("pe", lambda e, n=n, kt=kt, b=b: e.matmul(
                    banks[4 + n][:, :512], mT[:, kt, b * 128:(b + 1) * 128], wo_sb[:, kt, n * 512:(n + 1) * 512],
                    start=(kt == 0), stop=(kt == 15)), reads=[t_mT, t_wa, t_wb], writes=[t_bank[4 + n]])
            P.op("dve", lambda e, n=n, i2=i2: e.scalar_tensor_tensor(
                out=hs[i2][:, n * 512:(n + 1) * 512], in0=xs[i2][:, n * 512:(n + 1) * 512], scalar=float(alpha),
                in1=banks[4 + n][:, :512], op0=ALU.mult, op1=ALU.add),
                reads=[t_xs[i2], t_bank[4 + n]], writes=[t_hs[i2]])
        emit_ln(P, hs[i2], t_hs[i2], lng_sb, lnb_sb, t_gb, st, mv, t_st)
        P.dma("sp", h_out[b * 128:(b + 1) * 128, :], hs[i2][:], reads=[t_hs[i2]], writes=[Tk()], chan=t_hs[i2])
        for kt in range(16):
            bk = kt % 2
            P.op("pe", lambda e, kt=kt, bk=bk, i2=i2: e.transpose(banks[bk][:, :128], hs[i2][:, kt * 128:(kt + 1) * 128], id_sb[:]),
                 reads=[t_hs[i2], t_id], writes=[t_bank[bk]])
            P.op("act", lambda e, kt=kt, bk=bk: e.copy(hT[:, kt, :], banks[bk][:, :128]), reads=[t_bank[bk]], writes=[t_hT])
        for kt in range(16):
            P.op("pe", lambda e, kt=kt: e.matmul(banks[2][:, :32], hT[:, kt, :], wr_sb[:, kt, :], start=(kt == 0), stop=(kt == 15)),
                 reads=[t_hT, t_wr], writes=[t_bank[2]])
        LG, EX, SEL, GG = rt[:, 0, :], rt[:, 1, :], rt[:, 2, :], rt[:, 3, :]
        P.op("dve", lambda e: e.tensor_tensor(LG, banks[2][:, :32], br_sb[:], ALU.add), reads=[t_bank[2], t_br], writes=[t_rt])
        P.op("dve", lambda e: e.max(m8[:, 0:8], LG), reads=[t_rt], writes=[t_rt])
        P.op("dve", lambda e: e.tensor_scalar(m8[:, 8:9], m8[:, 0:1], -1.0, None, ALU.mult), reads=[t_rt], writes=[t_rt])
        P.op("act", lambda e: e.activation(out=EX, in_=LG, func=AF.Exp, bias=m8[:, 8:9], scale=1.0), reads=[t_rt], writes=[t_rt])
        P.op("dve", lambda e: e.tensor_scalar(SEL, LG, m8[:, 3:4], None, ALU.is_ge), reads=[t_rt], writes=[t_rt])
        P.op("dve", lambda e: e.tensor_tensor(EX, EX, SEL, ALU.mult), reads=[t_rt], writes=[t_rt])
        P.op("dve", lambda e: e.tensor_reduce(m8[:, 9:10], EX, AX.X, ALU.add), reads=[t_rt], writes=[t_rt])
        P.op("dve", lambda e: e.reciprocal(m8[:, 10:11], m8[:, 9:10]), reads=[t_rt], writes=[t_rt])
        P.op("dve", lambda e: e.tensor_scalar(GG, EX, m8[:, 10:11], None, ALU.mult), reads=[t_rt], writes=[t_rt])
        P.dma("sp", g_out[b * 128:(b + 1) * 128, :], GG, reads=[t_rt], writes=[Tk()], chan=t_rt)
    P.emit()
    return nc


def build_D(cap, nexp=4):
    nc = bass.Bass("TRN2", target_bir_lowering=False)

    def din(name, shape, dt):
        return nc.dram_tensor(name, list(shape), dt, kind="ExternalInput").ap()
    xeT = din("xeT", [nexp, 2048, cap], F32)
    wgu = din("wgu", [nexp, 2048, 4096], F32)
    bgu = din("bgu", [nexp, 128, 32], F32)
    wd = din("wd", [nexp, 2048, 2048], F32)
    bd = din("bd", [nexp, 128, 2048], F32)
    y = nc.dram_tensor("y", [nexp, cap, 2048], F32, kind="ExternalOutput").ap()
    P = Prog(nc)
    banks = [P.ps([128, 512], F32, f"bank{i}") for i in range(8)]
    t_bank = [Tk(f"bank{i}", excl=True) for i in range(8)]
    xe = P.sb([128, 16, cap], BF16, "xe")
    t_xe = Tk("xe")
    actT = P.sb([128, 16, cap], BF16, "actT")
    t_act = Tk("actT")
    NW = 3
    wt = [P.sb([128, 16, 512], BF16, f"wt{i}") for i in range(NW)]
    t_wt = [Tk(f"wt{i}") for i in range(NW)]
    bg_sb = P.sb([128, 32], F32, "bg_sb")
    t_bg = Tk("bg")
    bd_sb = P.sb([128, 2048], F32, "bd_sb")
    t_bd = Tk("bd")
    ep = [[P.sb([128, 512], F32, f"ep{p_}_{i}") for i in range(4)] for p_ in range(2)]
    t_ep = [Tk("ep0"), Tk("ep1")]
    itd = [0]
    pend = [None]
    yo = [P.sb([128, 512], F32, f"yo{i}") for i in range(2)]
    t_yo = [Tk(f"yo{i}") for i in range(2)]
    ncg = -(-cap // 512)
    cgw = -(-(cap // ncg) // 64) * 64
    cgs = [(c0, min(cgw, cap - c0)) for c0 in range(0, cap, cgw)]
    iw_ = 0
    iy = 0
    for ex in range(nexp):
        for h in range(2):
            P.dma("pool", xe[:, h * 8:(h + 1) * 8, :], xeT[ex].rearrange("(kt p) c -> p kt c", p=128)[:, h * 8:(h + 1) * 8, :],
                  writes=[t_xe], chan=t_xe)
        P.dma("sp", bg_sb[:], bgu[ex], writes=[t_bg])
        P.dma("sp", bd_sb[:], bd[ex], writes=[t_bd])
        wv = wgu[ex].rearrange("(kt p) c -> p kt c", p=128)
        for q in range(4):
            wi = []
            for half in range(2):
                w_i = iw_ % NW
                iw_ += 1
                c0 = half * 2048 + q * 512
                for hh in range(2):
                    P.dma("pool", wt[w_i][:, hh * 8:(hh + 1) * 8, :], wv[:, hh * 8:(hh + 1) * 8, c0:c0 + 512],
                          writes=[t_wt[w_i]], chan=t_wt[w_i])
                wi.append(w_i)
            for f in range(4):
                ffc = q * 4 + f
                for (c0, cw) in cgs:
                    pp = itd[0] % 2
                    itd[0] += 1
                    bg, bu = (0, 1) if pp == 0 else (4, 5)
                    e0, e1, e2, e3 = ep[pp]
                    for half in range(2):
                        bk = bg if half == 0 else bu
                        for kt in range(16):
                            P.op("pe", lambda e, bk=bk, wi_=wi[half], kt=kt, f=f, c0=c0, cw=cw: e.matmul(
                                banks[bk][:, :cw], wt[wi_][:, kt, f * 128:(f + 1) * 128], xe[:, kt, c0:c0 + cw],
                                start=(kt == 0), stop=(kt == 15)), reads=[t_wt[wi[half]], t_xe], writes=[t_bank[bk]])
                    P.op("dve", lambda e, ffc=ffc, cw=cw, bg=bg, e0=e0: e.tensor_scalar(e0[:, :cw], banks[bg][:, :cw], bg_sb[:, ffc:ffc + 1], 7.0, ALU.add, ALU.min),
                         reads=[t_bank[bg], t_bg], writes=[t_ep[pp]])
                    P.op("dve", lambda e, ffc=ffc, cw=cw, bu=bu, e1=e1: e.tensor_scalar(e1[:, :cw], banks[bu][:, :cw], bg_sb[:, 16 + ffc:17 + ffc], 7.0, ALU.add, ALU.min),
                         reads=[t_bank[bu], t_bg], writes=[t_ep[pp]])
                    P.op("dve", lambda e, cw=cw, e1=e1: e.tensor_scalar(e1[:, :cw], e1[:, :cw], -7.0, 1.0, ALU.max, ALU.add),
                         reads=[t_ep[pp]], writes=[t_ep[pp]])
                    P.op("act", lambda e, cw=cw, e0=e0, e2=e2: e.activation(out=e2[:, :cw], in_=e0[:, :cw], func=AF.Sigmoid, scale=1.702),
                         reads=[t_ep[pp]], writes=[t_ep[pp]])
                    if pend[0] is not None:
                        pend[0]()

                    def part2(pp=pp, cw=cw, ffc=ffc, c0=c0, e0=e0, e1=e1, e2=e2, e3=e3):
                        P.op("dve", lambda e: e.tensor_tensor(e3[:, :cw], e0[:, :cw], e2[:, :cw], ALU.mult),
                             reads=[t_ep[pp]], writes=[t_ep[pp]])
                        P.op("dve", lambda e: e.tensor_tensor(actT[:, ffc, c0:c0 + cw], e3[:, :cw], e1[:, :cw], ALU.mult),
                             reads=[t_ep[pp]], writes=[t_act])
                    pend[0] = part2
        if pend[0] is not None:
            pend[0]()
            pend[0] = None
        wdv = wd[ex].rearrange("(kt p) c -> p kt c", p=128)
        for n in range(4):
            w_i = iw_ % NW
            iw_ += 1
            for hh in range(2):
                P.dma("pool", wt[w_i][:, hh * 8:(hh + 1) * 8, :], wdv[:, hh * 8:(hh + 1) * 8, n * 512:(n + 1) * 512],
                      writes=[t_wt[w_i]], chan=t_wt[w_i])
            for sb_ in range(cap // 128):
                bk = 2 + iy % 2
                yi = iy % 2
                iy += 1
                for kt in range(16):
                    P.op("pe", lambda e, bk=bk, kt=kt, sb_=sb_, w_i=w_i: e.matmul(
                        banks[bk][:, :512], actT[:, kt, sb_ * 128:(sb_ + 1) * 128], wt[w_i][:, kt, :],
                        start=(kt == 0), stop=(kt == 15)), reads=[t_act, t_wt[w_i]], writes=[t_bank[bk]])
                P.op("dve", lambda e, bk=bk, yi=yi, n=n: e.tensor_tensor(yo[yi][:], banks[bk][:, :512], bd_sb[:, n * 512:(n + 1) * 512], ALU.add),
                     reads=[t_bank[bk], t_bd], writes=[t_yo[yi]])
                P.dma("sp", y[ex, sb_ * 128:(sb_ + 1) * 128, n * 512:(n + 1) * 512], yo[yi][:], reads=[t_yo[yi]], writes=[Tk()], chan=t_yo[yi])
    P.emit()
    return nc


def build_E(nq=NQ, alpha=2.0 ** 0.25):
    nblk = nq // 128
    nc = bass.Bass("TRN2", target_bir_lowering=False)

    def din(name, shape, dt):
        return nc.dram_tensor(name, list(shape), dt, kind="ExternalInput").ap()
    y4 = din("y4", [nq, 4, 2048], F32)
    g4 = din("g4", [nq, 4], F32)
    h = din("h", [nq, 2048], F32)
    lng = din("lng", [128, 2048], F32)
    lnb = din("lnb", [128, 2048], F32)
    out = nc.dram_tensor("out", [nq, 2048], F32, kind="ExternalOutput").ap()
    P = Prog(nc)
    lng_sb = P.sb([128, 2048], F32, "lng_sb")
    lnb_sb = P.sb([128, 2048], F32, "lnb_sb")
    t_gb = Tk("gb")
    P.dma("sp", lng_sb[:], lng, writes=[t_gb], chan=t_gb)
    P.dma("sp", lnb_sb[:], lnb, writes=[t_gb], chan=t_gb)
    ys = [P.sb([128, 4, 2048], F32, f"ys{i}") for i in range(2)]
    t_ys = [Tk(f"ys{i}") for i in range(2)]
    hs = [P.sb([128, 2048], F32, f"hs{i}") for i in range(2)]
    t_hs = [Tk(f"hs{i}") for i in range(2)]
    gs = [P.sb([128, 4], F32, f"gs{i}") for i in range(2)]
    t_gs = [Tk(f"gs{i}") for i in range(2)]
    st = P.sb([128, 4, 6], F32, "st")
    mv = P.sb([128, 8], F32, "mv")
    t_st = Tk("st")
    P.op("dve", lambda e: e.memset(mv[:, 4:5], 1e-5), writes=[t_st])
    for b in range(nblk):
        i = b % 2
        P.dma("sp", ys[i][:], y4[b * 128:(b + 1) * 128], writes=[t_ys[i]])
        P.dma("sp", hs[i][:], h[b * 128:(b + 1) * 128, :], writes=[t_hs[i]])
        P.dma("sp", gs[i][:], g4[b * 128:(b + 1) * 128, :], writes=[t_gs[i]])
        P.op("act", lambda e, i=i: e.mul(hs[i][:], hs[i][:], float(alpha)), reads=[t_hs[i]], writes=[t_hs[i]])
        for k in range(4):
            P.op("dve", lambda e, i=i, k=k: e.scalar_tensor_tensor(
                out=hs[i][:], in0=ys[i][:, k, :], scalar=gs[i][:, k:k + 1], in1=hs[i][:], op0=ALU.mult, op1=ALU.add),
                reads=[t_ys[i], t_gs[i], t_hs[i]], writes=[t_hs[i]])
        emit_ln(P, hs[i], t_hs[i], lng_sb, lnb_sb, t_gb, st, mv, t_st)
        P.dma("sp", out[b * 128:(b + 1) * 128, :], hs[i][:], reads=[t_hs[i]], writes=[Tk()], chan=t_hs[i])
    P.emit()
    return nc


FOX_KEYS = ("aqT", "akT", "av", "af", "bfg", "dfox", "ind", "tri", "ident4")
DSA_KEYS = ("iqT", "ikT2", "iw", "bqT", "bkT", "bv", "dadm", "ident4")


def scatter_rows(parts, width, dtype):
    full = np.empty((S, width), dtype)
    for c in range(NCORES):
        full[own_tokens(c)] = parts[c]
    return full


def kernel(x, w_in, b_forget, w_branch_a, w_branch_b, w_out, ln1_g, ln1_b, w_router, b_router,
           w_gate_up, b_gate_up, w_down, b_down, ln2_g, ln2_b):
    x2 = np.asarray(x, np.float32)[0]
    resA = launch_A(x2, np.asarray(w_in, np.float32)[0])
    pbf = scatter_rows([r[0] for r in resA], NBF, NPBF)
    pf = scatter_rows([r[1] for r in resA], NF, np.float32)
    del resA
    no = _new_offsets()
    t = {k: pbf[:, no[k]:no[k] + SZ[k]] for k in ORD_BF}
    t["af"] = pf[:, no["af"]:no["af"] + 8]
    t["iw"] = pf[:, no["iw"]:no["iw"] + 16]
    t["b_forget"] = np.asarray(b_forget, np.float32)[0]
    ga = pf[:, no["ga"]:no["ga"] + 2048]
    gb = pf[:, no["gb"]:no["gb"] + 2048]
    maps = [prep_B(c, t) for c in range(NCORES)]
    resF = run_spmd(build_B(part="fox"), [{k: m[k] for k in FOX_KEYS} for m in maps])
    a_parts = [r["ab"] for r in resF]
    resD = run_spmd(build_B(part="dsa"), [{k: m[k] for k in DSA_KEYS} for m in maps])
    b_parts = [r["ab"] for r in resD]
    del maps, resF, resD
    eye = np.eye(128, dtype=np.float32)
    lng1 = np.ascontiguousarray(np.broadcast_to(np.asarray(ln1_g, np.float32)[0], (128, 2048)))
    lnb1 = np.ascontiguousarray(np.broadcast_to(np.asarray(ln1_b, np.float32)[0], (128, 2048)))
    mapsC = []
    for c in range(NCORES):
        tok = own_tokens(c)
        abc = np.concatenate([a_parts[c], b_parts[c]], 1)
        mapsC.append(dict(
            abT=np.ascontiguousarray(abc.reshape(NQ, 16, 128).transpose(2, 1, 0)),
            gaT=np.ascontiguousarray(ga[tok].T.reshape(16, 128, NQ)),
            gbT=np.ascontiguousarray(gb[tok].T.reshape(16, 128, NQ)),
            wa=np.asarray(w_branch_a, np.float32)[0], wb=np.asarray(w_branch_b, np.float32)[0],
            wo=np.asarray(w_out, np.float32)[0], xq=np.ascontiguousarray(x2[tok]), lng=lng1, lnb=lnb1,
            wr=np.asarray(w_router, np.float32)[0],
            br=np.ascontiguousarray(np.broadcast_to(np.asarray(b_router, np.float32)[0], (128, 32))), ident=eye))
    resC = run_spmd(build_C(), mapsC)
    h_full = scatter_rows([r["h"] for r in resC], 2048, np.float32)
    G_full = scatter_rows([r["G"] for r in resC], 32, np.float32)
    del mapsC, resC
    sel = G_full > 0
    pos = np.cumsum(sel, 0) - 1
    counts = sel.sum(0)
    cap = int(max(128, -(-int(counts.max()) // 128) * 128))
    wgu = np.asarray(w_gate_up)[0]
    wdn = np.asarray(w_down)[0]
    bgu = np.asarray(b_gate_up, np.float32)[0]
    bdn = np.asarray(b_down, np.float32)[0]
    mapsD = []
    for c in range(NCORES):
        xeT = np.zeros((4, 2048, cap), np.float32)
        for i in range(4):
            e = 4 * c + i
            idx = np.nonzero(sel[:, e])[0]
            xeT[i, :, :len(idx)] = h_full[idx].T
        mapsD.append(dict(
            xeT=xeT, wgu=np.ascontiguousarray(wgu[4 * c:4 * c + 4]),
            bgu=np.ascontiguousarray(bgu[4 * c:4 * c + 4].reshape(4, 32, 128).transpose(0, 2, 1)),
            wd=np.ascontiguousarray(wdn[4 * c:4 * c + 4]),
            bd=np.ascontiguousarray(np.broadcast_to(bdn[4 * c:4 * c + 4][:, None, :], (4, 128, 2048)))))
    resD2 = run_spmd(build_D(cap), mapsD)
    Y = np.concatenate([r["y"] for r in resD2], 0)
    del mapsD, resD2
    ek = np.argsort(~sel, axis=1, kind="stable")[:, :4]
    ar = np.arange(S)
    lng2 = np.ascontiguousarray(np.broadcast_to(np.asarray(ln2_g, np.float32)[0], (128, 2048)))
    lnb2 = np.ascontiguousarray(np.broadcast_to(np.asarray(ln2_b, np.float32)[0], (128, 2048)))
    mapsE = []
    for c in range(NCORES):
        tok = own_tokens(c)
        y4 = np.empty((NQ, 4, 2048), np.float32)
        g4 = np.empty((NQ, 4), np.float32)
        for k in range(4):
            e_k = ek[tok, k]
            valid = sel[tok, e_k]
            p_k = np.where(valid, pos[tok, e_k], 0)
            y4[:, k] = Y[e_k, p_k]
            g4[:, k] = G_full[tok, e_k]
        mapsE.append(dict(y4=y4, g4=g4, h=np.ascontiguousarray(h_full[tok]), lng=lng2, lnb=lnb2))
    resE = run_spmd(build_E(), mapsE)
    out = scatter_rows([r["out"] for r in resE], 2048, np.float32)
    return out[None]
```

```python
import contextlib
import numpy as np
import ml_dtypes
import concourse.bass as bass
import concourse.mybir as mybir
from concourse.bass_utils import run_bass_kernel_spmd

F32 = mybir.dt.float32
BF16 = mybir.dt.bfloat16
AF = mybir.ActivationFunctionType
ALU = mybir.AluOpType
AX = mybir.AxisListType
NPBF = ml_dtypes.bfloat16

NCORES = 8
S = 8192
D = 2048
NQ = S // NCORES
NB = S // 128
NJ = NQ // 128
KT = D // 128
NEG = -60000.0
EPOCH = 30000


class Tk:
    __slots__ = ("name", "w", "rs", "excl")

    def __init__(self, name="", excl=False):
        self.name = name
        self.w = None
        self.rs = []
        self.excl = excl


class Ctx:
    def __init__(self, nc):
        self.nc = nc
        self.esems = {e: [] for e in Prog.ENG}
        self.ecount = {e: 0 for e in Prog.ENG}
        self.nsem = 0

    def esem(self, e, idx):
        k = idx // EPOCH
        while len(self.esems[e]) <= k:
            self.esems[e].append(self.nc.alloc_semaphore(name=f"s_{e}{len(self.esems[e])}"))
            self.nsem += 1
        return self.esems[e][k], idx % EPOCH + 1

    def csem(self):
        self.nsem += 1
        return self.nc.alloc_semaphore(name=f"c{self.nsem}")


class Prog:
    ENG = ("pe", "act", "dve", "pool", "sp")

    def __init__(self, nc, ctx=None):
        self.nc = nc
        self.ctx = ctx or Ctx(nc)
        self.ops = []
        self.stack = contextlib.ExitStack()
        self.chan_count = {}
        self.nt = 0

    def sb(self, shape, dt, name=None):
        self.nt += 1
        return self.stack.enter_context(self.nc.sbuf_tensor(name or f"t{self.nt}", list(shape), dt))

    def ps(self, shape, dt, name=None):
        self.nt += 1
        return self.stack.enter_context(self.nc.psum_tensor(name or f"p{self.nt}", list(shape), dt))

    def _rec(self, eng, fn, reads, writes, dma=False, chan=None, inc=16):
        writes = writes + [r for r in reads if r.excl and r not in writes]
        reads = [r for r in reads if not r.excl]
        deps = set()
        for r in reads:
            if r.w is not None:
                deps.add(r.w)
        for w in writes:
            if w.w is not None:
                deps.add(w.w)
            deps.update(w.rs)
        i = len(self.ops)
        deps.discard(i)
        if dma:
            if chan is None:
                chan = writes[0]
            ckey = (id(chan), eng, inc)
            n = self.chan_count.get(ckey, 0) + inc
            self.chan_count[ckey] = n
            tokv = n
        else:
            tokv = None
        self.ops.append(dict(eng=eng, fn=fn, deps=deps, dma=dma, chan=ckey if dma else None, tokv=tokv, inc=inc))
        for r in reads:
            r.rs.append(i)
        for w in writes:
            w.w = i
            w.rs = []
        return i

    def op(self, eng, fn, reads=(), writes=()):
        return self._rec(eng, fn, list(reads), list(writes))

    def dma(self, eng, out, in_, reads=(), writes=(), chan=None):
        return self._rec(eng, lambda e: e.dma_start(out=out, in_=in_), list(reads), list(writes), dma=True, chan=chan)

    def dma_fn(self, eng, fn, reads=(), writes=(), chan=None, inc=16):
        return self._rec(eng, fn, list(reads), list(writes), dma=True, chan=chan, inc=inc)

    def emit(self):
        nc = self.nc
        ctx = self.ctx
        ops = self.ops
        n = len(ops)
        per_eng = {e: [] for e in self.ENG}
        for i, o in enumerate(ops):
            per_eng[o["eng"]].append(i)
        need = [False] * n
        for i, o in enumerate(ops):
            for d in o["deps"]:
                od = ops[d]
                if od["dma"]:
                    continue
                if od["eng"] == o["eng"] and o["eng"] == "pe" and not o["dma"]:
                    continue
                need[d] = True
        for e in self.ENG:
            comp = [i for i in per_eng[e] if not ops[i]["dma"]]
            if comp:
                need[comp[-1]] = True
        last_chan = {}
        for i, o in enumerate(ops):
            if o["dma"]:
                last_chan[o["chan"]] = i
        sig = [None] * n
        base = dict(ctx.ecount)
        cnt = dict(ctx.ecount)
        for i, o in enumerate(ops):
            if not o["dma"] and need[i]:
                sig[i] = cnt[o["eng"]]
                cnt[o["eng"]] += 1
        csems = {ck: ctx.csem() for ck in self.chan_count}

        def run_engine(ename, e):
            known_e = {x: base[x] - 1 for x in self.ENG}
            known_c = {}
            for i in per_eng[ename]:
                o = ops[i]
                waits_e = {}
                waits_c = {}
                for d in o["deps"]:
                    od = ops[d]
                    if od["dma"]:
                        if od["tokv"] > known_c.get(od["chan"], 0):
                            waits_c[od["chan"]] = max(waits_c.get(od["chan"], 0), od["tokv"])
                    else:
                        if od["eng"] == ename and ename == "pe" and not o["dma"]:
                            continue
                        if sig[d] is not None and sig[d] > known_e[od["eng"]]:
                            waits_e[od["eng"]] = max(waits_e.get(od["eng"], -1), sig[d])
                for en, idx in waits_e.items():
                    sm, v = ctx.esem(en, idx)
                    e.wait_ge(sm, v)
                    known_e[en] = idx
                for ck, v in waits_c.items():
                    e.wait_ge(csems[ck], v)
                    known_c[ck] = v
                ins = o["fn"](e)
                if o["dma"]:
                    ins.then_inc(csems[o["chan"]], o["inc"])
                elif sig[i] is not None:
                    sm, v = ctx.esem(ename, sig[i])
                    ins.then_inc(sm, 1)
            for en in self.ENG:
                if cnt[en] > base[en] and cnt[en] - 1 > known_e[en]:
                    sm, v = ctx.esem(en, cnt[en] - 1)
                    e.wait_ge(sm, v)
            for ck, i in last_chan.items():
                v = ops[i]["tokv"]
                if v > known_c.get(ck, 0):
                    e.wait_ge(csems[ck], v)

        with nc.Block() as block:
            @block.tensor
            def _(e):
                run_engine("pe", e)

            @block.scalar
            def _(e):
                run_engine("act", e)

            @block.vector
            def _(e):
                run_engine("dve", e)

            @block.gpsimd
            def _(e):
                run_engine("pool", e)

            @block.sync
            def _(e):
                run_engine("sp", e)
        ctx.ecount = cnt
        self.stack.close()


def run_spmd(nc, in_maps):
    res = run_bass_kernel_spmd(nc, in_maps, core_ids=list(range(NCORES)))
    if getattr(res, "exec_time_ns", None):
        print(f"[launch] exec_time_ns={res.exec_time_ns}", flush=True)
    return res.results


SZ = dict(aq=1024, ak=1024, av=1024, af=8, bq=1024, bk=256, bv=256, iq=1024, ik=64, iw=16, ga=2048, gb=2048)
ORIG = ["aq", "ak", "av", "af", "bq", "bk", "bv", "iq", "ik", "iw", "ga", "gb"]
ORD_BF = ["bq", "bk", "iq", "ik", "aq", "ak", "av", "bv"]
ORD_F = ["af", "iw", "ga", "gb"]
NBF = sum(SZ[k] for k in ORD_BF)
NF = sum(SZ[k] for k in ORD_F)


def _orig_offsets():
    o, off = {}, 0
    for k in ORIG:
        o[k] = off
        off += SZ[k]
    return o


def _new_offsets():
    o, off = {}, 0
    for k in ORD_BF:
        o[k] = off
        off += SZ[k]
    off = 0
    for k in ORD_F:
        o[k] = off
        off += SZ[k]
    return o


def a_chunks():
    ch = []
    col = 0
    outc = 0
    for k in ORD_BF:
        kind = "rope128" if k in ("bq", "bk") else ("rope64" if k in ("iq", "ik") else "plain")
        n = SZ[k]
        o = 0
        while o < n:
            w = min(512, n - o)
            ch.append((col + o, w, kind, "bf", outc + o))
            o += w
        col += n
        outc += n
    ch.append((col, 24, "plain", "f", 0))
    col += 24
    outc = 24
    for k in ("ga", "gb"):
        for o in range(0, SZ[k], 512):
            ch.append((col + o, 512, "plain", "f", outc + o))
        col += SZ[k]
        outc += SZ[k]
    return ch


def build_A(nq=NQ, chunks=None):
    chunks = chunks or a_chunks()
    ncol = max(c[0] + c[1] for c in chunks)
    nbf = max([c[4] + c[1] for c in chunks if c[3] == "bf"] + [2])
    nf = max([c[4] + c[1] for c in chunks if c[3] == "f"] + [2])
    nblk = nq // 128
    nc = bass.Bass("TRN2", target_bir_lowering=False)
    xT = nc.dram_tensor("xT", [D, nq], F32, kind="ExternalInput").ap()
    w = nc.dram_tensor("w", [D, ncol], F32, kind="ExternalInput").ap()
    cs128 = nc.dram_tensor("cs128", [nq, 2, 4, 16], F32, kind="ExternalInput").ap()
    cs64 = nc.dram_tensor("cs64", [nq, 2, 8, 8], F32, kind="ExternalInput").ap()
    pbf = nc.dram_tensor("pbf", [nq, nbf], BF16, kind="ExternalOutput").ap()
    pf = nc.dram_tensor("pf", [nq, nf], F32, kind="ExternalOutput").ap()
    P = Prog(nc)
    xb = P.sb([128, KT, nq], BF16, "xb")
    t_xb = Tk("xb")
    xTv = xT.rearrange("(kt p) s -> p kt s", p=128)
    for h in range(2):
        P.dma("pool", xb[:, h * 8:(h + 1) * 8, :], xTv[:, h * 8:(h + 1) * 8, :], writes=[t_xb], chan=t_xb)
    c128 = P.sb([128, nblk, 2, 4, 16], F32, "c128")
    c64 = P.sb([128, nblk, 2, 8, 8], F32, "c64")
    t_cs = Tk("cs")
    P.dma("sp", c128[:], cs128.rearrange("(b p) a h r -> p b a h r", p=128), writes=[t_cs], chan=t_cs)
    P.dma("sp", c64[:], cs64.rearrange("(b p) a h r -> p b a h r", p=128), writes=[t_cs], chan=t_cs)
    NW = 3
    wb = [P.sb([128, KT, 512], BF16, f"wb{i}") for i in range(NW)]
    t_wb = [Tk(f"wb{i}") for i in range(NW)]
    NPS = 4
    psm = [P.ps([128, 512], F32, f"psA{i}") for i in range(NPS)]
    t_ps = [Tk(f"ps{i}", excl=True) for i in range(NPS)]
    NO = 4
    obf = [P.sb([128, 512], BF16, f"obf{i}") for i in range(NO)]
    of = [P.sb([128, 512], F32, f"of{i}") for i in range(NO)]
    t_o = [Tk(f"o{i}") for i in range(NO)]
    tmp = [P.sb([128, 8, 16], F32, f"rtmp{i}") for i in range(4)]
    t_tmp = Tk("rtmp")
    wv = w.rearrange("(kt p) c -> p kt c", p=128)
    it = 0
    for ci, (c0, cw, kind, grp, oc) in enumerate(chunks):
        wi = ci % NW
        for h in range(2):
            P.dma("pool", wb[wi][:, h * 8:(h + 1) * 8, :cw], wv[:, h * 8:(h + 1) * 8, c0:c0 + cw],
                  writes=[t_wb[wi]], chan=t_wb[wi])
        for b in range(nblk):
            pi = it % NPS
            oi = it % NO
            it += 1
            ps = psm[pi]
            for kt in range(KT):
                P.op("pe", lambda e, ps=ps, kt=kt, b=b, wi=wi, cw=cw: e.matmul(
                    ps[:, :cw], xb[:, kt, b * 128:(b + 1) * 128], wb[wi][:, kt, :cw],
                    start=(kt == 0), stop=(kt == KT - 1)),
                    reads=[t_xb, t_wb[wi]], writes=[t_ps[pi]])
            ot = obf[oi] if grp == "bf" else of[oi]
            if kind == "plain":
                eng = "act" if (it % 2 == 0) else "dve"
                if eng == "act":
                    P.op("act", lambda e, ot=ot, ps=ps, cw=cw: e.copy(ot[:, :cw], ps[:, :cw]),
                         reads=[t_ps[pi]], writes=[t_o[oi]])
                else:
                    P.op("dve", lambda e, ot=ot, ps=ps, cw=cw: e.tensor_copy(ot[:, :cw], ps[:, :cw]),
                         reads=[t_ps[pi]], writes=[t_o[oi]])
            else:
                hd, r = (128, 16) if kind == "rope128" else (64, 8)
                nh = cw // hd
                cst = c128 if kind == "rope128" else c64
                pv = ps[:, :cw].rearrange("p (h d) -> p h d", d=hd)
                ov = ot[:, :cw].rearrange("p (h d) -> p h d", d=hd)
                x1, x2 = pv[:, :, 0:r], pv[:, :, r:2 * r]
                cc, ss = cst[:, b, 0, :nh, :], cst[:, b, 1, :nh, :]
                tv = [t[:, :nh, :r] for t in tmp]
                P.op("act", lambda e, ov=ov, pv=pv, r=r: e.copy(ov[:, :, 2 * r:], pv[:, :, 2 * r:]),
                     reads=[t_ps[pi]], writes=[t_o[oi]])
                P.op("dve", lambda e, a=tv[0], x=x1, c=cc: e.tensor_tensor(a, x, c, ALU.mult),
                     reads=[t_ps[pi], t_cs], writes=[t_tmp])
                P.op("dve", lambda e, a=tv[1], x=x2, c=ss: e.tensor_tensor(a, x, c, ALU.mult),
                     reads=[t_ps[pi], t_cs], writes=[t_tmp])
                P.op("dve", lambda e, a=tv[2], x=x2, c=cc: e.tensor_tensor(a, x, c, ALU.mult),
                     reads=[t_ps[pi], t_cs], writes=[t_tmp])
                P.op("dve", lambda e, a=tv[3], x=x1, c=ss: e.tensor_tensor(a, x, c, ALU.mult),
                     reads=[t_ps[pi], t_cs], writes=[t_tmp])
                P.op("dve", lambda e, o=ov[:, :, 0:r], a=tv[0], b_=tv[1]: e.tensor_tensor(o, a, b_, ALU.subtract),
                     reads=[t_tmp], writes=[t_o[oi]])
                P.op("dve", lambda e, o=ov[:, :, r:2 * r], a=tv[2], b_=tv[3]: e.tensor_tensor(o, a, b_, ALU.add),
                     reads=[t_tmp], writes=[t_o[oi]])
            dst = pbf if grp == "bf" else pf
            P.dma("sp", dst[b * 128:(b + 1) * 128, oc:oc + cw], ot[:, :cw], reads=[t_o[oi]], writes=[Tk()], chan=t_o[oi])
    P.emit()
    return nc


def rope_tables(pos, rot_dim, theta=500000.0):
    inv = np.power(np.float32(theta), -np.arange(0, rot_dim, 2, dtype=np.float32) / np.float32(rot_dim)).astype(np.float32)
    ang = pos.astype(np.float32)[:, None] * inv[None, :]
    return np.cos(ang).astype(np.float32), np.sin(ang).astype(np.float32)


def own_tokens(c):
    return np.concatenate([np.arange((8 * j + c) * 128, (8 * j + c + 1) * 128) for j in range(NJ)])


def perm_w_in(w_in):
    oo = _orig_offsets()
    cols = []
    for k in ORD_BF + ORD_F:
        cols.append(np.arange(oo[k], oo[k] + SZ[k]))
    return np.ascontiguousarray(w_in[:, np.concatenate(cols)])


def launch_A(x, w_in):
    wp = perm_w_in(w_in)
    nc = build_A()
    in_maps = []
    for c in range(NCORES):
        tok = own_tokens(c)
        cos, sin = rope_tables(tok, 32)
        cs128 = np.stack([np.repeat(cos[:, None, :], 4, 1), np.repeat(sin[:, None, :], 4, 1)], 1)
        cos, sin = rope_tables(tok, 16)
        cs64 = np.stack([np.repeat(cos[:, None, :], 8, 1), np.repeat(sin[:, None, :], 8, 1)], 1)
        in_maps.append(dict(xT=np.ascontiguousarray(x[tok].T), w=wp,
                            cs128=np.ascontiguousarray(cs128, dtype=np.float32),
                            cs64=np.ascontiguousarray(cs64, dtype=np.float32)))
    res = run_spmd(nc, in_maps)
    return [(r["pbf"], r["pf"]) for r in res]


BIS_R = 16.0


def build_B(s=S, nsel=256, nheads=8, nbis=26, part="fox"):
    fox = part == "fox"
    dsa = part == "dsa"
    nb = s // 128
    nq = s // NCORES
    nj = nq // 128
    scale = 128 ** -0.5
    idx_scale = (16 ** -0.5) * (64 ** -0.5)
    nc = bass.Bass("TRN2", target_bir_lowering=False)

    def din(name, shape, dt):
        return nc.dram_tensor(name, list(shape), dt, kind="ExternalInput").ap()
    if fox:
        aqT = din("aqT", [128, 8, nq], BF16)
        akT = din("akT", [8, 128, s], BF16)
        av = din("av", [8, 128, nb, 128], BF16)
        af = din("af", [128, nb, 8], F32)
        bfg = din("bfg", [128, 8], F32)
        dfox = din("dfox", [128, 8, 128], BF16)
        ind = din("ind", [128, nj, nb, 8], F32)
        tri = din("tri", [128, 128], F32)
    if dsa:
        iqT = din("iqT", [128, 8, nq], BF16)
        ikT2 = din("ikT2", [128, s], BF16)
        iw = din("iw", [128, nj, 16], F32)
        bqT = din("bqT", [128, nj, 8, 128], BF16)
        bkT = din("bkT", [128, 2, s], BF16)
        bv = din("bv", [128, nb, 2, 128], BF16)
        dadm = din("dadm", [128, 8, 128], F32)
    ident4 = din("ident4", [128, 512], BF16)
    ab = nc.dram_tensor("ab", [nq, 1024], BF16, kind="ExternalOutput").ap()

    P = Prog(nc)
    banks = [P.ps([128, 512], F32, f"bank{i}") for i in range(8)]
    t_bank = [Tk(f"bank{i}", excl=True) for i in range(8)]

    def load(name, src, shape, dt, eng="sp"):
        t = P.sb(shape, dt, name)
        tk = Tk(name)
        P.dma(eng, t[:], src, writes=[tk])
        return t, tk
    id_sb, t_id = load("id_sb", ident4, [128, 512], BF16)
    if fox:
        aq_sb, t_aq = load("aq_sb", aqT, [128, 8, nq], BF16)
        af_sb, t_af = load("af_sb", af, [128, nb, 8], F32)
        bfg_sb, t_bfg = load("bfg_sb", bfg, [128, 8], F32)
        dfox_sb, t_dfox = load("dfox_sb", dfox, [128, 8, 128], BF16)
        ind_sb, t_ind = load("ind_sb", ind, [128, nj, nb, 8], F32)
        tri_sb, t_tri = load("tri_sb", tri, [128, 128], F32)
    ones_sb = P.sb([128, 128], F32, "ones_sb")
    t_ones = Tk("ones")
    P.op("pool", lambda e: e.memset(ones_sb[:], 1.0), writes=[t_ones])
    if fox:
        ab_sb = P.sb([128, nj, 1024], BF16, "ab_sb")
    t_ab = Tk("ab")

    rec = P.sb([128, 8], F32, "rec")
    t_rec = Tk("rec")
    if fox:
      L = P.sb([128, nb, 8], F32, "L")
      t_L = Tk("L")
      for b in range(nb):
          P.op("dve", lambda e, b=b: e.tensor_tensor(L[:, b, :], af_sb[:, b, :], bfg_sb[:], ALU.add),
               reads=[t_af, t_bfg], writes=[t_L])
      Lf = L[:].rearrange("p b h -> p (b h)")
      P.op("act", lambda e: e.activation(out=Lf, in_=Lf, func=AF.Exp, scale=-1.0), reads=[t_L], writes=[t_L])
      P.op("act", lambda e: e.activation(out=Lf, in_=Lf, func=AF.Ln, bias=1.0), reads=[t_L], writes=[t_L])
      CP = P.sb([128, nb, 8], F32, "CP")
      TOT = P.sb([128, nb, 8], F32, "TOT")
      PRE = P.sb([128, nb, 8], F32, "PRE")
      t_CP, t_TOT, t_PRE = Tk("CP"), Tk("TOT"), Tk("PRE")
      ncol = nb * 8
      for c0 in range(0, ncol, 512):
          cw = min(512, ncol - c0)
          P.op("pe", lambda e, c0=c0, cw=cw: e.matmul(banks[0][:, :cw], tri_sb[:], Lf[:, c0:c0 + cw], start=True, stop=True),
               reads=[t_tri, t_L], writes=[t_bank[0]])
          P.op("dve", lambda e, c0=c0, cw=cw: e.tensor_copy(CP[:].rearrange("p b h -> p (b h)")[:, c0:c0 + cw], banks[0][:, :cw]),
               reads=[t_bank[0]], writes=[t_CP])
          P.op("pe", lambda e, c0=c0, cw=cw: e.matmul(banks[1][:, :cw], ones_sb[:], Lf[:, c0:c0 + cw], start=True, stop=True),
               reads=[t_ones, t_L], writes=[t_bank[1]])
          P.op("dve", lambda e, c0=c0, cw=cw: e.tensor_copy(TOT[:].rearrange("p b h -> p (b h)")[:, c0:c0 + cw], banks[1][:, :cw]),
               reads=[t_bank[1]], writes=[t_TOT])
      P.op("dve", lambda e: e.memset(PRE[:, 0, :], 0.0), writes=[t_PRE])
      for b in range(1, nb):
          P.op("dve", lambda e, b=b: e.tensor_tensor(PRE[:, b, :], PRE[:, b - 1, :], TOT[:, b - 1, :], ALU.add),
               reads=[t_TOT, t_PRE], writes=[t_PRE])
      P.op("dve", lambda e: e.tensor_tensor(CP[:], CP[:], PRE[:], ALU.add), reads=[t_CP, t_PRE], writes=[t_CP])
      cref = P.sb([128, nj, 8], F32, "cref")
      t_cref = Tk("cref")
      tmpi = P.sb([128, nb, 8], F32, "tmpi")
      t_tmpi = Tk("tmpi")
      for j in range(nj):
          P.op("dve", lambda e, j=j: e.tensor_tensor(tmpi[:], TOT[:], ind_sb[:, j, :, :], ALU.mult),
               reads=[t_TOT, t_ind], writes=[t_tmpi])
          P.op("dve", lambda e, j=j: e.tensor_reduce(cref[:, j, :], tmpi[:].rearrange("p b h -> p h b"), AX.X, ALU.add),
               reads=[t_tmpi], writes=[t_cref])
      biasJ = P.sb([128, nj, nb, 8], F32, "biasJ")
      t_bias = Tk("biasJ")
      for j in range(nj):
          nk = NCORES * (j + 1)
          for h in range(8):
              P.op("dve", lambda e, j=j, h=h, nk=nk: e.tensor_scalar(
                  biasJ[:, j, :nk, h], CP[:, :nk, h], cref[:, j, h:h + 1], None, ALU.subtract),
                  reads=[t_CP, t_cref], writes=[t_bias])

      kT = [P.sb([128, s], BF16, f"kT{i}") for i in range(2)]
      t_kT = [Tk(f"kT{i}") for i in range(2)]
      vt = [P.sb([128, nb, 129], BF16, f"vt{i}") for i in range(2)]
      t_vt = [Tk(f"vt{i}") for i in range(2)]
      for i in range(2):
          P.op("pool", lambda e, i=i: e.memset(vt[i][:, :, 128:129], 1.0), writes=[t_vt[i]])
      pT = [P.sb([128, 128], BF16, f"pT{i}") for i in range(4)]
      t_pT = [Tk(f"pT{i}") for i in range(4)]
      ipt = 0
      for h in range(nheads):
          hi = h % 2
          P.dma("sp", kT[hi][:], akT[h], writes=[t_kT[hi]])
          P.dma("sp", vt[hi][:, :, 0:128], av[h], writes=[t_vt[hi]])
          for j in range(nj):
              nk = NCORES * (j + 1)
              ob = 2 + (j % 2)
              SB = (0, 1, 4, 5)
              LA = 2

              def qk(kb, j=j, h=h, hi=hi, nk=nk):
                  sbk = SB[(tile0 + kb) % 4]
                  diag = kb >= nk - NCORES
                  P.op("pe", lambda e, sbk=sbk, diag=diag: e.matmul(
                      banks[sbk][:, :128], kT[hi][:, kb * 128:(kb + 1) * 128], aq_sb[:, h, j * 128:(j + 1) * 128],
                      start=True, stop=not diag), reads=[t_kT[hi], t_aq], writes=[t_bank[sbk]])
                  if diag:
                      P.op("pe", lambda e, sbk=sbk: e.matmul(
                          banks[sbk][:, :128], id_sb[:, :128], dfox_sb[:, kb - (nk - NCORES), :],
                          start=False, stop=True), reads=[t_id, t_dfox], writes=[t_bank[sbk]])
              tile0 = ipt
              for kb in range(min(LA, nk)):
                  qk(kb)
              for kb in range(nk):
                  if kb + LA < nk:
                      qk(kb + LA)
                  sbk = SB[(tile0 + kb) % 4]
                  pi = (tile0 + kb) % 4
                  P.op("act", lambda e, pi=pi, sbk=sbk, j=j, kb=kb, h=h: e.activation(
                      out=pT[pi][:, :128], in_=banks[sbk][:, :128], func=AF.Exp,
                      bias=biasJ[:, j, kb, h:h + 1], scale=scale),
                      reads=[t_bank[sbk], t_bias], writes=[t_pT[pi]])
                  P.op("pe", lambda e, ob=ob, pi=pi, hi=hi, kb=kb, nk=nk: e.matmul(
                      banks[ob][:, :129], pT[pi][:, :128], vt[hi][:, kb, :],
                      start=(kb == 0), stop=(kb == nk - 1)), reads=[t_pT[pi], t_vt[hi]], writes=[t_bank[ob]])
              ipt += nk
              P.op("dve", lambda e, ob=ob: e.reciprocal(rec[:, 0:1], banks[ob][:, 128:129]),
                   reads=[t_bank[ob]], writes=[t_rec])
              P.op("dve", lambda e, ob=ob, j=j, h=h: e.tensor_scalar(
                  ab_sb[:, j, h * 128:(h + 1) * 128], banks[ob][:, :128], rec[:, 0:1], None, ALU.mult),
                  reads=[t_bank[ob], t_rec], writes=[t_ab])

    if dsa:
      ik_sb, t_ik = load("ik_sb", ikT2, [128, s], BF16)
      iw_sb, t_iw = load("iw_sb", iw, [128, nj, 16], F32)
      bk_sb, t_bk = load("bk_sb", bkT, [128, 2, s], BF16)
      dadm_sb, t_dadm = load("dadm_sb", dadm, [128, 8, 128], F32)
      bv_sb = P.sb([128, nb, 2, 129], BF16, "bv_sb")
      t_bv = Tk("bv")
      P.op("pool", lambda e: e.memset(bv_sb[:, :, :, 128:129], 1.0), writes=[t_bv])
      P.dma("sp", bv_sb[:, :, :, 0:128], bv, writes=[t_bv])
      scl = P.sb([128, nj, 16], F32, "scl")
      sgn = P.sb([128, nj, 16], F32, "sgn")
      t_scl, t_sgn = Tk("scl"), Tk("sgn")
      P.op("dve", lambda e: e.tensor_scalar(sgn[:], iw_sb[:], 0.0, 2.0, ALU.is_ge, ALU.mult),
           reads=[t_iw], writes=[t_sgn])
      P.op("dve", lambda e: e.tensor_scalar(sgn[:], sgn[:], -1.0, None, ALU.add), reads=[t_sgn], writes=[t_sgn])
      P.op("dve", lambda e: e.tensor_tensor(scl[:], iw_sb[:], sgn[:], ALU.mult), reads=[t_iw, t_sgn], writes=[t_scl])
      P.op("dve", lambda e: e.tensor_scalar(scl[:], scl[:], idx_scale, None, ALU.mult), reads=[t_scl], writes=[t_scl])
      wsc = P.sb([128, nj, 16], F32, "wsc")
      P.op("dve", lambda e: e.tensor_scalar(wsc[:], iw_sb[:], idx_scale, None, ALU.mult), reads=[t_iw, t_scl], writes=[t_scl])
      score = [P.sb([128, s], F32, f"score{i}") for i in range(2)]
      t_score = [Tk(f"score{i}") for i in range(2)]
      mb = P.sb([128, s], BF16, "mb")
      t_mb = Tk("mb")
      junk = P.sb([128, s], BF16, "junk")
      t_junk = Tk("junk")
      iqs = [P.sb([128, 8, 128], BF16, f"iqs{i}") for i in range(2)]
      t_iqs = [Tk(f"iqs{i}") for i in range(2)]
      bqs = [P.sb([128, 8, 128], BF16, f"bqs{i}") for i in range(2)]
      t_bqs = [Tk(f"bqs{i}") for i in range(2)]
      dg0 = P.sb([128, 16, 128], BF16, "dg0")
      t_dg0 = Tk("dg0")
      dg = [dg0, dg0]
      t_dg = [t_dg0, t_dg0]
      rl = [P.sb([128, 512], BF16, f"rl{i}") for i in range(2)]
      t_rl = [Tk(f"rl{i}") for i in range(2)]
      pT4 = [P.sb([128, 512], BF16, f"pT4{i}") for i in range(3)]
      t_pT4 = [Tk(f"pT4{i}") for i in range(3)]
      outt = [P.sb([128, 1024], BF16, f"outt{i}") for i in range(2)]
      t_outt = [Tk(f"outt{i}") for i in range(2)]
      bisv = [P.sb([128, 8], F32, f"bis{i}") for i in range(2)]
      t_bisv = [Tk(f"bis{i}") for i in range(2)]
      rc = P.sb([128, 4], F32, "rc")
      t_rc = Tk("rc")
      st8 = dict(iz=0, iatt=0)

      def indexer(j):
          ji = j % 2
          sc, tsc = score[ji], t_score[ji]
          nk = NCORES * (j + 1)
          ngrp = nk * 128 // 512
          P.dma("sp", iqs[ji][:], iqT[:, :, j * 128:(j + 1) * 128], writes=[t_iqs[ji]])
          P.dma("sp", bqs[ji][:], bqT[:, j, :, :], writes=[t_bqs[ji]])
          for h in range(16):
              P.op("dve", lambda e, h=h: e.tensor_scalar(dg[ji][:, h, :], id_sb[:, :128], wsc[:, j, h:h + 1], None, ALU.mult),
                   reads=[t_id, t_scl], writes=[t_dg[ji]])
          for kg in range(ngrp):
              def zmm(h, kg=kg):
                  zb = (st8["iz"] + h) % 2
                  pr = (h % 2) * 64
                  P.op("pe", lambda e, zb=zb, pr=pr, h=h, kg=kg: e.matmul(
                      banks[zb][:, :512], iqs[ji][pr:pr + 64, h // 2, :],
                      ik_sb[pr:pr + 64, kg * 512:(kg + 1) * 512], start=True, stop=True),
                      reads=[t_iqs[ji], t_ik], writes=[t_bank[zb]])
              zmm(0)
              for h in range(16):
                  if h + 1 < 16:
                      zmm(h + 1)
                  zb = (st8["iz"] + h) % 2
                  P.op("act", lambda e, zb=zb, h=h: e.activation(
                      out=rl[zb][:], in_=banks[zb][:, :512], func=AF.Relu),
                      reads=[t_bank[zb]], writes=[t_rl[zb]])
                  P.op("pe", lambda e, zb=zb, h=h: e.matmul(
                      banks[6][:, :512], dg[ji][:, h, :], rl[zb][:], start=(h == 0), stop=(h == 15)),
                      reads=[t_dg[ji], t_rl[zb]], writes=[t_bank[6]])
              st8["iz"] += 16
              lastg = kg - (ngrp - 2)
              if lastg >= 0:
                  init = dadm_sb[:, lastg * 4:(lastg + 1) * 4, :].rearrange("p a k -> p (a k)")
                  P.op("dve", lambda e, kg=kg, init=init: e.tensor_tensor(
                      sc[:, kg * 512:(kg + 1) * 512], banks[6][:, :512], init, ALU.add),
                      reads=[t_bank[6], t_dadm], writes=[tsc])
              else:
                  P.op("dve", lambda e, kg=kg: e.tensor_copy(sc[:, kg * 512:(kg + 1) * 512], banks[6][:, :512]),
                       reads=[t_bank[6]], writes=[tsc])
              yield

      def bisection(j):
          ji = j % 2
          sc, tsc = score[ji], t_score[ji]
          bv_, tb = bisv[ji], t_bisv[ji]
          LO, MID, CNT, FL = [bv_[:, i:i + 1] for i in range(4)]
          nkeys = NCORES * (j + 1) * 128
          P.op("dve", lambda e: e.memset(LO, -BIS_R), writes=[tb])
          P.op("dve", lambda e: e.memset(MID, 0.0), writes=[tb])
          for it in range(nbis):
              hw_ = BIS_R / (2.0 ** it)
              P.op("dve", lambda e: e.tensor_scalar(
                  junk[:, :nkeys], sc[:, :nkeys], MID, None, ALU.is_ge, ALU.add, accum_out=CNT),
                  reads=[tsc, tb], writes=[tb, t_junk])
              P.op("dve", lambda e, hw_=hw_: e.tensor_scalar(FL, CNT, nsel - 0.5, hw_, ALU.is_ge, ALU.mult),
                   reads=[tb], writes=[tb])
              P.op("dve", lambda e: e.tensor_tensor(LO, LO, FL, ALU.add), reads=[tb], writes=[tb])
              P.op("dve", lambda e, hw_=hw_: e.tensor_scalar(MID, LO, hw_ * 0.5, None, ALU.add), reads=[tb], writes=[tb])
              yield
          P.op("dve", lambda e: e.tensor_scalar(mb[:, :nkeys], sc[:, :nkeys], LO, NEG, ALU.is_lt, ALU.mult),
               reads=[tsc, tb], writes=[t_mb])

      def attention(j):
          ji = j % 2
          nk = NCORES * (j + 1)
          oi = j % 2
          SB = (4, 5, 7)
          LA = 2
          for g in range(2):
              obs = (2, 3)

              def qk2(kb, g=g):
                  sbk = SB[(st8["iatt"] + kb) % 3]
                  P.op("pe", lambda e, sbk=sbk, g=g, kb=kb: e.matmul(
                      banks[sbk][:, :512], bk_sb[:, g, kb * 128:(kb + 1) * 128],
                      bqs[ji][:, g * 4:(g + 1) * 4, :].rearrange("p a q -> p (a q)"), start=True, stop=False),
                      reads=[t_bk, t_bqs[ji]], writes=[t_bank[sbk]])
                  P.op("pe", lambda e, sbk=sbk, kb=kb: e.matmul(
                      banks[sbk][:, :512], mb[:, kb * 128:(kb + 1) * 128], id_sb[:, :512], start=False, stop=True),
                      reads=[t_mb, t_id], writes=[t_bank[sbk]])
              for kb in range(min(LA, nk)):
                  qk2(kb)
              for kb in range(nk):
                  if kb + LA < nk:
                      qk2(kb + LA)
                  sbk = SB[(st8["iatt"] + kb) % 3]
                  pi = (st8["iatt"] + kb) % 3
                  P.op("act", lambda e, pi=pi, sbk=sbk: e.activation(
                      out=pT4[pi][:], in_=banks[sbk][:, :512], func=AF.Exp, scale=scale),
                      reads=[t_bank[sbk]], writes=[t_pT4[pi]])
                  for hh in range(4):
                      ob = obs[hh // 2]
                      c0 = (hh % 2) * 129
                      P.op("pe", lambda e, ob=ob, c0=c0, pi=pi, hh=hh, kb=kb, g=g: e.matmul(
                          banks[ob][:, c0:c0 + 129], pT4[pi][:, hh * 128:(hh + 1) * 128], bv_sb[:, kb, g, :],
                          start=(kb == 0 and hh % 2 == 0), stop=(kb == nk - 1), skip_group_check=True),
                          reads=[t_pT4[pi], t_bv], writes=[t_bank[ob]])
              st8["iatt"] += nk
              for hh in range(4):
                  ob = obs[hh // 2]
                  c0 = (hh % 2) * 129
                  head = g * 4 + hh
                  P.op("act", lambda e, ob=ob, c0=c0: e.activation(out=rc[:, 0:1], in_=banks[ob][:, c0 + 128:c0 + 129], func=AF.Ln),
                       reads=[t_bank[ob]], writes=[t_rc])
                  P.op("act", lambda e: e.activation(out=rc[:, 1:2], in_=rc[:, 0:1], func=AF.Exp, scale=-1.0),
                       reads=[t_rc], writes=[t_rc])
                  P.op("act", lambda e, ob=ob, c0=c0, head=head: e.activation(
                      out=outt[oi][:, head * 128:(head + 1) * 128], in_=banks[ob][:, c0:c0 + 128], func=AF.Copy, scale=rc[:, 1:2]),
                      reads=[t_bank[ob], t_rc], writes=[t_outt[oi]])
          P.dma("sp", ab[j * 128:(j + 1) * 128, :], outt[oi][:], reads=[t_outt[oi]], writes=[Tk()], chan=t_outt[oi])

      order = list(range(nj))
      for _ in indexer(order[0]):
          pass
      for oi_, j in enumerate(order):
          bs = bisection(j)
          if oi_ + 1 < nj:
              jn = order[oi_ + 1]
              ng = NCORES * (jn + 1) * 128 // 512
              per = -(-nbis // ng)
              for _ in indexer(jn):
                  for _i in range(per):
                      next(bs, None)
          for _ in bs:
              pass
          attention(j)
    if fox:
        for j in range(nj):
            P.dma("sp", ab[j * 128:(j + 1) * 128, :], ab_sb[:, j, :], reads=[t_ab], writes=[Tk()], chan=t_ab)
    P.emit()
    return nc


def bf(a):
    return np.ascontiguousarray(a).astype(NPBF)


def prep_B(c, t, s=S):
    nb = s // 128
    nq = s // NCORES
    nj = nq // 128
    tok = np.concatenate([np.arange((NCORES * j + c) * 128, (NCORES * j + c + 1) * 128) for j in range(nj)])
    m = {}
    m["aqT"] = np.ascontiguousarray(t["aq"][tok].reshape(nq, 8, 128).transpose(2, 1, 0))
    m["akT"] = np.ascontiguousarray(t["ak"].reshape(s, 8, 128).transpose(1, 2, 0))
    m["av"] = np.ascontiguousarray(t["av"].reshape(nb, 128, 8, 128).transpose(2, 1, 0, 3))
    m["af"] = np.ascontiguousarray(t["af"].reshape(nb, 128, 8).transpose(1, 0, 2))
    m["bfg"] = np.ascontiguousarray(np.broadcast_to(t["b_forget"].reshape(1, 8), (128, 8))).astype(np.float32)
    kk = np.arange(8)[None, :, None]
    k = np.arange(128)[:, None, None]
    q = np.arange(128)[None, None, :]
    dfox = np.where(kk < c, 0.0, np.where(kk > c, NEG, np.where(k <= q, 0.0, NEG)))
    m["dfox"] = bf(np.broadcast_to(dfox, (128, 8, 128)))
    qq = np.arange(128)[:, None, None]
    k2 = np.arange(128)[None, None, :]
    dadm = np.where(kk < c, 0.0, np.where(kk > c, -1e30, np.where(k2 // 64 <= qq // 64, 0.0, -1e30)))
    m["dadm"] = np.ascontiguousarray(np.broadcast_to(dadm, (128, 8, 128))).astype(np.float32)
    ind = (np.arange(nb)[None, :] < (NCORES * np.arange(nj)[:, None] + c)).astype(np.float32)
    m["ind"] = np.ascontiguousarray(np.broadcast_to(ind[None, :, :, None], (128, nj, nb, 8))).astype(np.float32)
    m["iqT"] = np.ascontiguousarray(t["iq"][tok].reshape(nq, 8, 128).transpose(2, 1, 0))
    ikT = t["ik"].T
    m["ikT2"] = np.ascontiguousarray(np.concatenate([ikT, ikT], 0))
    m["iw"] = np.ascontiguousarray(t["iw"][tok].reshape(nj, 128, 16).transpose(1, 0, 2))
    m["bqT"] = np.ascontiguousarray(t["bq"][tok].reshape(nj, 128, 8, 128).transpose(3, 0, 2, 1))
    m["bkT"] = np.ascontiguousarray(t["bk"].reshape(s, 2, 128).transpose(2, 1, 0))
    m["bv"] = np.ascontiguousarray(t["bv"].reshape(nb, 128, 2, 128).transpose(1, 0, 2, 3))
    m["ident4"] = bf(np.tile(np.eye(128, dtype=np.float32), (1, 4)))
    m["tri"] = np.triu(np.ones((128, 128), np.float32))
    return m


def emit_ln(P, x, t_x, g_sb, b_sb, t_gb, st, mv, t_st, eps=1e-5):
    for k in range(4):
        P.op("dve", lambda e, k=k: e.bn_stats(st[:, k, :], x[:, k * 512:(k + 1) * 512]), reads=[t_x], writes=[t_st])
    P.op("dve", lambda e: e.bn_aggr(mv[:, 0:2], st[:].rearrange("p a b -> p (a b)")), reads=[t_st], writes=[t_st])
    P.op("act", lambda e: e.activation(out=mv[:, 2:3], in_=mv[:, 1:2], func=AF.Sqrt, bias=mv[:, 4:5], scale=1.0),
         reads=[t_st], writes=[t_st])
    P.op("dve", lambda e: e.reciprocal(mv[:, 3:4], mv[:, 2:3]), reads=[t_st], writes=[t_st])
    P.op("dve", lambda e: e.tensor_scalar(x[:], x[:], mv[:, 0:1], mv[:, 3:4], ALU.subtract, ALU.mult),
         reads=[t_x, t_st], writes=[t_x])
    P.op("dve", lambda e: e.tensor_tensor(x[:], x[:], g_sb[:], ALU.mult), reads=[t_x, t_gb], writes=[t_x])
    P.op("dve", lambda e: e.tensor_tensor(x[:], x[:], b_sb[:], ALU.add), reads=[t_x, t_gb], writes=[t_x])


def build_C(nq=NQ, alpha=2.0 ** 0.25):
    nblk = nq // 128
    ntg = max(nq // 512, 1)
    tgw = min(512, nq)
    nc = bass.Bass("TRN2", target_bir_lowering=False)

    def din(name, shape, dt):
        return nc.dram_tensor(name, list(shape), dt, kind="ExternalInput").ap()
    abT = din("abT", [128, 16, nq], BF16)
    gaT = din("gaT", [16, 128, nq], F32)
    gbT = din("gbT", [16, 128, nq], F32)
    wa = din("wa", [1024, 2048], F32)
    wb = din("wb", [1024, 2048], F32)
    wo = din("wo", [2048, 2048], F32)
    xq = din("xq", [nq, 2048], F32)
    lng = din("lng", [128, 2048], F32)
    lnb = din("lnb", [128, 2048], F32)
    wr = din("wr", [2048, 32], F32)
    br = din("br", [128, 32], F32)
    ident = din("ident", [128, 128], F32)
    h_out = nc.dram_tensor("h", [nq, 2048], F32, kind="ExternalOutput").ap()
    g_out = nc.dram_tensor("G", [nq, 32], F32, kind="ExternalOutput").ap()
    P = Prog(nc)
    banks = [P.ps([128, 512], F32, f"bank{i}") for i in range(8)]
    t_bank = [Tk(f"bank{i}", excl=True) for i in range(8)]

    def load(name, src, shape, dt, eng="sp"):
        t = P.sb(shape, dt, name)
        tk = Tk(name)
        P.dma(eng, t[:], src, writes=[tk])
        return t, tk
    ab_sb, t_abT = load("abT_sb", abT, [128, 16, nq], BF16)
    W_sb = P.sb([128, 16, 2048], BF16, "W_sb")
    t_wa, t_wb = Tk("wa"), Tk("wb")
    P.dma("pool", W_sb[:, 0:8, :], wa.rearrange("(kt p) n -> p kt n", p=128), writes=[t_wa])
    P.dma("pool", W_sb[:, 8:16, :], wb.rearrange("(kt p) n -> p kt n", p=128), writes=[t_wb])
    wa_sb = W_sb[:, 0:8, :]
    wb_sb = W_sb[:, 8:16, :]
    mT = P.sb([128, 16, nq], BF16, "mT")
    t_mT = Tk("mT")
    gsb = [P.sb([128, 2, nq], F32, f"gsb{i}") for i in range(2)]
    t_g = [Tk(f"g{i}") for i in range(2)]
    m12 = [[P.sb([128, 512], F32, f"m12{p_}_{i}") for i in range(2)] for p_ in range(2)]
    t_m12 = [Tk("m12a"), Tk("m12b")]
    itc = [0]
    for c in range(16):
        gi = c % 2
        P.dma("sp", gsb[gi][:, 0, :], gaT[c], writes=[t_g[gi]])
        P.dma("sp", gsb[gi][:, 1, :], gbT[c], writes=[t_g[gi]])
        P.op("act", lambda e, gi=gi: e.activation(out=gsb[gi][:], in_=gsb[gi][:], func=AF.Sigmoid),
             reads=[t_g[gi]], writes=[t_g[gi]])
        for tg in range(ntg):
            ts_ = slice(tg * tgw, (tg + 1) * tgw)
            pp = itc[0] % 2
            itc[0] += 1
            ba, bb = (0, 1) if pp == 0 else (2, 3)
            ma, mb_ = m12[pp]
            for kt in range(8):
                P.op("pe", lambda e, kt=kt, c=c, ts_=ts_, ba=ba: e.matmul(
                    banks[ba][:, :tgw], wa_sb[:, kt, c * 128:(c + 1) * 128], ab_sb[:, kt, ts_],
                    start=(kt == 0), stop=(kt == 7)), reads=[t_wa, t_abT], writes=[t_bank[ba]])
            for kt in range(8):
                P.op("pe", lambda e, kt=kt, c=c, ts_=ts_, bb=bb: e.matmul(
                    banks[bb][:, :tgw], wb_sb[:, kt, c * 128:(c + 1) * 128], ab_sb[:, 8 + kt, ts_],
                    start=(kt == 0), stop=(kt == 7)), reads=[t_wb, t_abT], writes=[t_bank[bb]])
            P.op("dve", lambda e, gi=gi, ts_=ts_, ba=ba, ma=ma: e.tensor_tensor(ma[:, :tgw], banks[ba][:, :tgw], gsb[gi][:, 0, ts_], ALU.mult),
                 reads=[t_bank[ba], t_g[gi]], writes=[t_m12[pp]])
            P.op("dve", lambda e, gi=gi, ts_=ts_, bb=bb, mb_=mb_: e.tensor_tensor(mb_[:, :tgw], banks[bb][:, :tgw], gsb[gi][:, 1, ts_], ALU.mult),
                 reads=[t_bank[bb], t_g[gi]], writes=[t_m12[pp]])
            P.op("dve", lambda e, c=c, ts_=ts_, ma=ma, mb_=mb_: e.tensor_tensor(mT[:, c, ts_], ma[:, :tgw], mb_[:, :tgw], ALU.add),
                 reads=[t_m12[pp]], writes=[t_mT])
    wo_sb = W_sb
    wov = wo.rearrange("(kt p) n -> p kt n", p=128)
    P.dma("pool", W_sb[:, 0:8, :], wov[:, 0:8, :], writes=[t_wa])
    P.dma("pool", W_sb[:, 8:16, :], wov[:, 8:16, :], writes=[t_wb])
    lng_sb, t_lng = load("lng_sb", lng, [128, 2048], F32)
    lnb_sb, t_lnb = load("lnb_sb", lnb, [128, 2048], F32)
    t_gb = Tk("gb")
    P.op("pool", lambda e: e.engine_nop(), reads=[t_lng, t_lnb], writes=[t_gb])
    wr_sb, t_wr = load("wr_sb", wr.rearrange("(kt p) n -> p kt n", p=128), [128, 16, 32], F32)
    br_sb, t_br = load("br_sb", br, [128, 32], F32)
    id_sb, t_id = load("id_sb", ident, [128, 128], F32)
    xs0 = P.sb([128, 2048], F32, "xs0")
    t_xs0 = Tk("xs0")
    xs = [xs0, xs0]
    t_xs = [t_xs0, t_xs0]
    hs = [P.sb([128, 2048], F32, f"hs{i}") for i in range(2)]
    t_hs = [Tk(f"hs{i}") for i in range(2)]
    st = P.sb([128, 4, 6], F32, "st")
    mv = P.sb([128, 8], F32, "mv")
    t_st = Tk("st")
    P.op("dve", lambda e: e.memset(mv[:, 4:5], 1e-5), writes=[t_st])
    hT = P.sb([128, 16, 128], F32, "hT")
    t_hT = Tk("hT")
    rt = P.sb([128, 4, 32], F32, "rt")
    m8 = P.sb([128, 16], F32, "m8")
    t_rt = Tk("rt")
    for b in range(nblk):
        i2 = b % 2
        P.dma("sp", xs[i2][:], xq[b * 128:(b + 1) * 128, :], writes=[t_xs[i2]])
        for n in range(4):
            for kt in range(16):
                P.op("pe", lambda e, n=n, kt=kt, b=b: e.matmul(
                    banks[4 + n][:, :512], mT[:, kt, b * 128:(b + 1) * 128], wo_sb[:, kt, n * 512:(n + 1) * 512],
                    start=(kt == 0), stop=(kt == 15)), reads=[t_mT, t_wa, t_wb], writes=[t_bank[4 + n]])
            P.op("dve", lambda e, n=n, i2=i2: e.scalar_tensor_tensor(
                out=hs[i2][:, n * 512:(n + 1) * 512], in0=xs[i2][:, n * 512:(n + 1) * 512], scalar=float(alpha),
                in1=banks[4 + n][:, :512], op0=ALU.mult, op1=ALU.add),
                reads=[t_xs[i2], t_bank[4 + n]], writes=[t_hs[i2]])
        emit_ln(P, hs[i2], t_hs[i2], lng_sb, lnb_sb, t_gb, st, mv, t_st)
        P.dma("sp", h_out[b * 128:(b + 1) * 128, :], hs[i2][:], reads=[t_hs[i2]], writes=[Tk()], chan=t_hs[i2])
        for kt in range(16):
            bk = kt % 2
            P.op("pe", lambda e, kt=kt, bk=bk, i2=i2: e.transpose(banks[bk][:, :128], hs[i2][:, kt * 128:(kt + 1) * 128], id_sb[:]),
                 reads=[t_hs[i2], t_id], writes=[t_bank[bk]])
            P.op("act", lambda e, kt=kt, bk=bk: e.copy(hT[:, kt, :], banks[bk][:, :128]), reads=[t_bank[bk]], writes=[t_hT])
        for kt in range(16):
            P.op("pe", lambda e, kt=kt: e.matmul(banks[2][:, :32], hT[:, kt, :], wr_sb[:, kt, :], start=(kt == 0), stop=(kt == 15)),
                 reads=[t_hT, t_wr], writes=[t_bank[2]])
        LG, EX, SEL, GG = rt[:, 0, :], rt[:, 1, :], rt[:, 2, :], rt[:, 3, :]
        P.op("dve", lambda e: e.tensor_tensor(LG, banks[2][:, :32], br_sb[:], ALU.add), reads=[t_bank[2], t_br], writes=[t_rt])
        P.op("dve", lambda e: e.max(m8[:, 0:8], LG), reads=[t_rt], writes=[t_rt])
        P.op("dve", lambda e: e.tensor_scalar(m8[:, 8:9], m8[:, 0:1], -1.0, None, ALU.mult), reads=[t_rt], writes=[t_rt])
        P.op("act", lambda e: e.activation(out=EX, in_=LG, func=AF.Exp, bias=m8[:, 8:9], scale=1.0), reads=[t_rt], writes=[t_rt])
        P.op("dve", lambda e: e.tensor_scalar(SEL, LG, m8[:, 3:4], None, ALU.is_ge), reads=[t_rt], writes=[t_rt])
        P.op("dve", lambda e: e.tensor_tensor(EX, EX, SEL, ALU.mult), reads=[t_rt], writes=[t_rt])
        P.op("dve", lambda e: e.tensor_reduce(m8[:, 9:10], EX, AX.X, ALU.add), reads=[t_rt], writes=[t_rt])
        P.op("dve", lambda e: e.reciprocal(m8[:, 10:11], m8[:, 9:10]), reads=[t_rt], writes=[t_rt])
        P.op("dve", lambda e: e.tensor_scalar(GG, EX, m8[:, 10:11], None, ALU.mult), reads=[t_rt], writes=[t_rt])
        P.dma("sp", g_out[b * 128:(b + 1) * 128, :], GG, reads=[t_rt], writes=[Tk()], chan=t_rt)
    P.emit()
    return nc


def build_D(cap, nexp=4):
    nc = bass.Bass("TRN2", target_bir_lowering=False)

    def din(name, shape, dt):
        return nc.dram_tensor(name, list(shape), dt, kind="ExternalInput").ap()
    xeT = din("xeT", [nexp, 2048, cap], F32)
    wgu = din("wgu", [nexp, 2048, 4096], F32)
    bgu = din("bgu", [nexp, 128, 32], F32)
    wd = din("wd", [nexp, 2048, 2048], F32)
    bd = din("bd", [nexp, 128, 2048], F32)
    y = nc.dram_tensor("y", [nexp, cap, 2048], F32, kind="ExternalOutput").ap()
    P = Prog(nc)
    banks = [P.ps([128, 512], F32, f"bank{i}") for i in range(8)]
    t_bank = [Tk(f"bank{i}", excl=True) for i in range(8)]
    xe = P.sb([128, 16, cap], BF16, "xe")
    t_xe = Tk("xe")
    actT = P.sb([128, 16, cap], BF16, "actT")
    t_act = Tk("actT")
    NW = 3
    wt = [P.sb([128, 16, 512], BF16, f"wt{i}") for i in range(NW)]
    t_wt = [Tk(f"wt{i}") for i in range(NW)]
    bg_sb = P.sb([128, 32], F32, "bg_sb")
    t_bg = Tk("bg")
    bd_sb = P.sb([128, 2048], F32, "bd_sb")
    t_bd = Tk("bd")
    ep = [[P.sb([128, 512], F32, f"ep{p_}_{i}") for i in range(4)] for p_ in range(2)]
    t_ep = [Tk("ep0"), Tk("ep1")]
    itd = [0]
    pend = [None]
    yo = [P.sb([128, 512], F32, f"yo{i}") for i in range(2)]
    t_yo = [Tk(f"yo{i}") for i in range(2)]
    ncg = -(-cap // 512)
    cgw = -(-(cap // ncg) // 64) * 64
    cgs = [(c0, min(cgw, cap - c0)) for c0 in range(0, cap, cgw)]
    iw_ = 0
    iy = 0
    for ex in range(nexp):
        for h in range(2):
            P.dma("pool", xe[:, h * 8:(h + 1) * 8, :], xeT[ex].rearrange("(kt p) c -> p kt c", p=128)[:, h * 8:(h + 1) * 8, :],
                  writes=[t_xe], chan=t_xe)
        P.dma("sp", bg_sb[:], bgu[ex], writes=[t_bg])
        P.dma("sp", bd_sb[:], bd[ex], writes=[t_bd])
        wv = wgu[ex].rearrange("(kt p) c -> p kt c", p=128)
        for q in range(4):
            wi = []
            for half in range(2):
                w_i = iw_ % NW
                iw_ += 1
                c0 = half * 2048 + q * 512
                for hh in range(2):
                    P.dma("pool", wt[w_i][:, hh * 8:(hh + 1) * 8, :], wv[:, hh * 8:(hh + 1) * 8, c0:c0 + 512],
                          writes=[t_wt[w_i]], chan=t_wt[w_i])
                wi.append(w_i)
            for f in range(4):
                ffc = q * 4 + f
                for (c0, cw) in cgs:
                    pp = itd[0] % 2
                    itd[0] += 1
                    bg, bu = (0, 1) if pp == 0 else (4, 5)
                    e0, e1, e2, e3 = ep[pp]
                    for half in range(2):
                        bk = bg if half == 0 else bu
                        for kt in range(16):
                            P.op("pe", lambda e, bk=bk, wi_=wi[half], kt=kt, f=f, c0=c0, cw=cw: e.matmul(
                                banks[bk][:, :cw], wt[wi_][:, kt, f * 128:(f + 1) * 128], xe[:, kt, c0:c0 + cw],
                                start=(kt == 0), stop=(kt == 15)), reads=[t_wt[wi[half]], t_xe], writes=[t_bank[bk]])
                    P.op("dve", lambda e, ffc=ffc, cw=cw, bg=bg, e0=e0: e.tensor_scalar(e0[:, :cw], banks[bg][:, :cw], bg_sb[:, ffc:ffc + 1], 7.0, ALU.add, ALU.min),
                         reads=[t_bank[bg], t_bg], writes=[t_ep[pp]])
                    P.op("dve", lambda e, ffc=ffc, cw=cw, bu=bu, e1=e1: e.tensor_scalar(e1[:, :cw], banks[bu][:, :cw], bg_sb[:, 16 + ffc:17 + ffc], 7.0, ALU.add, ALU.min),
                         reads=[t_bank[bu], t_bg], writes=[t_ep[pp]])
                    P.op("dve", lambda e, cw=cw, e1=e1: e.tensor_scalar(e1[:, :cw], e1[:, :cw], -7.0, 1.0, ALU.max, ALU.add),
                         reads=[t_ep[pp]], writes=[t_ep[pp]])
                    P.op("act", lambda e, cw=cw, e0=e0, e2=e2: e.activation(out=e2[:, :cw], in_=e0[:, :cw], func=AF.Sigmoid, scale=1.702),
                         reads=[t_ep[pp]], writes=[t_ep[pp]])
                    if pend[0] is not None:
                        pend[0]()

                    def part2(pp=pp, cw=cw, ffc=ffc, c0=c0, e0=e0, e1=e1, e2=e2, e3=e3):
                        P.op("dve", lambda e: e.tensor_tensor(e3[:, :cw], e0[:, :cw], e2[:, :cw], ALU.mult),
                             reads=[t_ep[pp]], writes=[t_ep[pp]])
                        P.op("dve", lambda e: e.tensor_tensor(actT[:, ffc, c0:c0 + cw], e3[:, :cw], e1[:, :cw], ALU.mult),
                             reads=[t_ep[pp]], writes=[t_act])
                    pend[0] = part2
        if pend[0] is not None:
            pend[0]()
            pend[0] = None
        wdv = wd[ex].rearrange("(kt p) c -> p kt c", p=128)
        for n in range(4):
            w_i = iw_ % NW
            iw_ += 1
            for hh in range(2):
                P.dma("pool", wt[w_i][:, hh * 8:(hh + 1) * 8, :], wdv[:, hh * 8:(hh + 1) * 8, n * 512:(n + 1) * 512],
                      writes=[t_wt[w_i]], chan=t_wt[w_i])
            for sb_ in range(cap // 128):
                bk = 2 + iy % 2
                yi = iy % 2
                iy += 1
                for kt in range(16):
                    P.op("pe", lambda e, bk=bk, kt=kt, sb_=sb_, w_i=w_i: e.matmul(
                        banks[bk][:, :512], actT[:, kt, sb_ * 128:(sb_ + 1) * 128], wt[w_i][:, kt, :],
                        start=(kt == 0), stop=(kt == 15)), reads=[t_act, t_wt[w_i]], writes=[t_bank[bk]])
                P.op("dve", lambda e, bk=bk, yi=yi, n=n: e.tensor_tensor(yo[yi][:], banks[bk][:, :512], bd_sb[:, n * 512:(n + 1) * 512], ALU.add),
                     reads=[t_bank[bk], t_bd], writes=[t_yo[yi]])
                P.dma("sp", y[ex, sb_ * 128:(sb_ + 1) * 128, n * 512:(n + 1) * 512], yo[yi][:], reads=[t_yo[yi]], writes=[Tk()], chan=t_yo[yi])
    P.emit()
    return nc


def build_E(nq=NQ, alpha=2.0 ** 0.25):
    nblk = nq // 128
    nc = bass.Bass("TRN2", target_bir_lowering=False)

    def din(name, shape, dt):
        return nc.dram_tensor(name, list(shape), dt, kind="ExternalInput").ap()
    y4 = din("y4", [nq, 4, 2048], F32)
    g4 = din("g4", [nq, 4], F32)
    h = din("h", [nq, 2048], F32)
    lng = din("lng", [128, 2048], F32)
    lnb = din("lnb", [128, 2048], F32)
    out = nc.dram_tensor("out", [nq, 2048], F32, kind="ExternalOutput").ap()
    P = Prog(nc)
    lng_sb = P.sb([128, 2048], F32, "lng_sb")
    lnb_sb = P.sb([128, 2048], F32, "lnb_sb")
    t_gb = Tk("gb")
    P.dma("sp", lng_sb[:], lng, writes=[t_gb], chan=t_gb)
    P.dma("sp", lnb_sb[:], lnb, writes=[t_gb], chan=t_gb)
    ys = [P.sb([128, 4, 2048], F32, f"ys{i}") for i in range(2)]
    t_ys = [Tk(f"ys{i}") for i in range(2)]
    hs = [P.sb([128, 2048], F32, f"hs{i}") for i in range(2)]
    t_hs = [Tk(f"hs{i}") for i in range(2)]
    gs = [P.sb([128, 4], F32, f"gs{i}") for i in range(2)]
    t_gs = [Tk(f"gs{i}") for i in range(2)]
    st = P.sb([128, 4, 6], F32, "st")
    mv = P.sb([128, 8], F32, "mv")
    t_st = Tk("st")
    P.op("dve", lambda e: e.memset(mv[:, 4:5], 1e-5), writes=[t_st])
    for b in range(nblk):
        i = b % 2
        P.dma("sp", ys[i][:], y4[b * 128:(b + 1) * 128], writes=[t_ys[i]])
        P.dma("sp", hs[i][:], h[b * 128:(b + 1) * 128, :], writes=[t_hs[i]])
        P.dma("sp", gs[i][:], g4[b * 128:(b + 1) * 128, :], writes=[t_gs[i]])
        P.op("act", lambda e, i=i: e.mul(hs[i][:], hs[i][:], float(alpha)), reads=[t_hs[i]], writes=[t_hs[i]])
        for k in range(4):
            P.op("dve", lambda e, i=i, k=k: e.scalar_tensor_tensor(
                out=hs[i][:], in0=ys[i][:, k, :], scalar=gs[i][:, k:k + 1], in1=hs[i][:], op0=ALU.mult, op1=ALU.add),
                reads=[t_ys[i], t_gs[i], t_hs[i]], writes=[t_hs[i]])
        emit_ln(P, hs[i], t_hs[i], lng_sb, lnb_sb, t_gb, st, mv, t_st)
        P.dma("sp", out[b * 128:(b + 1) * 128, :], hs[i][:], reads=[t_hs[i]], writes=[Tk()], chan=t_hs[i])
    P.emit()
    return nc


FOX_KEYS = ("aqT", "akT", "av", "af", "bfg", "dfox", "ind", "tri", "ident4")
DSA_KEYS = ("iqT", "ikT2", "iw", "bqT", "bkT", "bv", "dadm", "ident4")


def scatter_rows(parts, width, dtype):
    full = np.empty((S, width), dtype)
    for c in range(NCORES):
        full[own_tokens(c)] = parts[c]
    return full


def kernel(x, w_in, b_forget, w_branch_a, w_branch_b, w_out, ln1_g, ln1_b, w_router, b_router,
           w_gate_up, b_gate_up, w_down, b_down, ln2_g, ln2_b):
    x2 = np.asarray(x, np.float32)[0]
    resA = launch_A(x2, np.asarray(w_in, np.float32)[0])
    pbf = scatter_rows([r[0] for r in resA], NBF, NPBF)
    pf = scatter_rows([r[1] for r in resA], NF, np.float32)
    del resA
    no = _new_offsets()
    t = {k: pbf[:, no[k]:no[k] + SZ[k]] for k in ORD_BF}
    t["af"] = pf[:, no["af"]:no["af"] + 8]
    t["iw"] = pf[:, no["iw"]:no["iw"] + 16]
    t["b_forget"] = np.asarray(b_forget, np.float32)[0]
    ga = pf[:, no["ga"]:no["ga"] + 2048]
    gb = pf[:, no["gb"]:no["gb"] + 2048]
    maps = [prep_B(c, t) for c in range(NCORES)]
    resF = run_spmd(build_B(part="fox"), [{k: m[k] for k in FOX_KEYS} for m in maps])
    a_parts = [r["ab"] for r in resF]
    resD = run_spmd(build_B(part="dsa"), [{k: m[k] for k in DSA_KEYS} for m in maps])
    b_parts = [r["ab"] for r in resD]
    del maps, resF, resD
    eye = np.eye(128, dtype=np.float32)
    lng1 = np.ascontiguousarray(np.broadcast_to(np.asarray(ln1_g, np.float32)[0], (128, 2048)))
    lnb1 = np.ascontiguousarray(np.broadcast_to(np.asarray(ln1_b, np.float32)[0], (128, 2048)))
    mapsC = []
    for c in range(NCORES):
        tok = own_tokens(c)
        abc = np.concatenate([a_parts[c], b_parts[c]], 1)
        mapsC.append(dict(
            abT=np.ascontiguousarray(abc.reshape(NQ, 16, 128).transpose(2, 1, 0)),
            gaT=np.ascontiguousarray(ga[tok].T.reshape(16, 128, NQ)),
            gbT=np.ascontiguousarray(gb[tok].T.reshape(16, 128, NQ)),
            wa=np.asarray(w_branch_a, np.float32)[0], wb=np.asarray(w_branch_b, np.float32)[0],
            wo=np.asarray(w_out, np.float32)[0], xq=np.ascontiguousarray(x2[tok]), lng=lng1, lnb=lnb1,
            wr=np.asarray(w_router, np.float32)[0],
            br=np.ascontiguousarray(np.broadcast_to(np.asarray(b_router, np.float32)[0], (128, 32))), ident=eye))
    resC = run_spmd(build_C(), mapsC)
    h_full = scatter_rows([r["h"] for r in resC], 2048, np.float32)
    G_full = scatter_rows([r["G"] for r in resC], 32, np.float32)
    del mapsC, resC
    sel = G_full > 0
    pos = np.cumsum(sel, 0) - 1
    counts = sel.sum(0)
    cap = int(max(128, -(-int(counts.max()) // 128) * 128))
    wgu = np.asarray(w_gate_up)[0]
    wdn = np.asarray(w_down)[0]
    bgu = np.asarray(b_gate_up, np.float32)[0]
    bdn = np.asarray(b_down, np.float32)[0]
    mapsD = []
    for c in range(NCORES):
        xeT = np.zeros((4, 2048, cap), np.float32)
        for i in range(4):
            e = 4 * c + i
            idx = np.nonzero(sel[:, e])[0]
            xeT[i, :, :len(idx)] = h_full[idx].T
        mapsD.append(dict(
            xeT=xeT, wgu=np.ascontiguousarray(wgu[4 * c:4 * c + 4]),
            bgu=np.ascontiguousarray(bgu[4 * c:4 * c + 4].reshape(4, 32, 128).transpose(0, 2, 1)),
            wd=np.ascontiguousarray(wdn[4 * c:4 * c + 4]),
            bd=np.ascontiguousarray(np.broadcast_to(bdn[4 * c:4 * c + 4][:, None, :], (4, 128, 2048)))))
    resD2 = run_spmd(build_D(cap), mapsD)
    Y = np.concatenate([r["y"] for r in resD2], 0)
    del mapsD, resD2
    ek = np.argsort(~sel, axis=1, kind="stable")[:, :4]
    ar = np.arange(S)
    lng2 = np.ascontiguousarray(np.broadcast_to(np.asarray(ln2_g, np.float32)[0], (128, 2048)))
    lnb2 = np.ascontiguousarray(np.broadcast_to(np.asarray(ln2_b, np.float32)[0], (128, 2048)))
    mapsE = []
    for c in range(NCORES):
        tok = own_tokens(c)
        y4 = np.empty((NQ, 4, 2048), np.float32)
        g4 = np.empty((NQ, 4), np.float32)
        for k in range(4):
            e_k = ek[tok, k]
            valid = sel[tok, e_k]
            p_k = np.where(valid, pos[tok, e_k], 0)
            y4[:, k] = Y[e_k, p_k]
            g4[:, k] = G_full[tok, e_k]
        mapsE.append(dict(y4=y4, g4=g4, h=np.ascontiguousarray(h_full[tok]), lng=lng2, lnb=lnb2))
    resE = run_spmd(build_E(), mapsE)
    out = scatter_rows([r["out"] for r in resE], 2048, np.float32)
    return out[None]
```

```python
import contextlib
import numpy as np
import ml_dtypes
import concourse.bass as bass
import concourse.mybir as mybir
from concourse.bass_utils import run_bass_kernel_spmd

F32 = mybir.dt.float32
BF16 = mybir.dt.bfloat16
AF = mybir.ActivationFunctionType
ALU = mybir.AluOpType
AX = mybir.AxisListType
NPBF = ml_dtypes.bfloat16

NCORES = 8
S = 8192
D = 2048
NQ = S // NCORES
NB = S // 128
NJ = NQ // 128
KT = D // 128
NEG = -60000.0
EPOCH = 30000


class Tk:
    __slots__ = ("name", "w", "rs", "excl")

    def __init__(self, name="", excl=False):
        self.name = name
        self.w = None
        self.rs = []
        self.excl = excl


class Ctx:
    def __init__(self, nc):
        self.nc = nc
        self.nphase = 0
        self.esems = {e: [] for e in Prog.ENG}
        self.ecount = {e: 0 for e in Prog.ENG}
        self.nsem = 0

    def esem(self, e, idx):
        k = idx // EPOCH
        while len(self.esems[e]) <= k:
            self.esems[e].append(self.nc.alloc_semaphore(name=f"s_{e}{len(self.esems[e])}"))
            self.nsem += 1
        return self.esems[e][k], idx % EPOCH + 1

    def csem(self):
        self.nsem += 1
        return self.nc.alloc_semaphore(name=f"c{self.nsem}")


class Prog:
    ENG = ("pe", "act", "dve", "pool", "sp")

    def __init__(self, nc, ctx=None):
        self.nc = nc
        self.ctx = ctx or Ctx(nc)
        self.tag = f"ph{self.ctx.nphase}_"
        self.ctx.nphase += 1
        self.ops = []
        self.stack = contextlib.ExitStack()
        self.chan_count = {}
        self.nt = 0

    def sb(self, shape, dt, name=None):
        self.nt += 1
        return self.stack.enter_context(self.nc.sbuf_tensor(self.tag + (name or f"t{self.nt}"), list(shape), dt))

    def ps(self, shape, dt, name=None):
        self.nt += 1
        return self.stack.enter_context(self.nc.psum_tensor(self.tag + (name or f"p{self.nt}"), list(shape), dt))

    def _rec(self, eng, fn, reads, writes, dma=False, chan=None, inc=16):
        writes = writes + [r for r in reads if r.excl and r not in writes]
        reads = [r for r in reads if not r.excl]
        deps = set()
        for r in reads:
            if r.w is not None:
                deps.add(r.w)
        for w in writes:
            if w.w is not None:
                deps.add(w.w)
            deps.update(w.rs)
        i = len(self.ops)
        deps.discard(i)
        if dma:
            if chan is None:
                chan = writes[0]
            ckey = (id(chan), eng, inc)
            n = self.chan_count.get(ckey, 0) + inc
            self.chan_count[ckey] = n
            tokv = n
        else:
            tokv = None
        self.ops.append(dict(eng=eng, fn=fn, deps=deps, dma=dma, chan=ckey if dma else None, tokv=tokv, inc=inc))
        for r in reads:
            r.rs.append(i)
        for w in writes:
            w.w = i
            w.rs = []
        return i

    def op(self, eng, fn, reads=(), writes=()):
        return self._rec(eng, fn, list(reads), list(writes))

    def dma(self, eng, out, in_, reads=(), writes=(), chan=None):
        return self._rec(eng, lambda e: e.dma_start(out=out, in_=in_), list(reads), list(writes), dma=True, chan=chan)

    def dma_fn(self, eng, fn, reads=(), writes=(), chan=None, inc=16):
        return self._rec(eng, fn, list(reads), list(writes), dma=True, chan=chan, inc=inc)

    def emit(self):
        nc = self.nc
        ctx = self.ctx
        ops = self.ops
        n = len(ops)
        per_eng = {e: [] for e in self.ENG}
        for i, o in enumerate(ops):
            per_eng[o["eng"]].append(i)
        need = [False] * n
        for i, o in enumerate(ops):
            for d in o["deps"]:
                od = ops[d]
                if od["dma"]:
                    continue
                if od["eng"] == o["eng"] and o["eng"] == "pe" and not o["dma"]:
                    continue
                need[d] = True
        for e in self.ENG:
            comp = [i for i in per_eng[e] if not ops[i]["dma"]]
            if comp:
                need[comp[-1]] = True
        last_chan = {}
        for i, o in enumerate(ops):
            if o["dma"]:
                last_chan[o["chan"]] = i
        sig = [None] * n
        base = dict(ctx.ecount)
        cnt = dict(ctx.ecount)
        for i, o in enumerate(ops):
            if not o["dma"] and need[i]:
                sig[i] = cnt[o["eng"]]
                cnt[o["eng"]] += 1
        csems = {ck: ctx.csem() for ck in self.chan_count}

        def run_engine(ename, e):
            known_e = {x: base[x] - 1 for x in self.ENG}
            known_c = {}
            for i in per_eng[ename]:
                o = ops[i]
                waits_e = {}
                waits_c = {}
                for d in o["deps"]:
                    od = ops[d]
                    if od["dma"]:
                        if od["tokv"] > known_c.get(od["chan"], 0):
                            waits_c[od["chan"]] = max(waits_c.get(od["chan"], 0), od["tokv"])
                    else:
                        if od["eng"] == ename and ename == "pe" and not o["dma"]:
                            continue
                        if sig[d] is not None and sig[d] > known_e[od["eng"]]:
                            waits_e[od["eng"]] = max(waits_e.get(od["eng"], -1), sig[d])
                for en, idx in waits_e.items():
                    sm, v = ctx.esem(en, idx)
                    e.wait_ge(sm, v)
                    known_e[en] = idx
                for ck, v in waits_c.items():
                    e.wait_ge(csems[ck], v)
                    known_c[ck] = v
                ins = o["fn"](e)
                if o["dma"]:
                    ins.then_inc(csems[o["chan"]], o["inc"])
                elif sig[i] is not None:
                    sm, v = ctx.esem(ename, sig[i])
                    ins.then_inc(sm, 1)
            for en in self.ENG:
                if cnt[en] > base[en] and cnt[en] - 1 > known_e[en]:
                    sm, v = ctx.esem(en, cnt[en] - 1)
                    e.wait_ge(sm, v)
            for ck, i in last_chan.items():
                v = ops[i]["tokv"]
                if v > known_c.get(ck, 0):
                    e.wait_ge(csems[ck], v)

        with nc.Block() as block:
            @block.tensor
            def _(e):
                run_engine("pe", e)

            @block.scalar
            def _(e):
                run_engine("act", e)

            @block.vector
            def _(e):
                run_engine("dve", e)

            @block.gpsimd
            def _(e):
                run_engine("pool", e)

            @block.sync
            def _(e):
                run_engine("sp", e)
        ctx.ecount = cnt
        self.stack.close()


def run_spmd(nc, in_maps):
    res = run_bass_kernel_spmd(nc, in_maps, core_ids=list(range(NCORES)))
    if getattr(res, "exec_time_ns", None):
        print(f"[launch] exec_time_ns={res.exec_time_ns}", flush=True)
    return res.results


SZ = dict(aq=1024, ak=1024, av=1024, af=8, bq=1024, bk=256, bv=256, iq=1024, ik=64, iw=16, ga=2048, gb=2048)
ORIG = ["aq", "ak", "av", "af", "bq", "bk", "bv", "iq", "ik", "iw", "ga", "gb"]
ORD_BF = ["bq", "bk", "iq", "ik", "aq", "ak", "av", "bv"]
ORD_F = ["af", "iw", "ga", "gb"]
NBF = sum(SZ[k] for k in ORD_BF)
NF = sum(SZ[k] for k in ORD_F)


def _orig_offsets():
    o, off = {}, 0
    for k in ORIG:
        o[k] = off
        off += SZ[k]
    return o


def _new_offsets():
    o, off = {}, 0
    for k in ORD_BF:
        o[k] = off
        off += SZ[k]
    off = 0
    for k in ORD_F:
        o[k] = off
        off += SZ[k]
    return o


def a_chunks():
    ch = []
    col = 0
    outc = 0
    for k in ORD_BF:
        kind = "rope128" if k in ("bq", "bk") else ("rope64" if k in ("iq", "ik") else "plain")
        n = SZ[k]
        o = 0
        while o < n:
            w = min(512, n - o)
            ch.append((col + o, w, kind, "bf", outc + o))
            o += w
        col += n
        outc += n
    ch.append((col, 24, "plain", "f", 0))
    col += 24
    outc = 24
    for k in ("ga", "gb"):
        for o in range(0, SZ[k], 512):
            ch.append((col + o, 512, "plain", "f", outc + o))
        col += SZ[k]
        outc += SZ[k]
    return ch


def build_A(nq=NQ, chunks=None):
    chunks = chunks or a_chunks()
    ncol = max(c[0] + c[1] for c in chunks)
    nbf = max([c[4] + c[1] for c in chunks if c[3] == "bf"] + [2])
    nf = max([c[4] + c[1] for c in chunks if c[3] == "f"] + [2])
    nblk = nq // 128
    nc = bass.Bass("TRN2", target_bir_lowering=False)
    xT = nc.dram_tensor("xT", [D, nq], F32, kind="ExternalInput").ap()
    w = nc.dram_tensor("w", [D, ncol], F32, kind="ExternalInput").ap()
    cs128 = nc.dram_tensor("cs128", [nq, 2, 4, 16], F32, kind="ExternalInput").ap()
    cs64 = nc.dram_tensor("cs64", [nq, 2, 8, 8], F32, kind="ExternalInput").ap()
    pbf = nc.dram_tensor("pbf", [nq, nbf], BF16, kind="ExternalOutput").ap()
    pf = nc.dram_tensor("pf", [nq, nf], F32, kind="ExternalOutput").ap()
    P = Prog(nc)
    xb = P.sb([128, KT, nq], BF16, "xb")
    t_xb = Tk("xb")
    xTv = xT.rearrange("(kt p) s -> p kt s", p=128)
    for h in range(2):
        P.dma("pool", xb[:, h * 8:(h + 1) * 8, :], xTv[:, h * 8:(h + 1) * 8, :], writes=[t_xb], chan=t_xb)
    c128 = P.sb([128, nblk, 2, 4, 16], F32, "c128")
    c64 = P.sb([128, nblk, 2, 8, 8], F32, "c64")
    t_cs = Tk("cs")
    P.dma("sp", c128[:], cs128.rearrange("(b p) a h r -> p b a h r", p=128), writes=[t_cs], chan=t_cs)
    P.dma("sp", c64[:], cs64.rearrange("(b p) a h r -> p b a h r", p=128), writes=[t_cs], chan=t_cs)
    NW = 3
    wb = [P.sb([128, KT, 512], BF16, f"wb{i}") for i in range(NW)]
    t_wb = [Tk(f"wb{i}") for i in range(NW)]
    NPS = 4
    psm = [P.ps([128, 512], F32, f"psA{i}") for i in range(NPS)]
    t_ps = [Tk(f"ps{i}", excl=True) for i in range(NPS)]
    NO = 4
    obf = [P.sb([128, 512], BF16, f"obf{i}") for i in range(NO)]
    of = [P.sb([128, 512], F32, f"of{i}") for i in range(NO)]
    t_o = [Tk(f"o{i}") for i in range(NO)]
    tmp = [P.sb([128, 8, 16], F32, f"rtmp{i}") for i in range(4)]
    t_tmp = Tk("rtmp")
    wv = w.rearrange("(kt p) c -> p kt c", p=128)
    it = 0
    for ci, (c0, cw, kind, grp, oc) in enumerate(chunks):
        wi = ci % NW
        for h in range(2):
            P.dma("pool", wb[wi][:, h * 8:(h + 1) * 8, :cw], wv[:, h * 8:(h + 1) * 8, c0:c0 + cw],
                  writes=[t_wb[wi]], chan=t_wb[wi])
        for b in range(nblk):
            pi = it % NPS
            oi = it % NO
            it += 1
            ps = psm[pi]
            for kt in range(KT):
                P.op("pe", lambda e, ps=ps, kt=kt, b=b, wi=wi, cw=cw: e.matmul(
                    ps[:, :cw], xb[:, kt, b * 128:(b + 1) * 128], wb[wi][:, kt, :cw],
                    start=(kt == 0), stop=(kt == KT - 1)),
                    reads=[t_xb, t_wb[wi]], writes=[t_ps[pi]])
            ot = obf[oi] if grp == "bf" else of[oi]
            if kind == "plain":
                eng = "act" if (it % 2 == 0) else "dve"
                if eng == "act":
                    P.op("act", lambda e, ot=ot, ps=ps, cw=cw: e.copy(ot[:, :cw], ps[:, :cw]),
                         reads=[t_ps[pi]], writes=[t_o[oi]])
                else:
                    P.op("dve", lambda e, ot=ot, ps=ps, cw=cw: e.tensor_copy(ot[:, :cw], ps[:, :cw]),
                         reads=[t_ps[pi]], writes=[t_o[oi]])
            else:
                hd, r = (128, 16) if kind == "rope128" else (64, 8)
                nh = cw // hd
                cst = c128 if kind == "rope128" else c64
                pv = ps[:, :cw].rearrange("p (h d) -> p h d", d=hd)
                ov = ot[:, :cw].rearrange("p (h d) -> p h d", d=hd)
                x1, x2 = pv[:, :, 0:r], pv[:, :, r:2 * r]
                cc, ss = cst[:, b, 0, :nh, :], cst[:, b, 1, :nh, :]
                tv = [t[:, :nh, :r] for t in tmp]
                P.op("act", lambda e, ov=ov, pv=pv, r=r: e.copy(ov[:, :, 2 * r:], pv[:, :, 2 * r:]),
                     reads=[t_ps[pi]], writes=[t_o[oi]])
                P.op("dve", lambda e, a=tv[0], x=x1, c=cc: e.tensor_tensor(a, x, c, ALU.mult),
                     reads=[t_ps[pi], t_cs], writes=[t_tmp])
                P.op("dve", lambda e, a=tv[1], x=x2, c=ss: e.tensor_tensor(a, x, c, ALU.mult),
                     reads=[t_ps[pi], t_cs], writes=[t_tmp])
                P.op("dve", lambda e, a=tv[2], x=x2, c=cc: e.tensor_tensor(a, x, c, ALU.mult),
                     reads=[t_ps[pi], t_cs], writes=[t_tmp])
                P.op("dve", lambda e, a=tv[3], x=x1, c=ss: e.tensor_tensor(a, x, c, ALU.mult),
                     reads=[t_ps[pi], t_cs], writes=[t_tmp])
                P.op("dve", lambda e, o=ov[:, :, 0:r], a=tv[0], b_=tv[1]: e.tensor_tensor(o, a, b_, ALU.subtract),
                     reads=[t_tmp], writes=[t_o[oi]])
                P.op("dve", lambda e, o=ov[:, :, r:2 * r], a=tv[2], b_=tv[3]: e.tensor_tensor(o, a, b_, ALU.add),
                     reads=[t_tmp], writes=[t_o[oi]])
            dst = pbf if grp == "bf" else pf
            P.dma("sp", dst[b * 128:(b + 1) * 128, oc:oc + cw], ot[:, :cw], reads=[t_o[oi]], writes=[Tk()], chan=t_o[oi])
    P.emit()
    return nc


def rope_tables(pos, rot_dim, theta=500000.0):
    inv = np.power(np.float32(theta), -np.arange(0, rot_dim, 2, dtype=np.float32) / np.float32(rot_dim)).astype(np.float32)
    ang = pos.astype(np.float32)[:, None] * inv[None, :]
    return np.cos(ang).astype(np.float32), np.sin(ang).astype(np.float32)


def own_tokens(c):
    return np.concatenate([np.arange((8 * j + c) * 128, (8 * j + c + 1) * 128) for j in range(NJ)])


def perm_w_in(w_in):
    oo = _orig_offsets()
    cols = []
    for k in ORD_BF + ORD_F:
        cols.append(np.arange(oo[k], oo[k] + SZ[k]))
    return np.ascontiguousarray(w_in[:, np.concatenate(cols)])


def launch_A(x, w_in):
    wp = perm_w_in(w_in)
    nc = build_A()
    in_maps = []
    for c in range(NCORES):
        tok = own_tokens(c)
        cos, sin = rope_tables(tok, 32)
        cs128 = np.stack([np.repeat(cos[:, None, :], 4, 1), np.repeat(sin[:, None, :], 4, 1)], 1)
        cos, sin = rope_tables(tok, 16)
        cs64 = np.stack([np.repeat(cos[:, None, :], 8, 1), np.repeat(sin[:, None, :], 8, 1)], 1)
        in_maps.append(dict(xT=np.ascontiguousarray(x[tok].T), w=wp,
                            cs128=np.ascontiguousarray(cs128, dtype=np.float32),
                            cs64=np.ascontiguousarray(cs64, dtype=np.float32)))
    res = run_spmd(nc, in_maps)
    return [(r["pbf"], r["pf"]) for r in res]


BIS_R = 16.0


def build_B(s=S, nsel=256, nheads=8, nbis=26, part="fox", nc=None, ctx=None, sfx=""):
    fox = part == "fox"
    dsa = part == "dsa"
    nb = s // 128
    nq = s // NCORES
    nj = nq // 128
    scale = 128 ** -0.5
    idx_scale = (16 ** -0.5) * (64 ** -0.5)
    if nc is None:
        nc = bass.Bass("TRN2", target_bir_lowering=False)

    def din(name, shape, dt):
        return nc.dram_tensor(name, list(shape), dt, kind="ExternalInput").ap()
    if fox:
        aqT = din("aqT", [128, 8, nq], BF16)
        akT = din("akT", [8, 128, s], BF16)
        av = din("av", [8, 128, nb, 128], BF16)
        af = din("af", [128, nb, 8], F32)
        bfg = din("bfg", [128, 8], F32)
        dfox = din("dfox", [128, 8, 128], BF16)
        ind = din("ind", [128, nj, nb, 8], F32)
        tri = din("tri", [128, 128], F32)
    if dsa:
        iqT = din("iqT", [128, 8, nq], BF16)
        ikT2 = din("ikT2", [128, s], BF16)
        iw = din("iw", [128, nj, 16], F32)
        bqT = din("bqT", [128, nj, 8, 128], BF16)
        bkT = din("bkT", [128, 2, s], BF16)
        bv = din("bv", [128, nb, 2, 128], BF16)
        dadm = din("dadm", [128, 8, 128], F32)
    ident4 = din("ident4" + sfx, [128, 512], BF16)
    ab = nc.dram_tensor("ab" + sfx, [nq, 1024], BF16, kind="ExternalOutput").ap()

    P = Prog(nc, ctx)
    banks = [P.ps([128, 512], F32, f"bank{i}") for i in range(8)]
    t_bank = [Tk(f"bank{i}", excl=True) for i in range(8)]

    def load(name, src, shape, dt, eng="sp"):
        t = P.sb(shape, dt, name)
        tk = Tk(name)
        P.dma(eng, t[:], src, writes=[tk])
        return t, tk
    id_sb, t_id = load("id_sb", ident4, [128, 512], BF16)
    if fox:
        aq_sb, t_aq = load("aq_sb", aqT, [128, 8, nq], BF16)
        af_sb, t_af = load("af_sb", af, [128, nb, 8], F32)
        bfg_sb, t_bfg = load("bfg_sb", bfg, [128, 8], F32)
        dfox_sb, t_dfox = load("dfox_sb", dfox, [128, 8, 128], BF16)
        ind_sb, t_ind = load("ind_sb", ind, [128, nj, nb, 8], F32)
        tri_sb, t_tri = load("tri_sb", tri, [128, 128], F32)
    ones_sb = P.sb([128, 128], F32, "ones_sb")
    t_ones = Tk("ones")
    P.op("pool", lambda e: e.memset(ones_sb[:], 1.0), writes=[t_ones])
    if fox:
        ab_sb = P.sb([128, nj, 1024], BF16, "ab_sb")
    t_ab = Tk("ab")

    rec = P.sb([128, 8], F32, "rec")
    t_rec = Tk("rec")
    if fox:
      L = P.sb([128, nb, 8], F32, "L")
      t_L = Tk("L")
      for b in range(nb):
          P.op("dve", lambda e, b=b: e.tensor_tensor(L[:, b, :], af_sb[:, b, :], bfg_sb[:], ALU.add),
               reads=[t_af, t_bfg], writes=[t_L])
      Lf = L[:].rearrange("p b h -> p (b h)")
      P.op("act", lambda e: e.activation(out=Lf, in_=Lf, func=AF.Exp, scale=-1.0), reads=[t_L], writes=[t_L])
      P.op("act", lambda e: e.activation(out=Lf, in_=Lf, func=AF.Ln, bias=1.0), reads=[t_L], writes=[t_L])
      CP = P.sb([128, nb, 8], F32, "CP")
      TOT = P.sb([128, nb, 8], F32, "TOT")
      PRE = P.sb([128, nb, 8], F32, "PRE")
      t_CP, t_TOT, t_PRE = Tk("CP"), Tk("TOT"), Tk("PRE")
      ncol = nb * 8
      for c0 in range(0, ncol, 512):
          cw = min(512, ncol - c0)
          P.op("pe", lambda e, c0=c0, cw=cw: e.matmul(banks[0][:, :cw], tri_sb[:], Lf[:, c0:c0 + cw], start=True, stop=True),
               reads=[t_tri, t_L], writes=[t_bank[0]])
          P.op("dve", lambda e, c0=c0, cw=cw: e.tensor_copy(CP[:].rearrange("p b h -> p (b h)")[:, c0:c0 + cw], banks[0][:, :cw]),
               reads=[t_bank[0]], writes=[t_CP])
          P.op("pe", lambda e, c0=c0, cw=cw: e.matmul(banks[1][:, :cw], ones_sb[:], Lf[:, c0:c0 + cw], start=True, stop=True),
               reads=[t_ones, t_L], writes=[t_bank[1]])
          P.op("dve", lambda e, c0=c0, cw=cw: e.tensor_copy(TOT[:].rearrange("p b h -> p (b h)")[:, c0:c0 + cw], banks[1][:, :cw]),
               reads=[t_bank[1]], writes=[t_TOT])
      P.op("dve", lambda e: e.memset(PRE[:, 0, :], 0.0), writes=[t_PRE])
      for b in range(1, nb):
          P.op("dve", lambda e, b=b: e.tensor_tensor(PRE[:, b, :], PRE[:, b - 1, :], TOT[:, b - 1, :], ALU.add),
               reads=[t_TOT, t_PRE], writes=[t_PRE])
      P.op("dve", lambda e: e.tensor_tensor(CP[:], CP[:], PRE[:], ALU.add), reads=[t_CP, t_PRE], writes=[t_CP])
      cref = P.sb([128, nj, 8], F32, "cref")
      t_cref = Tk("cref")
      tmpi = P.sb([128, nb, 8], F32, "tmpi")
      t_tmpi = Tk("tmpi")
      for j in range(nj):
          P.op("dve", lambda e, j=j: e.tensor_tensor(tmpi[:], TOT[:], ind_sb[:, j, :, :], ALU.mult),
               reads=[t_TOT, t_ind], writes=[t_tmpi])
          P.op("dve", lambda e, j=j: e.tensor_reduce(cref[:, j, :], tmpi[:].rearrange("p b h -> p h b"), AX.X, ALU.add),
               reads=[t_tmpi], writes=[t_cref])
      biasJ = P.sb([128, nj, nb, 8], F32, "biasJ")
      t_bias = Tk("biasJ")
      for j in range(nj):
          nk = NCORES * (j + 1)
          for h in range(8):
              P.op("dve", lambda e, j=j, h=h, nk=nk: e.tensor_scalar(
                  biasJ[:, j, :nk, h], CP[:, :nk, h], cref[:, j, h:h + 1], None, ALU.subtract),
                  reads=[t_CP, t_cref], writes=[t_bias])

      kT = [P.sb([128, s], BF16, f"kT{i}") for i in range(2)]
      t_kT = [Tk(f"kT{i}") for i in range(2)]
      vt = [P.sb([128, nb, 129], BF16, f"vt{i}") for i in range(2)]
      t_vt = [Tk(f"vt{i}") for i in range(2)]
      for i in range(2):
          P.op("pool", lambda e, i=i: e.memset(vt[i][:, :, 128:129], 1.0), writes=[t_vt[i]])
      pT = [P.sb([128, 128], BF16, f"pT{i}") for i in range(4)]
      t_pT = [Tk(f"pT{i}") for i in range(4)]
      ipt = 0
      for h in range(nheads):
          hi = h % 2
          P.dma("sp", kT[hi][:], akT[h], writes=[t_kT[hi]])
          P.dma("sp", vt[hi][:, :, 0:128], av[h], writes=[t_vt[hi]])
          for j in range(nj):
              nk = NCORES * (j + 1)
              ob = 2 + (j % 2)
              SB = (0, 1, 4, 5)
              LA = 2

              def qk(kb, j=j, h=h, hi=hi, nk=nk):
                  sbk = SB[(tile0 + kb) % 4]
                  diag = kb >= nk - NCORES
                  P.op("pe", lambda e, sbk=sbk, diag=diag: e.matmul(
                      banks[sbk][:, :128], kT[hi][:, kb * 128:(kb + 1) * 128], aq_sb[:, h, j * 128:(j + 1) * 128],
                      start=True, stop=not diag), reads=[t_kT[hi], t_aq], writes=[t_bank[sbk]])
                  if diag:
                      P.op("pe", lambda e, sbk=sbk: e.matmul(
                          banks[sbk][:, :128], id_sb[:, :128], dfox_sb[:, kb - (nk - NCORES), :],
                          start=False, stop=True), reads=[t_id, t_dfox], writes=[t_bank[sbk]])
              tile0 = ipt
              for kb in range(min(LA, nk)):
                  qk(kb)
              for kb in range(nk):
                  if kb + LA < nk:
                      qk(kb + LA)
                  sbk = SB[(tile0 + kb) % 4]
                  pi = (tile0 + kb) % 4
                  P.op("act", lambda e, pi=pi, sbk=sbk, j=j, kb=kb, h=h: e.activation(
                      out=pT[pi][:, :128], in_=banks[sbk][:, :128], func=AF.Exp,
                      bias=biasJ[:, j, kb, h:h + 1], scale=scale),
                      reads=[t_bank[sbk], t_bias], writes=[t_pT[pi]])
                  P.op("pe", lambda e, ob=ob, pi=pi, hi=hi, kb=kb, nk=nk: e.matmul(
                      banks[ob][:, :129], pT[pi][:, :128], vt[hi][:, kb, :],
                      start=(kb == 0), stop=(kb == nk - 1)), reads=[t_pT[pi], t_vt[hi]], writes=[t_bank[ob]])
              ipt += nk
              P.op("dve", lambda e, ob=ob: e.reciprocal(rec[:, 0:1], banks[ob][:, 128:129]),
                   reads=[t_bank[ob]], writes=[t_rec])
              P.op("dve", lambda e, ob=ob, j=j, h=h: e.tensor_scalar(
                  ab_sb[:, j, h * 128:(h + 1) * 128], banks[ob][:, :128], rec[:, 0:1], None, ALU.mult),
                  reads=[t_bank[ob], t_rec], writes=[t_ab])

    if dsa:
      ik_sb, t_ik = load("ik_sb", ikT2, [128, s], BF16)
      iw_sb, t_iw = load("iw_sb", iw, [128, nj, 16], F32)
      bk_sb, t_bk = load("bk_sb", bkT, [128, 2, s], BF16)
      dadm_sb, t_dadm = load("dadm_sb", dadm, [128, 8, 128], F32)
      bv_sb = P.sb([128, nb, 2, 129], BF16, "bv_sb")
      t_bv = Tk("bv")
      P.op("pool", lambda e: e.memset(bv_sb[:, :, :, 128:129], 1.0), writes=[t_bv])
      P.dma("sp", bv_sb[:, :, :, 0:128], bv, writes=[t_bv])
      scl = P.sb([128, nj, 16], F32, "scl")
      sgn = P.sb([128, nj, 16], F32, "sgn")
      t_scl, t_sgn = Tk("scl"), Tk("sgn")
      P.op("dve", lambda e: e.tensor_scalar(sgn[:], iw_sb[:], 0.0, 2.0, ALU.is_ge, ALU.mult),
           reads=[t_iw], writes=[t_sgn])
      P.op("dve", lambda e: e.tensor_scalar(sgn[:], sgn[:], -1.0, None, ALU.add), reads=[t_sgn], writes=[t_sgn])
      P.op("dve", lambda e: e.tensor_tensor(scl[:], iw_sb[:], sgn[:], ALU.mult), reads=[t_iw, t_sgn], writes=[t_scl])
      P.op("dve", lambda e: e.tensor_scalar(scl[:], scl[:], idx_scale, None, ALU.mult), reads=[t_scl], writes=[t_scl])
      wsc = P.sb([128, nj, 16], F32, "wsc")
      P.op("dve", lambda e: e.tensor_scalar(wsc[:], iw_sb[:], idx_scale, None, ALU.mult), reads=[t_iw, t_scl], writes=[t_scl])
      score = [P.sb([128, s], F32, f"score{i}") for i in range(2)]
      t_score = [Tk(f"score{i}") for i in range(2)]
      mb = P.sb([128, s], BF16, "mb")
      t_mb = Tk("mb")
      junk = P.sb([128, s], BF16, "junk")
      t_junk = Tk("junk")
      iqs = [P.sb([128, 8, 128], BF16, f"iqs{i}") for i in range(2)]
      t_iqs = [Tk(f"iqs{i}") for i in range(2)]
      bqs = [P.sb([128, 8, 128], BF16, f"bqs{i}") for i in range(2)]
      t_bqs = [Tk(f"bqs{i}") for i in range(2)]
      dg0 = P.sb([128, 16, 128], BF16, "dg0")
      t_dg0 = Tk("dg0")
      dg = [dg0, dg0]
      t_dg = [t_dg0, t_dg0]
      rl = [P.sb([128, 512], BF16, f"rl{i}") for i in range(2)]
      t_rl = [Tk(f"rl{i}") for i in range(2)]
      pT4 = [P.sb([128, 512], BF16, f"pT4{i}") for i in range(3)]
      t_pT4 = [Tk(f"pT4{i}") for i in range(3)]
      outt = [P.sb([128, 1024], BF16, f"outt{i}") for i in range(2)]
      t_outt = [Tk(f"outt{i}") for i in range(2)]
      bisv = [P.sb([128, 8], F32, f"bis{i}") for i in range(2)]
      t_bisv = [Tk(f"bis{i}") for i in range(2)]
      rc = P.sb([128, 4], F32, "rc")
      t_rc = Tk("rc")
      st8 = dict(iz=0, iatt=0)

      def indexer(j):
          ji = j % 2
          sc, tsc = score[ji], t_score[ji]
          nk = NCORES * (j + 1)
          ngrp = nk * 128 // 512
          P.dma("sp", iqs[ji][:], iqT[:, :, j * 128:(j + 1) * 128], writes=[t_iqs[ji]])
          P.dma("sp", bqs[ji][:], bqT[:, j, :, :], writes=[t_bqs[ji]])
          for h in range(16):
              P.op("dve", lambda e, h=h: e.tensor_scalar(dg[ji][:, h, :], id_sb[:, :128], wsc[:, j, h:h + 1], None, ALU.mult),
                   reads=[t_id, t_scl], writes=[t_dg[ji]])
          for kg in range(ngrp):
              def zmm(h, kg=kg):
                  zb = (st8["iz"] + h) % 2
                  pr = (h % 2) * 64
                  P.op("pe", lambda e, zb=zb, pr=pr, h=h, kg=kg: e.matmul(
                      banks[zb][:, :512], iqs[ji][pr:pr + 64, h // 2, :],
                      ik_sb[pr:pr + 64, kg * 512:(kg + 1) * 512], start=True, stop=True),
                      reads=[t_iqs[ji], t_ik], writes=[t_bank[zb]])
              zmm(0)
              for h in range(16):
                  if h + 1 < 16:
                      zmm(h + 1)
                  zb = (st8["iz"] + h) % 2
                  P.op("act", lambda e, zb=zb, h=h: e.activation(
                      out=rl[zb][:], in_=banks[zb][:, :512], func=AF.Relu),
                      reads=[t_bank[zb]], writes=[t_rl[zb]])
                  P.op("pe", lambda e, zb=zb, h=h: e.matmul(
                      banks[6][:, :512], dg[ji][:, h, :], rl[zb][:], start=(h == 0), stop=(h == 15)),
                      reads=[t_dg[ji], t_rl[zb]], writes=[t_bank[6]])
              st8["iz"] += 16
              lastg = kg - (ngrp - 2)
              if lastg >= 0:
                  init = dadm_sb[:, lastg * 4:(lastg + 1) * 4, :].rearrange("p a k -> p (a k)")
                  P.op("dve", lambda e, kg=kg, init=init: e.tensor_tensor(
                      sc[:, kg * 512:(kg + 1) * 512], banks[6][:, :512], init, ALU.add),
                      reads=[t_bank[6], t_dadm], writes=[tsc])
              else:
                  P.op("dve", lambda e, kg=kg: e.tensor_copy(sc[:, kg * 512:(kg + 1) * 512], banks[6][:, :512]),
                       reads=[t_bank[6]], writes=[tsc])
              yield

      def bisection(j):
          ji = j % 2
          sc, tsc = score[ji], t_score[ji]
          bv_, tb = bisv[ji], t_bisv[ji]
          LO, MID, CNT, FL = [bv_[:, i:i + 1] for i in range(4)]
          nkeys = NCORES * (j + 1) * 128
          P.op("dve", lambda e: e.memset(LO, -BIS_R), writes=[tb])
          P.op("dve", lambda e: e.memset(MID, 0.0), writes=[tb])
          for it in range(nbis):
              hw_ = BIS_R / (2.0 ** it)
              P.op("dve", lambda e: e.tensor_scalar(
                  junk[:, :nkeys], sc[:, :nkeys], MID, None, ALU.is_ge, ALU.add, accum_out=CNT),
                  reads=[tsc, tb], writes=[tb, t_junk])
              P.op("dve", lambda e, hw_=hw_: e.tensor_scalar(FL, CNT, nsel - 0.5, hw_, ALU.is_ge, ALU.mult),
                   reads=[tb], writes=[tb])
              P.op("dve", lambda e: e.tensor_tensor(LO, LO, FL, ALU.add), reads=[tb], writes=[tb])
              P.op("dve", lambda e, hw_=hw_: e.tensor_scalar(MID, LO, hw_ * 0.5, None, ALU.add), reads=[tb], writes=[tb])
              yield
          P.op("dve", lambda e: e.tensor_scalar(mb[:, :nkeys], sc[:, :nkeys], LO, NEG, ALU.is_lt, ALU.mult),
               reads=[tsc, tb], writes=[t_mb])

      def attention(j):
          ji = j % 2
          nk = NCORES * (j + 1)
          oi = j % 2
          SB = (4, 5, 7)
          LA = 2
          for g in range(2):
              obs = (2, 3)

              def qk2(kb, g=g):
                  sbk = SB[(st8["iatt"] + kb) % 3]
                  P.op("pe", lambda e, sbk=sbk, g=g, kb=kb: e.matmul(
                      banks[sbk][:, :512], bk_sb[:, g, kb * 128:(kb + 1) * 128],
                      bqs[ji][:, g * 4:(g + 1) * 4, :].rearrange("p a q -> p (a q)"), start=True, stop=False),
                      reads=[t_bk, t_bqs[ji]], writes=[t_bank[sbk]])
                  P.op("pe", lambda e, sbk=sbk, kb=kb: e.matmul(
                      banks[sbk][:, :512], mb[:, kb * 128:(kb + 1) * 128], id_sb[:, :512], start=False, stop=True),
                      reads=[t_mb, t_id], writes=[t_bank[sbk]])
              for kb in range(min(LA, nk)):
                  qk2(kb)
              for kb in range(nk):
                  if kb + LA < nk:
                      qk2(kb + LA)
                  sbk = SB[(st8["iatt"] + kb) % 3]
                  pi = (st8["iatt"] + kb) % 3
                  P.op("act", lambda e, pi=pi, sbk=sbk: e.activation(
                      out=pT4[pi][:], in_=banks[sbk][:, :512], func=AF.Exp, scale=scale),
                      reads=[t_bank[sbk]], writes=[t_pT4[pi]])
                  for hh in range(4):
                      ob = obs[hh // 2]
                      c0 = (hh % 2) * 129
                      P.op("pe", lambda e, ob=ob, c0=c0, pi=pi, hh=hh, kb=kb, g=g: e.matmul(
                          banks[ob][:, c0:c0 + 129], pT4[pi][:, hh * 128:(hh + 1) * 128], bv_sb[:, kb, g, :],
                          start=(kb == 0 and hh % 2 == 0), stop=(kb == nk - 1), skip_group_check=True),
                          reads=[t_pT4[pi], t_bv], writes=[t_bank[ob]])
              st8["iatt"] += nk
              for hh in range(4):
                  ob = obs[hh // 2]
                  c0 = (hh % 2) * 129
                  head = g * 4 + hh
                  P.op("act", lambda e, ob=ob, c0=c0: e.activation(out=rc[:, 0:1], in_=banks[ob][:, c0 + 128:c0 + 129], func=AF.Ln),
                       reads=[t_bank[ob]], writes=[t_rc])
                  P.op("act", lambda e: e.activation(out=rc[:, 1:2], in_=rc[:, 0:1], func=AF.Exp, scale=-1.0),
                       reads=[t_rc], writes=[t_rc])
                  P.op("act", lambda e, ob=ob, c0=c0, head=head: e.activation(
                      out=outt[oi][:, head * 128:(head + 1) * 128], in_=banks[ob][:, c0:c0 + 128], func=AF.Copy, scale=rc[:, 1:2]),
                      reads=[t_bank[ob], t_rc], writes=[t_outt[oi]])
          P.dma("sp", ab[j * 128:(j + 1) * 128, :], outt[oi][:], reads=[t_outt[oi]], writes=[Tk()], chan=t_outt[oi])

      order = list(range(nj))
      for _ in indexer(order[0]):
          pass
      for oi_, j in enumerate(order):
          bs = bisection(j)
          if oi_ + 1 < nj:
              jn = order[oi_ + 1]
              ng = NCORES * (jn + 1) * 128 // 512
              per = -(-nbis // ng)
              for _ in indexer(jn):
                  for _i in range(per):
                      next(bs, None)
          for _ in bs:
              pass
          attention(j)
    if fox:
        for j in range(nj):
            P.dma("sp", ab[j * 128:(j + 1) * 128, :], ab_sb[:, j, :], reads=[t_ab], writes=[Tk()], chan=t_ab)
    P.emit()
    return nc


def bf(a):
    return np.ascontiguousarray(a).astype(NPBF)


def prep_B(c, t, s=S):
    nb = s // 128
    nq = s // NCORES
    nj = nq // 128
    tok = np.concatenate([np.arange((NCORES * j + c) * 128, (NCORES * j + c + 1) * 128) for j in range(nj)])
    m = {}
    m["aqT"] = np.ascontiguousarray(t["aq"][tok].reshape(nq, 8, 128).transpose(2, 1, 0))
    m["akT"] = np.ascontiguousarray(t["ak"].reshape(s, 8, 128).transpose(1, 2, 0))
    m["av"] = np.ascontiguousarray(t["av"].reshape(nb, 128, 8, 128).transpose(2, 1, 0, 3))
    m["af"] = np.ascontiguousarray(t["af"].reshape(nb, 128, 8).transpose(1, 0, 2))
    m["bfg"] = np.ascontiguousarray(np.broadcast_to(t["b_forget"].reshape(1, 8), (128, 8))).astype(np.float32)
    kk = np.arange(8)[None, :, None]
    k = np.arange(128)[:, None, None]
    q = np.arange(128)[None, None, :]
    dfox = np.where(kk < c, 0.0, np.where(kk > c, NEG, np.where(k <= q, 0.0, NEG)))
    m["dfox"] = bf(np.broadcast_to(dfox, (128, 8, 128)))
    qq = np.arange(128)[:, None, None]
    k2 = np.arange(128)[None, None, :]
    dadm = np.where(kk < c, 0.0, np.where(kk > c, -1e30, np.where(k2 // 64 <= qq // 64, 0.0, -1e30)))
    m["dadm"] = np.ascontiguousarray(np.broadcast_to(dadm, (128, 8, 128))).astype(np.float32)
    ind = (np.arange(nb)[None, :] < (NCORES * np.arange(nj)[:, None] + c)).astype(np.float32)
    m["ind"] = np.ascontiguousarray(np.broadcast_to(ind[None, :, :, None], (128, nj, nb, 8))).astype(np.float32)
    m["iqT"] = np.ascontiguousarray(t["iq"][tok].reshape(nq, 8, 128).transpose(2, 1, 0))
    ikT = t["ik"].T
    m["ikT2"] = np.ascontiguousarray(np.concatenate([ikT, ikT], 0))
    m["iw"] = np.ascontiguousarray(t["iw"][tok].reshape(nj, 128, 16).transpose(1, 0, 2))
    m["bqT"] = np.ascontiguousarray(t["bq"][tok].reshape(nj, 128, 8, 128).transpose(3, 0, 2, 1))
    m["bkT"] = np.ascontiguousarray(t["bk"].reshape(s, 2, 128).transpose(2, 1, 0))
    m["bv"] = np.ascontiguousarray(t["bv"].reshape(nb, 128, 2, 128).transpose(1, 0, 2, 3))
    m["ident4"] = bf(np.tile(np.eye(128, dtype=np.float32), (1, 4)))
    m["tri"] = np.triu(np.ones((128, 128), np.float32))
    return m


def emit_ln(P, x, t_x, g_sb, b_sb, t_gb, st, mv, t_st, eps=1e-5):
    for k in range(4):
        P.op("dve", lambda e, k=k: e.bn_stats(st[:, k, :], x[:, k * 512:(k + 1) * 512]), reads=[t_x], writes=[t_st])
    P.op("dve", lambda e: e.bn_aggr(mv[:, 0:2], st[:].rearrange("p a b -> p (a b)")), reads=[t_st], writes=[t_st])
    P.op("act", lambda e: e.activation(out=mv[:, 2:3], in_=mv[:, 1:2], func=AF.Sqrt, bias=mv[:, 4:5], scale=1.0),
         reads=[t_st], writes=[t_st])
    P.op("dve", lambda e: e.reciprocal(mv[:, 3:4], mv[:, 2:3]), reads=[t_st], writes=[t_st])
    P.op("dve", lambda e: e.tensor_scalar(x[:], x[:], mv[:, 0:1], mv[:, 3:4], ALU.subtract, ALU.mult),
         reads=[t_x, t_st], writes=[t_x])
    P.op("dve", lambda e: e.tensor_tensor(x[:], x[:], g_sb[:], ALU.mult), reads=[t_x, t_gb], writes=[t_x])
    P.op("dve", lambda e: e.tensor_tensor(x[:], x[:], b_sb[:], ALU.add), reads=[t_x, t_gb], writes=[t_x])


def build_C(nq=NQ, alpha=2.0 ** 0.25):
    nblk = nq // 128
    ntg = max(nq // 512, 1)
    tgw = min(512, nq)
    nc = bass.Bass("TRN2", target_bir_lowering=False)

    def din(name, shape, dt):
        return nc.dram_tensor(name, list(shape), dt, kind="ExternalInput").ap()
    abT = din("abT", [128, 16, nq], BF16)
    gaT = din("gaT", [16, 128, nq], F32)
    gbT = din("gbT", [16, 128, nq], F32)
    wa = din("wa", [1024, 2048], F32)
    wb = din("wb", [1024, 2048], F32)
    wo = din("wo", [2048, 2048], F32)
    xq = din("xq", [nq, 2048], F32)
    lng = din("lng", [128, 2048], F32)
    lnb = din("lnb", [128, 2048], F32)
    wr = din("wr", [2048, 32], F32)
    br = din("br", [128, 32], F32)
    ident = din("ident", [128, 128], F32)
    h_out = nc.dram_tensor("h", [nq, 2048], F32, kind="ExternalOutput").ap()
    g_out = nc.dram_tensor("G", [nq, 32], F32, kind="ExternalOutput").ap()
    P = Prog(nc)
    banks = [P.ps([128, 512], F32, f"bank{i}") for i in range(8)]
    t_bank = [Tk(f"bank{i}", excl=True) for i in range(8)]

    def load(name, src, shape, dt, eng="sp"):
        t = P.sb(shape, dt, name)
        tk = Tk(name)
        P.dma(eng, t[:], src, writes=[tk])
        return t, tk
    ab_sb, t_abT = load("abT_sb", abT, [128, 16, nq], BF16)
    W_sb = P.sb([128, 16, 2048], BF16, "W_sb")
    t_wa, t_wb = Tk("wa"), Tk("wb")
    P.dma("pool", W_sb[:, 0:8, :], wa.rearrange("(kt p) n -> p kt n", p=128), writes=[t_wa])
    P.dma("pool", W_sb[:, 8:16, :], wb.rearrange("(kt p) n -> p kt n", p=128), writes=[t_wb])
    wa_sb = W_sb[:, 0:8, :]
    wb_sb = W_sb[:, 8:16, :]
    mT = P.sb([128, 16, nq], BF16, "mT")
    t_mT = Tk("mT")
    gsb = [P.sb([128, 2, nq], F32, f"gsb{i}") for i in range(2)]
    t_g = [Tk(f"g{i}") for i in range(2)]
    m12 = [[P.sb([128, 512], F32, f"m12{p_}_{i}") for i in range(2)] for p_ in range(2)]
    t_m12 = [Tk("m12a"), Tk("m12b")]
    itc = [0]
    for c in range(16):
        gi = c % 2
        P.dma("sp", gsb[gi][:, 0, :], gaT[c], writes=[t_g[gi]])
        P.dma("sp", gsb[gi][:, 1, :], gbT[c], writes=[t_g[gi]])
        P.op("act", lambda e, gi=gi: e.activation(out=gsb[gi][:], in_=gsb[gi][:], func=AF.Sigmoid),
             reads=[t_g[gi]], writes=[t_g[gi]])
        for tg in range(ntg):
            ts_ = slice(tg * tgw, (tg + 1) * tgw)
            pp = itc[0] % 2
            itc[0] += 1
            ba, bb = (0, 1) if pp == 0 else (2, 3)
            ma, mb_ = m12[pp]
            for kt in range(8):
                P.op("pe", lambda e, kt=kt, c=c, ts_=ts_, ba=ba: e.matmul(
                    banks[ba][:, :tgw], wa_sb[:, kt, c * 128:(c + 1) * 128], ab_sb[:, kt, ts_],
                    start=(kt == 0), stop=(kt == 7)), reads=[t_wa, t_abT], writes=[t_bank[ba]])
            for kt in range(8):
                P.op("pe", lambda e, kt=kt, c=c, ts_=ts_, bb=bb: e.matmul(
                    banks[bb][:, :tgw], wb_sb[:, kt, c * 128:(c + 1) * 128], ab_sb[:, 8 + kt, ts_],
                    start=(kt == 0), stop=(kt == 7)), reads=[t_wb, t_abT], writes=[t_bank[bb]])
            P.op("dve", lambda e, gi=gi, ts_=ts_, ba=ba, ma=ma: e.tensor_tensor(ma[:, :tgw], banks[ba][:, :tgw], gsb[gi][:, 0, ts_], ALU.mult),
                 reads=[t_bank[ba], t_g[gi]], writes=[t_m12[pp]])
            P.op("dve", lambda e, gi=gi, ts_=ts_, bb=bb, mb_=mb_: e.tensor_tensor(mb_[:, :tgw], banks[bb][:, :tgw], gsb[gi][:, 1, ts_], ALU.mult),
                 reads=[t_bank[bb], t_g[gi]], writes=[t_m12[pp]])
            P.op("dve", lambda e, c=c, ts_=ts_, ma=ma, mb_=mb_: e.tensor_tensor(mT[:, c, ts_], ma[:, :tgw], mb_[:, :tgw], ALU.add),
                 reads=[t_m12[pp]], writes=[t_mT])
    wo_sb = W_sb
    wov = wo.rearrange("(kt p) n -> p kt n", p=128)
    P.dma("pool", W_sb[:, 0:8, :], wov[:, 0:8, :], writes=[t_wa])
    P.dma("pool", W_sb[:, 8:16, :], wov[:, 8:16, :], writes=[t_wb])
    lng_sb, t_lng = load("lng_sb", lng, [128, 2048], F32)
    lnb_sb, t_lnb = load("lnb_sb", lnb, [128, 2048], F32)
    t_gb = Tk("gb")
    P.op("pool", lambda e: e.engine_nop(), reads=[t_lng, t_lnb], writes=[t_gb])
    wr_sb, t_wr = load("wr_sb", wr.rearrange("(kt p) n -> p kt n", p=128), [128, 16, 32], F32)
    br_sb, t_br = load("br_sb", br, [128, 32], F32)
    id_sb, t_id = load("id_sb", ident, [128, 128], F32)
    xs0 = P.sb([128, 2048], F32, "xs0")
    t_xs0 = Tk("xs0")
    xs = [xs0, xs0]
    t_xs = [t_xs0, t_xs0]
    hs = [P.sb([128, 2048], F32, f"hs{i}") for i in range(2)]
    t_hs = [Tk(f"hs{i}") for i in range(2)]
    st = P.sb([128, 4, 6], F32, "st")
    mv = P.sb([128, 8], F32, "mv")
    t_st = Tk("st")
    P.op("dve", lambda e: e.memset(mv[:, 4:5], 1e-5), writes=[t_st])
    hT = P.sb([128, 16, 128], F32, "hT")
    t_hT = Tk("hT")
    rt = P.sb([128, 4, 32], F32, "rt")
    m8 = P.sb([128, 16], F32, "m8")
    t_rt = Tk("rt")
    for b in range(nblk):
        i2 = b % 2
        P.dma("sp", xs[i2][:], xq[b * 128:(b + 1) * 128, :], writes=[t_xs[i2]])
        for n in range(4):
            for kt in range(16):
                P.op("pe", lambda e, n=n, kt=kt, b=b: e.matmul(
                    banks[4 + n][:, :512], mT[:, kt, b * 128:(b + 1) * 128], wo_sb[:, kt, n * 512:(n + 1) * 512],
                    start=(kt == 0), stop=(kt == 15)), reads=[t_mT, t_wa, t_wb], writes=[t_bank[4 + n]])
            P.op("dve", lambda e, n=n, i2=i2: e.scalar_tensor_tensor(
                out=hs[i2][:, n * 512:(n + 1) * 512], in0=xs[i2][:, n * 512:(n + 1) * 512], scalar=float(alpha),
                in1=banks[4 + n][:, :512], op0=ALU.mult, op1=ALU.add),
                reads=[t_xs[i2], t_bank[4 + n]], writes=[t_hs[i2]])
        emit_ln(P, hs[i2], t_hs[i2], lng_sb, lnb_sb, t_gb, st, mv, t_st)
        P.dma("sp", h_out[b * 128:(b + 1) * 128, :], hs[i2][:], reads=[t_hs[i2]], writes=[Tk()], chan=t_hs[i2])
        for kt in range(16):
            bk = kt % 2
            P.op("pe", lambda e, kt=kt, bk=bk, i2=i2: e.transpose(banks[bk][:, :128], hs[i2][:, kt * 128:(kt + 1) * 128], id_sb[:]),
                 reads=[t_hs[i2], t_id], writes=[t_bank[bk]])
            P.op("act", lambda e, kt=kt, bk=bk: e.copy(hT[:, kt, :], banks[bk][:, :128]), reads=[t_bank[bk]], writes=[t_hT])
        for kt in range(16):
            P.op("pe", lambda e, kt=kt: e.matmul(banks[2][:, :32], hT[:, kt, :], wr_sb[:, kt, :], start=(kt == 0), stop=(kt == 15)),
                 reads=[t_hT, t_wr], writes=[t_bank[2]])
        LG, EX, SEL, GG = rt[:, 0, :], rt[:, 1, :], rt[:, 2, :], rt[:, 3, :]
        P.op("dve", lambda e: e.tensor_tensor(LG, banks[2][:, :32], br_sb[:], ALU.add), reads=[t_bank[2], t_br], writes=[t_rt])
        P.op("dve", lambda e: e.max(m8[:, 0:8], LG), reads=[t_rt], writes=[t_rt])
        P.op("dve", lambda e: e.tensor_scalar(m8[:, 8:9], m8[:, 0:1], -1.0, None, ALU.mult), reads=[t_rt], writes=[t_rt])
        P.op("act", lambda e: e.activation(out=EX, in_=LG, func=AF.Exp, bias=m8[:, 8:9], scale=1.0), reads=[t_rt], writes=[t_rt])
        P.op("dve", lambda e: e.tensor_scalar(SEL, LG, m8[:, 3:4], None, ALU.is_ge), reads=[t_rt], writes=[t_rt])
        P.op("dve", lambda e: e.tensor_tensor(EX, EX, SEL, ALU.mult), reads=[t_rt], writes=[t_rt])
        P.op("dve", lambda e: e.tensor_reduce(m8[:, 9:10], EX, AX.X, ALU.add), reads=[t_rt], writes=[t_rt])
        P.op("dve", lambda e: e.reciprocal(m8[:, 10:11], m8[:, 9:10]), reads=[t_rt], writes=[t_rt])
        P.op("dve", lambda e: e.tensor_scalar(GG, EX, m8[:, 10:11], None, ALU.mult), reads=[t_rt], writes=[t_rt])
        P.dma("sp", g_out[b * 128:(b + 1) * 128, :], GG, reads=[t_rt], writes=[Tk()], chan=t_rt)
    P.emit()
    return nc


def build_D(cap, nexp=4):
    nc = bass.Bass("TRN2", target_bir_lowering=False)

    def din(name, shape, dt):
        return nc.dram_tensor(name, list(shape), dt, kind="ExternalInput").ap()
    xeT = din("xeT", [nexp, 2048, cap], F32)
    wgu = din("wgu", [nexp, 2048, 4096], F32)
    bgu = din("bgu", [nexp, 128, 32], F32)
    wd = din("wd", [nexp, 2048, 2048], F32)
    bd = din("bd", [nexp, 128, 2048], F32)
    y = nc.dram_tensor("y", [nexp, cap, 2048], F32, kind="ExternalOutput").ap()
    P = Prog(nc)
    banks = [P.ps([128, 512], F32, f"bank{i}") for i in range(8)]
    t_bank = [Tk(f"bank{i}", excl=True) for i in range(8)]
    xe = P.sb([128, 16, cap], BF16, "xe")
    t_xe = Tk("xe")
    actT = P.sb([128, 16, cap], BF16, "actT")
    t_act = Tk("actT")
    NW = 3
    wt = [P.sb([128, 16, 512], BF16, f"wt{i}") for i in range(NW)]
    t_wt = [Tk(f"wt{i}") for i in range(NW)]
    bg_sb = P.sb([128, 32], F32, "bg_sb")
    t_bg = Tk("bg")
    bd_sb = P.sb([128, 2048], F32, "bd_sb")
    t_bd = Tk("bd")
    ep = [[P.sb([128, 512], F32, f"ep{p_}_{i}") for i in range(4)] for p_ in range(2)]
    t_ep = [Tk("ep0"), Tk("ep1")]
    itd = [0]
    pend = [None]
    yo = [P.sb([128, 512], F32, f"yo{i}") for i in range(2)]
    t_yo = [Tk(f"yo{i}") for i in range(2)]
    ncg = -(-cap // 512)
    cgw = -(-(cap // ncg) // 64) * 64
    cgs = [(c0, min(cgw, cap - c0)) for c0 in range(0, cap, cgw)]
    iw_ = 0
    iy = 0
    for ex in range(nexp):
        for h in range(2):
            P.dma("pool", xe[:, h * 8:(h + 1) * 8, :], xeT[ex].rearrange("(kt p) c -> p kt c", p=128)[:, h * 8:(h + 1) * 8, :],
                  writes=[t_xe], chan=t_xe)
        P.dma("sp", bg_sb[:], bgu[ex], writes=[t_bg])
        P.dma("sp", bd_sb[:], bd[ex], writes=[t_bd])
        wv = wgu[ex].rearrange("(kt p) c -> p kt c", p=128)
        for q in range(4):
            wi = []
            for half in range(2):
                w_i = iw_ % NW
                iw_ += 1
                c0 = half * 2048 + q * 512
                for hh in range(2):
                    P.dma("pool", wt[w_i][:, hh * 8:(hh + 1) * 8, :], wv[:, hh * 8:(hh + 1) * 8, c0:c0 + 512],
                          writes=[t_wt[w_i]], chan=t_wt[w_i])
                wi.append(w_i)
            for f in range(4):
                ffc = q * 4 + f
                for (c0, cw) in cgs:
                    pp = itd[0] % 2
                    itd[0] += 1
                    bg, bu = (0, 1) if pp == 0 else (4, 5)
                    e0, e1, e2, e3 = ep[pp]
                    for half in range(2):
                        bk = bg if half == 0 else bu
                        for kt in range(16):
                            P.op("pe", lambda e, bk=bk, wi_=wi[half], kt=kt, f=f, c0=c0, cw=cw: e.matmul(
                                banks[bk][:, :cw], wt[wi_][:, kt, f * 128:(f + 1) * 128], xe[:, kt, c0:c0 + cw],
                                start=(kt == 0), stop=(kt == 15)), reads=[t_wt[wi[half]], t_xe], writes=[t_bank[bk]])
                    P.op("dve", lambda e, ffc=ffc, cw=cw, bg=bg, e0=e0: e.tensor_scalar(e0[:, :cw], banks[bg][:, :cw], bg_sb[:, ffc:ffc + 1], 7.0, ALU.add, ALU.min),
                         reads=[t_bank[bg], t_bg], writes=[t_ep[pp]])
                    P.op("dve", lambda e, ffc=ffc, cw=cw, bu=bu, e1=e1: e.tensor_scalar(e1[:, :cw], banks[bu][:, :cw], bg_sb[:, 16 + ffc:17 + ffc], 7.0, ALU.add, ALU.min),
                         reads=[t_bank[bu], t_bg], writes=[t_ep[pp]])
                    P.op("dve", lambda e, cw=cw, e1=e1: e.tensor_scalar(e1[:, :cw], e1[:, :cw], -7.0, 1.0, ALU.max, ALU.add),
                         reads=[t_ep[pp]], writes=[t_ep[pp]])
                    P.op("act", lambda e, cw=cw, e0=e0, e2=e2: e.activation(out=e2[:, :cw], in_=e0[:, :cw], func=AF.Sigmoid, scale=1.702),
                         reads=[t_ep[pp]], writes=[t_ep[pp]])
                    if pend[0] is not None:
                        pend[0]()

                    def part2(pp=pp, cw=cw, ffc=ffc, c0=c0, e0=e0, e1=e1, e2=e2, e3=e3):
                        P.op("dve", lambda e: e.tensor_tensor(e3[:, :cw], e0[:, :cw], e2[:, :cw], ALU.mult),
                             reads=[t_ep[pp]], writes=[t_ep[pp]])
                        P.op("dve", lambda e: e.tensor_tensor(actT[:, ffc, c0:c0 + cw], e3[:, :cw], e1[:, :cw], ALU.mult),
                             reads=[t_ep[pp]], writes=[t_act])
                    pend[0] = part2
        if pend[0] is not None:
            pend[0]()
            pend[0] = None
        wdv = wd[ex].rearrange("(kt p) c -> p kt c", p=128)
        for n in range(4):
            w_i = iw_ % NW
            iw_ += 1
            for hh in range(2):
                P.dma("pool", wt[w_i][:, hh * 8:(hh + 1) * 8, :], wdv[:, hh * 8:(hh + 1) * 8, n * 512:(n + 1) * 512],
                      writes=[t_wt[w_i]], chan=t_wt[w_i])
            for sb_ in range(cap // 128):
                bk = 2 + iy % 2
                yi = iy % 2
                iy += 1
                for kt in range(16):
                    P.op("pe", lambda e, bk=bk, kt=kt, sb_=sb_, w_i=w_i: e.matmul(
                        banks[bk][:, :512], actT[:, kt, sb_ * 128:(sb_ + 1) * 128], wt[w_i][:, kt, :],
                        start=(kt == 0), stop=(kt == 15)), reads=[t_act, t_wt[w_i]], writes=[t_bank[bk]])
                P.op("dve", lambda e, bk=bk, yi=yi, n=n: e.tensor_tensor(yo[yi][:], banks[bk][:, :512], bd_sb[:, n * 512:(n + 1) * 512], ALU.add),
                     reads=[t_bank[bk], t_bd], writes=[t_yo[yi]])
                P.dma("sp", y[ex, sb_ * 128:(sb_ + 1) * 128, n * 512:(n + 1) * 512], yo[yi][:], reads=[t_yo[yi]], writes=[Tk()], chan=t_yo[yi])
    P.emit()
    return nc


def build_E(nq=NQ, alpha=2.0 ** 0.25):
    nblk = nq // 128
    nc = bass.Bass("TRN2", target_bir_lowering=False)

    def din(name, shape, dt):
        return nc.dram_tensor(name, list(shape), dt, kind="ExternalInput").ap()
    y4 = din("y4", [nq, 4, 2048], F32)
    g4 = din("g4", [nq, 4], F32)
    h = din("h", [nq, 2048], F32)
    lng = din("lng", [128, 2048], F32)
    lnb = din("lnb", [128, 2048], F32)
    out = nc.dram_tensor("out", [nq, 2048], F32, kind="ExternalOutput").ap()
    P = Prog(nc)
    lng_sb = P.sb([128, 2048], F32, "lng_sb")
    lnb_sb = P.sb([128, 2048], F32, "lnb_sb")
    t_gb = Tk("gb")
    P.dma("sp", lng_sb[:], lng, writes=[t_gb], chan=t_gb)
    P.dma("sp", lnb_sb[:], lnb, writes=[t_gb], chan=t_gb)
    ys = [P.sb([128, 4, 2048], F32, f"ys{i}") for i in range(2)]
    t_ys = [Tk(f"ys{i}") for i in range(2)]
    hs = [P.sb([128, 2048], F32, f"hs{i}") for i in range(2)]
    t_hs = [Tk(f"hs{i}") for i in range(2)]
    gs = [P.sb([128, 4], F32, f"gs{i}") for i in range(2)]
    t_gs = [Tk(f"gs{i}") for i in range(2)]
    st = P.sb([128, 4, 6], F32, "st")
    mv = P.sb([128, 8], F32, "mv")
    t_st = Tk("st")
    P.op("dve", lambda e: e.memset(mv[:, 4:5], 1e-5), writes=[t_st])
    for b in range(nblk):
        i = b % 2
        P.dma("sp", ys[i][:], y4[b * 128:(b + 1) * 128], writes=[t_ys[i]])
        P.dma("sp", hs[i][:], h[b * 128:(b + 1) * 128, :], writes=[t_hs[i]])
        P.dma("sp", gs[i][:], g4[b * 128:(b + 1) * 128, :], writes=[t_gs[i]])
        P.op("act", lambda e, i=i: e.mul(hs[i][:], hs[i][:], float(alpha)), reads=[t_hs[i]], writes=[t_hs[i]])
        for k in range(4):
            P.op("dve", lambda e, i=i, k=k: e.scalar_tensor_tensor(
                out=hs[i][:], in0=ys[i][:, k, :], scalar=gs[i][:, k:k + 1], in1=hs[i][:], op0=ALU.mult, op1=ALU.add),
                reads=[t_ys[i], t_gs[i], t_hs[i]], writes=[t_hs[i]])
        emit_ln(P, hs[i], t_hs[i], lng_sb, lnb_sb, t_gb, st, mv, t_st)
        P.dma("sp", out[b * 128:(b + 1) * 128, :], hs[i][:], reads=[t_hs[i]], writes=[Tk()], chan=t_hs[i])
    P.emit()
    return nc


def build_B_fused(s=S):
    nc = bass.Bass("TRN2", target_bir_lowering=False)
    ctx = Ctx(nc)
    build_B(s=s, part="fox", nc=nc, ctx=ctx, sfx="A")
    build_B(s=s, part="dsa", nc=nc, ctx=ctx, sfx="B")
    return nc


FOX_KEYS = ("aqT", "akT", "av", "af", "bfg", "dfox", "ind", "tri", "ident4")
DSA_KEYS = ("iqT", "ikT2", "iw", "bqT", "bkT", "bv", "dadm", "ident4")


def scatter_rows(parts, width, dtype):
    full = np.empty((S, width), dtype)
    for c in range(NCORES):
        full[own_tokens(c)] = parts[c]
    return full


def kernel(x, w_in, b_forget, w_branch_a, w_branch_b, w_out, ln1_g, ln1_b, w_router, b_router,
           w_gate_up, b_gate_up, w_down, b_down, ln2_g, ln2_b):
    x2 = np.asarray(x, np.float32)[0]
    resA = launch_A(x2, np.asarray(w_in, np.float32)[0])
    pbf = scatter_rows([r[0] for r in resA], NBF, NPBF)
    pf = scatter_rows([r[1] for r in resA], NF, np.float32)
    del resA
    no = _new_offsets()
    t = {k: pbf[:, no[k]:no[k] + SZ[k]] for k in ORD_BF}
    t["af"] = pf[:, no["af"]:no["af"] + 8]
    t["iw"] = pf[:, no["iw"]:no["iw"] + 16]
    t["b_forget"] = np.asarray(b_forget, np.float32)[0]
    ga = pf[:, no["ga"]:no["ga"] + 2048]
    gb = pf[:, no["gb"]:no["gb"] + 2048]
    maps = [prep_B(c, t) for c in range(NCORES)]
    mapsB = []
    for m in maps:
        d = {k: m[k] for k in FOX_KEYS + DSA_KEYS if k != "ident4"}
        d["ident4A"] = m["ident4"]
        d["ident4B"] = m["ident4"]
        mapsB.append(d)
    resB = run_spmd(build_B_fused(), mapsB)
    a_parts = [r["abA"] for r in resB]
    b_parts = [r["abB"] for r in resB]
    del maps, mapsB, resB
    eye = np.eye(128, dtype=np.float32)
    lng1 = np.ascontiguousarray(np.broadcast_to(np.asarray(ln1_g, np.float32)[0], (128, 2048)))
    lnb1 = np.ascontiguousarray(np.broadcast_to(np.asarray(ln1_b, np.float32)[0], (128, 2048)))
    mapsC = []
    for c in range(NCORES):
        tok = own_tokens(c)
        abc = np.concatenate([a_parts[c], b_parts[c]], 1)
        mapsC.append(dict(
            abT=np.ascontiguousarray(abc.reshape(NQ, 16, 128).transpose(2, 1, 0)),
            gaT=np.ascontiguousarray(ga[tok].T.reshape(16, 128, NQ)),
            gbT=np.ascontiguousarray(gb[tok].T.reshape(16, 128, NQ)),
            wa=np.asarray(w_branch_a, np.float32)[0], wb=np.asarray(w_branch_b, np.float32)[0],
            wo=np.asarray(w_out, np.float32)[0], xq=np.ascontiguousarray(x2[tok]), lng=lng1, lnb=lnb1,
            wr=np.asarray(w_router, np.float32)[0],
            br=np.ascontiguousarray(np.broadcast_to(np.asarray(b_router, np.float32)[0], (128, 32))), ident=eye))
    resC = run_spmd(build_C(), mapsC)
    h_full = scatter_rows([r["h"] for r in resC], 2048, np.float32)
    G_full = scatter_rows([r["G"] for r in resC], 32, np.float32)
    del mapsC, resC
    sel = G_full > 0
    pos = np.cumsum(sel, 0) - 1
    counts = sel.sum(0)
    cap = int(max(128, -(-int(counts.max()) // 128) * 128))
    wgu = np.asarray(w_gate_up)[0]
    wdn = np.asarray(w_down)[0]
    bgu = np.asarray(b_gate_up, np.float32)[0]
    bdn = np.asarray(b_down, np.float32)[0]
    mapsD = []
    for c in range(NCORES):
        xeT = np.zeros((4, 2048, cap), np.float32)
        for i in range(4):
            e = 4 * c + i
            idx = np.nonzero(sel[:, e])[0]
            xeT[i, :, :len(idx)] = h_full[idx].T
        mapsD.append(dict(
            xeT=xeT, wgu=np.ascontiguousarray(wgu[4 * c:4 * c + 4]),
            bgu=np.ascontiguousarray(bgu[4 * c:4 * c + 4].reshape(4, 32, 128).transpose(0, 2, 1)),
            wd=np.ascontiguousarray(wdn[4 * c:4 * c + 4]),
            bd=np.ascontiguousarray(np.broadcast_to(bdn[4 * c:4 * c + 4][:, None, :], (4, 128, 2048)))))
    resD2 = run_spmd(build_D(cap), mapsD)
    Y = np.concatenate([r["y"] for r in resD2], 0)
    del mapsD, resD2
    ek = np.argsort(~sel, axis=1, kind="stable")[:, :4]
    ar = np.arange(S)
    lng2 = np.ascontiguousarray(np.broadcast_to(np.asarray(ln2_g, np.float32)[0], (128, 2048)))
    lnb2 = np.ascontiguousarray(np.broadcast_to(np.asarray(ln2_b, np.float32)[0], (128, 2048)))
    mapsE = []
    for c in range(NCORES):
        tok = own_tokens(c)
        y4 = np.empty((NQ, 4, 2048), np.float32)
        g4 = np.empty((NQ, 4), np.float32)
        for k in range(4):
            e_k = ek[tok, k]
            valid = sel[tok, e_k]
            p_k = np.where(valid, pos[tok, e_k], 0)
            y4[:, k] = Y[e_k, p_k]
            g4[:, k] = G_full[tok, e_k]
        mapsE.append(dict(y4=y4, g4=g4, h=np.ascontiguousarray(h_full[tok]), lng=lng2, lnb=lnb2))
    resE = run_spmd(build_E(), mapsE)
    out = scatter_rows([r["out"] for r in resE], 2048, np.float32)
    return out[None]
```
